# Optimizing a Trainium2 kernel written in Bass

```python
import jax, jax.numpy as jnp
from jax import lax
import numpy as np

D_MODEL = 1024
BATCH = 4
SEQ = 4096
DEPTH = 1

CONV_WIDTH = 512
CONV_K = 3
RET_HEADS = 8
RET_DK = 64
RET_DV = 128
RET_CHUNK = 128
QK_WIDTH = RET_HEADS * RET_DK
V_WIDTH = RET_HEADS * RET_DV
ROPE_BASE = 10000.0
IN_SPLITS = (CONV_WIDTH, CONV_WIDTH, CONV_WIDTH, QK_WIDTH, QK_WIDTH, V_WIDTH, V_WIDTH, D_MODEL, D_MODEL)
IN_WIDTH = sum(IN_SPLITS)
SPLIT_POINTS = [sum(IN_SPLITS[:i + 1]) for i in range(len(IN_SPLITS) - 1)]
MEM_LEN = 256
XA_HEADS = 4
XA_HEAD_DIM = D_MODEL // XA_HEADS
N_GROUPS = 4
EXPERTS_PER_GROUP = 8
N_EXPERTS = N_GROUPS * EXPERTS_PER_GROUP
TOP_K = 2
EXPERT_HIDDEN = D_MODEL // 2
EXPERT_BLOCK = 128
EPS = 1e-6

kernel_name = "hybrid_conv_retention_xattn_hmoe"


def rmsnorm(x, g):
    xf = x.astype(jnp.float32)
    y = xf * lax.rsqrt(jnp.mean(xf * xf, axis=-1, keepdims=True) + EPS)
    return (y * g.astype(jnp.float32)).astype(x.dtype)


def short_conv_branch(xin, bg, cg, conv_w, w_out):
    u = cg * xin
    rhs = conv_w[:, None, :].astype(u.dtype)
    c = lax.conv_general_dilated(u, rhs, window_strides=(1,), padding=((CONV_K - 1, 0),),
                                 dimension_numbers=('NWC', 'WIO', 'NWC'),
                                 feature_group_count=CONV_WIDTH)
    return (bg * c) @ w_out


def rotary(t, cos, sin):
    t1, t2 = jnp.split(t.astype(jnp.float32), 2, axis=-1)
    return jnp.concatenate([t1 * cos - t2 * sin, t1 * sin + t2 * cos], axis=-1).astype(t.dtype)


def retention_branch(q, k, v, g, w_out):
    b, s, _ = q.shape
    n = s // RET_CHUNK
    pos = jnp.arange(s, dtype=jnp.float32)
    inv_freq = ROPE_BASE ** (-jnp.arange(0, RET_DK, 2, dtype=jnp.float32) / RET_DK)
    ang = pos[:, None] * inv_freq[None, :]
    cos, sin = jnp.cos(ang)[:, None, :], jnp.sin(ang)[:, None, :]
    q = rotary(q.reshape(b, s, RET_HEADS, RET_DK), cos, sin)
    k = rotary(k.reshape(b, s, RET_HEADS, RET_DK), cos, sin) * (RET_DK ** -0.5)
    v = v.reshape(b, s, RET_HEADS, RET_DV)

    def to_chunks(t, d):
        return t.reshape(b, n, RET_CHUNK, RET_HEADS, d).transpose(0, 3, 1, 2, 4)

    qc, kc, vc = to_chunks(q, RET_DK), to_chunks(k, RET_DK), to_chunks(v, RET_DV)

    log_g = jnp.log(1.0 - jnp.power(2.0, -5.0 - jnp.arange(RET_HEADS, dtype=jnp.float32)))
    idx = jnp.arange(RET_CHUNK, dtype=jnp.float32)
    diff = idx[:, None] - idx[None, :]
    decay = jnp.where(diff >= 0, jnp.exp(jnp.maximum(diff, 0.0)[None] * log_g[:, None, None]), 0.0)
    zeta = jnp.exp((RET_CHUNK - 1 - idx)[None, :] * log_g[:, None])
    xi = jnp.exp((idx + 1)[None, :] * log_g[:, None])
    chunk_decay = jnp.exp(RET_CHUNK * log_g)

    scores = jnp.einsum('bhnqd,bhnkd->bhnqk', qc, kc) * decay[None, :, None]
    inner = jnp.einsum('bhnqk,bhnke->bhnqe', scores, vc)
    kv = jnp.einsum('bhnkd,bhnke->nbhde', kc * zeta[None, :, None, :, None], vc)

    def step(state, kv_n):
        return chunk_decay[None, :, None, None] * state + kv_n, state

    _, state_prev = lax.scan(step, jnp.zeros_like(kv[0]), kv)
    cross = jnp.einsum('bhnqd,nbhde->bhnqe', qc, state_prev) * xi[None, :, None, :, None]

    o = (inner + cross).astype(jnp.float32).transpose(0, 2, 3, 1, 4).reshape(b, s, RET_HEADS, RET_DV)
    mu = jnp.mean(o, axis=-1, keepdims=True)
    var = jnp.mean(jnp.square(o - mu), axis=-1, keepdims=True)
    o = ((o - mu) * lax.rsqrt(var + EPS)).reshape(b, s, V_WIDTH).astype(g.dtype)
    return (jax.nn.silu(g) * o) @ w_out


def cross_attn(hn, mn, w_q, w_kv, w_o):
    b, s, d = hn.shape
    m = mn.shape[1]
    q = (hn @ w_q).reshape(b, s, XA_HEADS, XA_HEAD_DIM)
    k, v = jnp.split(mn @ w_kv, 2, axis=-1)
    k = k.reshape(b, m, XA_HEADS, XA_HEAD_DIM)
    v = v.reshape(b, m, XA_HEADS, XA_HEAD_DIM)
    scores = jnp.einsum('bshd,bmhd->bhsm', q, k).astype(jnp.float32) * (XA_HEAD_DIM ** -0.5)
    p = jax.nn.softmax(scores, axis=-1).astype(v.dtype)
    o = jnp.einsum('bhsm,bmhd->bshd', p, v).reshape(b, s, d)
    return o @ w_o


def hier_moe(xn, w_group, b_group, w_router, b_router, w_gate, w_up, w_down):
    b, s, d = xn.shape
    t = b * s
    xf = xn.reshape(t, d)
    grp_prob = jax.nn.softmax((xf @ w_group + b_group).astype(jnp.float32), axis=-1)
    p_g, g_idx = lax.top_k(grp_prob, 1)
    exp_logits = (xf @ w_router + b_router).astype(jnp.float32).reshape(t, N_GROUPS, EXPERTS_PER_GROUP)
    sel = jnp.take_along_axis(exp_logits, g_idx[:, :, None], axis=1)[:, 0]
    p_e, e_local = lax.top_k(jax.nn.softmax(sel, axis=-1), TOP_K)
    weights = p_g * p_e / jnp.sum(p_e, axis=-1, keepdims=True)
    expert_id = g_idx * EXPERTS_PER_GROUP + e_local

    m = t * TOP_K
    flat_e = expert_id.reshape(-1)
    flat_w = weights.reshape(-1)
    flat_tok = jnp.repeat(jnp.arange(t, dtype=jnp.int32), TOP_K)
    order = jnp.argsort(flat_e)
    sorted_e = flat_e[order]
    counts = jnp.bincount(flat_e, length=N_EXPERTS)
    starts = jnp.cumsum(counts) - counts
    padded = (counts + EXPERT_BLOCK - 1) // EXPERT_BLOCK * EXPERT_BLOCK
    padded_ends = jnp.cumsum(padded)
    padded_starts = padded_ends - padded
    dest = padded_starts[sorted_e] + jnp.arange(m) - starts[sorted_e]
    n_rows = -(-(m + N_EXPERTS * (EXPERT_BLOCK - 1)) // EXPERT_BLOCK) * EXPERT_BLOCK
    n_blocks = n_rows // EXPERT_BLOCK
    row_tok = jnp.zeros((n_rows,), jnp.int32).at[dest].set(flat_tok[order])
    row_w = jnp.zeros((n_rows,), jnp.float32).at[dest].set(flat_w[order])
    block_e = jnp.minimum(jnp.searchsorted(padded_ends, jnp.arange(n_blocks) * EXPERT_BLOCK, side='right'),
                          N_EXPERTS - 1)
    xs = xf[row_tok].reshape(n_blocks, EXPERT_BLOCK, d)

    def expert_block(args):
        xb, e = args
        hid = jax.nn.silu(xb @ w_gate[e]) * (xb @ w_up[e])
        return hid @ w_down[e]

    ys = lax.map(expert_block, (xs, block_e)).reshape(n_rows, d)
    out = jnp.zeros((t, d), xn.dtype).at[row_tok].add((ys * row_w[:, None]).astype(xn.dtype))
    return out.reshape(b, s, d)


def _w(key, shape, fan_in):
    return jax.random.normal(key, shape, jnp.float32) * (fan_in ** -0.5)


def _gain(key, shape):
    return 1.0 + 0.01 * jax.random.normal(key, shape, jnp.float32)


def setup_inputs(seed: int = 0) -> dict:
    key = jax.random.key(seed)
    ks = jax.random.split(key, 22)
    L, D = DEPTH, D_MODEL
    return {
        "x": jax.random.normal(ks[0], (BATCH, SEQ, D), jnp.float32),
        "mem": jax.random.normal(ks[1], (BATCH, MEM_LEN, D), jnp.float32),
        "mix_norm_g": _gain(ks[2], (L, D)),
        "w_in": _w(ks[3], (L, D, IN_WIDTH), D),
        "conv_w": _w(ks[4], (L, CONV_K, CONV_WIDTH), CONV_K),
        "w_conv_out": _w(ks[5], (L, CONV_WIDTH, D), CONV_WIDTH),
        "w_ret_out": _w(ks[6], (L, V_WIDTH, D), V_WIDTH),
        "w_mix_out": _w(ks[7], (L, D, D), D),
        "xa_norm_g": _gain(ks[8], (L, D)),
        "mem_norm_g": _gain(ks[9], (L, D)),
        "w_xa_q": _w(ks[10], (L, D, D), D),
        "w_xa_kv": _w(ks[11], (L, D, 2 * D), D),
        "w_xa_o": _w(ks[12], (L, D, D), D),
        "moe_norm_g": _gain(ks[13], (L, D)),
        "w_group": _w(ks[14], (L, D, N_GROUPS), D),
        "b_group": 0.01 * jax.random.normal(ks[15], (L, N_GROUPS), jnp.float32),
        "w_router": _w(ks[16], (L, D, N_EXPERTS), D),
        "b_router": 0.01 * jax.random.normal(ks[17], (L, N_EXPERTS), jnp.float32),
        "w_gate": _w(ks[18], (L, N_EXPERTS, D, EXPERT_HIDDEN), D),
        "w_up": _w(ks[19], (L, N_EXPERTS, D, EXPERT_HIDDEN), D),
        "w_down": _w(ks[20], (L, N_EXPERTS, EXPERT_HIDDEN, D), EXPERT_HIDDEN),
        "final_norm_g": _gain(ks[21], (D,)),
    }


def reference(x, mem, mix_norm_g, w_in, conv_w, w_conv_out, w_ret_out, w_mix_out,
              xa_norm_g, mem_norm_g, w_xa_q, w_xa_kv, w_xa_o, moe_norm_g,
              w_group, b_group, w_router, b_router, w_gate, w_up, w_down, final_norm_g):
    h = x
    for l in range(DEPTH):
        xn = rmsnorm(h, mix_norm_g[l])
        xin, bg, cg, q, k, v, g, gate_c, gate_r = jnp.split(xn @ w_in[l], SPLIT_POINTS, axis=-1)
        y_conv = short_conv_branch(xin, bg, cg, conv_w[l], w_conv_out[l])
        y_ret = retention_branch(q, k, v, g, w_ret_out[l])
        merged = jax.nn.sigmoid(gate_c) * y_conv + jax.nn.sigmoid(gate_r) * y_ret
        h = h + merged @ w_mix_out[l]
        h = h + cross_attn(rmsnorm(h, xa_norm_g[l]), rmsnorm(mem, mem_norm_g[l]),
                           w_xa_q[l], w_xa_kv[l], w_xa_o[l])
        h = h + hier_moe(rmsnorm(h, moe_norm_g[l]), w_group[l], b_group[l], w_router[l], b_router[l],
                         w_gate[l], w_up[l], w_down[l])
    return rmsnorm(h, final_norm_g)
```

```python
import math
import numpy as np
from contextlib import ExitStack
import concourse.bass as bass
import concourse.mybir as mybir
from concourse.bass_utils import run_bass_kernel_spmd

F32 = mybir.dt.float32
BF16 = mybir.dt.bfloat16
I32 = mybir.dt.int32
U8 = mybir.dt.uint8
AF = mybir.ActivationFunctionType
ALU = mybir.AluOpType
AX = mybir.AxisListType

D = 1024
NT = 16
TOK = 2048
NE = 32
CAP = 256
CAPR = CAP + 1
EPS = 1e-6
INW = 6656
DBG = False


class Op:
    __slots__ = ("eng", "fn", "reads", "writes", "dma", "key", "idx", "sig", "ev", "deps", "xdeps", "bsize")

    def __init__(self, eng, fn, reads, writes, dma, key):
        self.eng = eng
        self.fn = fn
        self.reads = tuple(reads)
        self.writes = tuple(writes)
        self.dma = dma
        self.key = key
        self.sig = False
        self.ev = None
        self.deps = ()
        self.xdeps = ()
        self.bsize = 1


class Sched:
    ENGS = ("pe", "act", "dve", "pool", "sp")

    def __init__(self):
        self.ops = []
        self.last_eng = {}
        self.last_key = {}

    def add(self, eng, fn, reads=(), writes=(), dma=False, key=None, bsize=1):
        if dma:
            assert key is not None
        op = Op(eng, fn, reads, writes, dma, key)
        op.bsize = bsize
        op.idx = len(self.ops)
        self.ops.append(op)
        if dma:
            self.last_key[key] = op.idx
        elif fn is not None:
            self.last_eng[eng] = op.idx
        return op

    def barrier(self):
        deps = tuple(self.last_eng.values()) + tuple(self.last_key.values())
        for e in self.ENGS:
            op = self.add(e, None)
            op.xdeps = deps

    def resolve(self):
        last_w = {}
        readers = {}
        for op in self.ops:
            deps = set(op.xdeps)
            for r in op.reads:
                w = last_w.get(r)
                if w is not None:
                    deps.add(w)
            for w_ in op.writes:
                w = last_w.get(w_)
                if w is not None:
                    deps.add(w)
                for rd in readers.get(w_, {}).values():
                    deps.add(rd)
            deps.discard(op.idx)
            dl = []
            for d in sorted(deps):
                dop = self.ops[d]
                if dop.fn is None:
                    continue
                if op.eng == "pe" and dop.eng == "pe" and not dop.dma and not op.dma:
                    continue
                dop.sig = True
                dl.append(d)
            op.deps = tuple(dl)
            rk = ("dma", op.idx) if op.dma else op.eng
            for r in op.reads:
                readers.setdefault(r, {})[rk] = op.idx
            for w_ in op.writes:
                last_w[w_] = op.idx
                readers[w_] = {}
        cnt = {e: 0 for e in self.ENGS}
        keycnt = {}
        import os
        if os.environ.get("ALLSIG"):
            for op in self.ops:
                if not op.dma and op.fn is not None and op.eng != "sp":
                    op.sig = True
        for op in self.ops:
            if op.dma:
                keycnt[op.key] = keycnt.get(op.key, 0) + 16
                q = 16 * op.bsize
                op.ev = (("dma", op.key), (keycnt[op.key] + q - 1) // q * q)
            elif op.sig:
                cnt[op.eng] += 1
                op.ev = (("eng", op.eng), cnt[op.eng])
        self.keys = list(keycnt.keys())
        self.cnt = cnt
        return self

    def run_engine(self, eng, eobj, sems):
        waited = {}
        for op in self.ops:
            if op.eng != eng:
                continue
            need = {}
            for d in op.deps:
                sk, val = self.ops[d].ev
                if need.get(sk, 0) < val:
                    need[sk] = val
            for sk, val in need.items():
                if waited.get(sk, 0) >= val:
                    continue
                eobj.wait_ge(sems[sk], val)
                waited[sk] = val
            if op.fn is None:
                continue
            ins = op.fn(eobj)
            if op.dma:
                ins.then_inc(sems[op.ev[0]], 16)
            elif op.sig:
                ins.then_inc(sems[op.ev[0]], 1)


_DTSZ = {F32: 4, BF16: 2, I32: 4, U8: 1}


class Arena:
    def __init__(self, ap, size):
        self.ap = ap
        self.size = size
        self.off = 0
        self.peak = 0
        self.limit = size

    def mark(self):
        return self.off

    def release(self, m):
        self.off = m

    def alloc(self, shape, dt):
        n = 1
        for s in shape[1:]:
            n *= s
        nbytes = n * _DTSZ[dt]
        off = (self.off + 31) // 32 * 32
        assert off + nbytes <= self.limit, ("SBUF arena overflow", off, nbytes, self.limit)
        self.off = off + nbytes
        self.peak = max(self.peak, self.off)
        v = self.ap[:, off:off + nbytes].bitcast(dt)
        if len(shape) == 3:
            v = v.rearrange("p (a b) -> p a b", a=shape[1])
        elif len(shape) == 4:
            v = v.rearrange("p (a b c) -> p a b c", a=shape[1], b=shape[2])
        return v


def build_program(stop=None):
    nc = bass.Bass("TRN2", target_bir_lowering=False)

    def din(name, shape, dt=F32):
        return nc.dram_tensor(name, list(shape), dt, kind="ExternalInput").ap()

    xc = din("xc", [TOK, D])
    xp = din("xp", [TOK, D])
    memc = din("memc", [256, D])
    w_in = din("w_in", [D, INW])
    conv_wT = din("conv_wT", [512, 3])
    w_conv_out = din("w_conv_out", [512, D])
    w_ret_out = din("w_ret_out", [D, D])
    w_mix_out = din("w_mix_out", [D, D])
    w_xa_q = din("w_xa_q", [D, D])
    w_xa_kv = din("w_xa_kv", [D, 2 * D])
    w_xa_o = din("w_xa_o", [D, D])
    g_mix = din("g_mix", [D])
    g_xa = din("g_xa", [D])
    g_mem = din("g_mem", [D])
    g_moe = din("g_moe", [D])
    g_fin = din("g_fin", [D])
    w_rt = din("w_rt", [D, 36])
    b_rt = din("b_rt", [36])
    w_gate = din("w_gate", [NE, D, 512])
    w_up = din("w_up", [NE, D, 512])
    w_down = din("w_down", [NE, 512, D])
    c_bf = din("c_bf", [128, 384])
    c_cs_own = din("c_cs_own", [128, 2, NT, 32])
    c_cs_pre = din("c_cs_pre", [128, 2, NT, 32])
    c_gq = din("c_gq", [128, 4, 128])
    c_gk = din("c_gk", [128, 4, 128])
    c_zt = din("c_zt", [128, 8])
    c_ct = din("c_ct", [128, 4, 128])
    c_mask = din("c_mask", [128, 4, 128])
    c_eb = din("c_eb", [128, 2 * NE])
    out = nc.dram_tensor("out", [TOK, D], F32, kind="ExternalOutput").ap()
    XG = nc.dram_tensor("xg_scr", [NE * CAPR, D], BF16, kind="Internal").ap()
    YG = nc.dram_tensor("yg_scr", [NE * CAP, D], BF16, kind="Internal").ap()
    if DBG:
        dbg_m = nc.dram_tensor("dbg_m", [128, 8 * TOK], F32, kind="ExternalOutput").ap()
        dbg_h = nc.dram_tensor("dbg_h", [128, NT * D], F32, kind="ExternalOutput").ap()

    S = Sched()
    es = ExitStack()
    ARENA_BYTES = 207 * 1024
    arena_t = es.enter_context(nc.sbuf_tensor("arena", [128, ARENA_BYTES], U8))
    A = Arena(arena_t, ARENA_BYTES)
    ps_t = es.enter_context(nc.psum_tensor("ps", [128, 4096], F32))

    def bank(i, n=1):
        return ps_t[:, i * 512:(i + n) * 512]

    def bank_bf(i):
        return ps_t[:, i * 512:(i + 1) * 512].bitcast(BF16).rearrange("p (a b) -> p a b", a=8)

    def PR(i):
        return ("ps", i)

    uid = [0]

    def ukey(p):
        uid[0] += 1
        return "%s%d" % (p, uid[0])

    def PE(fn, r, w):
        return S.add("pe", fn, r, w)

    def ACT(fn, r, w):
        return S.add("act", fn, r, w)

    def DVE(fn, r, w):
        return S.add("dve", fn, r, w)

    def POOL(fn, r, w):
        return S.add("pool", fn, r, w)

    def DMA(eng, out_, in_, r, w, key, nb=1):
        return S.add(eng, lambda e: e.dma_start(out=out_, in_=in_), r, w, dma=True, key=key, bsize=nb)

    def mm(out_, lhsT, rhs, start, stop, r, w):
        return PE(lambda e: e.matmul(out_, lhsT=lhsT, rhs=rhs, start=start, stop=stop), r, w)

    def tp(out_, in_, r, w):
        return PE(lambda e: e.transpose(out=out_, in_=in_, identity=ident), r + ["ident"], w)

    def load_w(dst, src, rows_k, res, key):
        for k in range(rows_k):
            DMA("pool", dst[:, k, :], src[k * 128:(k + 1) * 128, :], [], [(res, k)], key, nb=rows_k)

    def wres(res, n):
        return [(res, k) for k in range(n)]

    ident3 = A.alloc([128, 3, 128], BF16)
    ident = ident3[:, 0, :]
    ustrict = ident3[:, 1, :]
    onesb = ident3[:, 2, :]
    DMA("pool", ident3, c_bf.rearrange("p (a b) -> p a b", a=3), [], ["ident"], "c_bf")
    MT_BYTES = 8 * TOK * 2
    mergedT = arena_t[:, ARENA_BYTES - MT_BYTES:ARENA_BYTES].bitcast(BF16).rearrange("p (a b) -> p a b", a=8)
    A.limit = ARENA_BYTES - MT_BYTES
    stat = A.alloc([128, 64], F32)
    wts = A.alloc([128, NT, 2], F32)
    sidx = A.alloc([128, NT, 2], I32)
    gidx = A.alloc([128, NT, 2], I32)
    ssq = stat[:, 0:1]
    std = stat[:, 1:2]
    rstd = stat[:, 2:3]
    persist_mark = A.mark()

    def rmsnorm(xt_ap, xt_res, g_bc, xs_ap, junk_ap, xs_res="xs", junk_res="junk", g_res="gbc"):
        ACT(lambda e: e.activation(out=junk_ap, in_=xt_ap, func=AF.Square, accum_out=ssq), [xt_res], [junk_res, "ssq"])
        ACT(lambda e: e.activation(out=std, in_=ssq, func=AF.Sqrt, bias=EPS, scale=1.0 / D), ["ssq"], ["std"])
        DVE(lambda e: e.reciprocal(out=rstd, in_=std), ["std"], ["rstd"])
        DVE(lambda e: e.scalar_tensor_tensor(out=xs_ap, in0=xt_ap, scalar=rstd, in1=g_bc, op0=ALU.mult, op1=ALU.mult),
            [xt_res, "rstd", g_res], [xs_res])

    def transpose8(src_ap, src_res, dst_ap, dst_res, nblk=8, copy_eng="act"):
        pb = bank_bf(0)
        for k in range(nblk):
            tp(pb[:, k, :], src_ap[:, k * 128:(k + 1) * 128], [src_res], [PR(0)])
        if copy_eng == "act":
            ACT(lambda e: e.copy(out=dst_ap, in_=pb[:, 0:nblk, :]), [PR(0)], [dst_res])
        else:
            DVE(lambda e: e.tensor_copy(out=dst_ap, in_=pb[:, 0:nblk, :]), [PR(0)], [dst_res])

    def dump_m():
        dtmp = A.alloc([128, 8, 512], F32)
        for g in range(4):
            DVE(lambda e, g=g: e.tensor_copy(out=dtmp, in_=mergedT[:, :, g * 512:(g + 1) * 512]), [("mT", oc, g) for oc in range(8)] + ["dbgo"], ["dtmp"])
            DMA("sp", dbg_m.rearrange("p (k t) -> p k t", k=8)[:, :, g * 512:(g + 1) * 512], dtmp, ["dtmp"], ["dbgo"], "dbgo")
        S.barrier()

    class _Stop(Exception):
        pass

    def phases():
        WinA = A.alloc([128, 8, 2560], BF16)
        Wco = A.alloc([128, 4, 1024], BF16)
        gbc_A = A.alloc([128, D], F32)
        convw = A.alloc([128, 4, 3], F32)
        xt2_A = [A.alloc([128, D], F32) for _ in range(2)]
        xs_A = A.alloc([128, D], BF16)
        junk_A = A.alloc([128, D], BF16)
        xnT4 = [A.alloc([128, 8, 512], BF16) for _ in range(2)]
        xin_sb = A.alloc([128, 4, 512], F32)
        u = A.alloc([128, 4, 514], F32)
        cc = A.alloc([128, 4, 512], F32)
        bc = A.alloc([128, 4, 512], BF16)
        sg2 = [A.alloc([128, 512], F32) for _ in range(2)]

        zt_ = A.alloc([128, 4112], BF16)
        POOL(lambda e: e.memset(zt_, 0.0), [], ["zfill"])
        XGf = XG.rearrange("r d -> (r d)").rearrange("(p n) -> p n", p=128)
        DMA("sp", gbc_A, g_mix.partition_broadcast(128), [], ["gbc"], "gbc")
        DMA("sp", convw, conv_wT.rearrange("(c p) k -> p c k", p=128), [], ["convw"], "convw")
        for k in range(8):
            DMA("pool", WinA[:, k, 0:1536], w_in[k * 128:(k + 1) * 128, 0:1536], [], [("WinA", k)], "WinA", nb=8)
        for k in range(4):
            DMA("pool", Wco[:, k, :], w_conv_out[k * 128:(k + 1) * 128, :], [], [("Wco", k)], "Wco", nb=4)
        for k in range(8):
            DMA("pool", WinA[:, k, 1536:2560], w_in[k * 128:(k + 1) * 128, 4608:5632], [], [("WinAg", k)], "WinAg", nb=8)
        DMA("sp", xt2_A[1], xp[TOK - 128:TOK, :], [], [("xt", 1)], "xt1")
        rmsnorm(xt2_A[1], ("xt", 1), gbc_A, xs_A, junk_A, xs_res=("xsA", 0))
        transpose8(xs_A, ("xsA", 0), xnT4[1][:, :, 0:128], ("xnT4", 1, 0))
        for c in range(4):
            for k in range(8):
                mm(bank(1)[:, 0:128], WinA[:, k, c * 128:(c + 1) * 128], xnT4[1][:, k, 0:128], k == 0, k == 7, [("xnT4", 1, 0)] + wres("WinA", 8), [PR(1)])
            ACT(lambda e, c=c: e.copy(out=xin_sb[:, c, 0:128], in_=bank(1)[:, 0:128]), [PR(1)], [("xin", c)])
            for k in range(8):
                mm(bank(2)[:, 0:128], WinA[:, k, 1024 + c * 128:1024 + (c + 1) * 128], xnT4[1][:, k, 0:128], k == 0, k == 7, [("xnT4", 1, 0)] + wres("WinA", 8), [PR(2)])
            DVE(lambda e, c=c: e.tensor_tensor(out=u[:, c, 0:2], in0=bank(2)[:, 126:128], in1=xin_sb[:, c, 126:128], op=ALU.mult),
                [PR(2), ("xin", c)], ["u"])

        projA = [1, 2, 3, 6, 7]
        pa_i = [0]

        def next_proj(pool):
            b = pool[pa_i[0] % len(pool)]
            pa_i[0] += 1
            return b

        xs4 = [xs_A] + [A.alloc([128, D], BF16) for _ in range(3)]
        sg8 = list(sg2) + [A.alloc([128, 512], F32) for _ in range(6)]

        def hnA(g, t):
            tile = g * 4 + t
            xb = xt2_A[tile % 2]
            xr = ("xt", tile % 2)
            DMA("sp", xb, xc[tile * 128:(tile + 1) * 128, :], [], [xr], "xt%d" % (tile % 2))
            ACT(lambda e: e.activation(out=junk_A, in_=xb, func=AF.Square, accum_out=ssq), [xr], ["junk", "ssq"])
            ACT(lambda e: e.activation(out=std, in_=ssq, func=AF.Sqrt, bias=EPS, scale=1.0 / D), ["ssq"], ["std"])
            DVE(lambda e: e.reciprocal(out=rstd, in_=std), ["std"], ["rstd"])
            DVE(lambda e: e.scalar_tensor_tensor(out=xs4[t], in0=xb, scalar=rstd, in1=gbc_A, op0=ALU.mult, op1=ALU.mult),
                [xr, "rstd", "gbc"], [("xsA", t)])

        def htA(g, t):
            transpose8(xs4[t], ("xsA", t), xnT4[g % 2][:, :, t * 128:(t + 1) * 128], ("xnT4", g % 2, t))

        def xinA(g):
            gb = g % 2
            xnr = [("xnT4", gb, t) for t in range(4)]
            for c in range(4):
                b = next_proj(projA)
                for k in range(8):
                    mm(bank(b), WinA[:, k, c * 128:(c + 1) * 128], xnT4[gb][:, k, :], k == 0, k == 7, xnr + wres("WinA", 8), [PR(b)])
                ACT(lambda e, b=b, c=c: e.copy(out=xin_sb[:, c, :], in_=bank(b)), [PR(b)], [("xin", c)])

        def cgA(g):
            gb = g % 2
            xnr = [("xnT4", gb, t) for t in range(4)]
            for c in range(4):
                b = next_proj(projA)
                for k in range(8):
                    mm(bank(b), WinA[:, k, 1024 + c * 128:1024 + (c + 1) * 128], xnT4[gb][:, k, :], k == 0, k == 7, xnr + wres("WinA", 8), [PR(b)])
                DVE(lambda e, b=b, c=c: e.tensor_tensor(out=u[:, c, 2:514], in0=bank(b), in1=xin_sb[:, c, :], op=ALU.mult),
                    [PR(b), ("xin", c)], ["u"])
                POOL(lambda e, c=c: e.tensor_scalar(out=cc[:, c, :], in0=u[:, c, 0:512], scalar1=convw[:, c, 0:1], scalar2=None, op0=ALU.mult),
                     ["u", "convw"], [("cc", c)])
                DVE(lambda e, c=c: e.scalar_tensor_tensor(out=cc[:, c, :], in0=u[:, c, 1:513], scalar=convw[:, c, 1:2], in1=cc[:, c, :], op0=ALU.mult, op1=ALU.add),
                    ["u", "convw", ("cc", c)], [("cc", c)])
                DVE(lambda e, c=c: e.scalar_tensor_tensor(out=cc[:, c, :], in0=u[:, c, 2:514], scalar=convw[:, c, 2:3], in1=cc[:, c, :], op0=ALU.mult, op1=ALU.add),
                    ["u", "convw", ("cc", c)], [("cc", c)])
            POOL(lambda e: e.tensor_copy(out=u[:, :, 0:2], in_=u[:, :, 512:514]), ["u"], ["u"])

        def bgA(g):
            gb = g % 2
            xnr = [("xnT4", gb, t) for t in range(4)]
            for c in range(4):
                b = next_proj(projA)
                for k in range(8):
                    mm(bank(b), WinA[:, k, 512 + c * 128:512 + (c + 1) * 128], xnT4[gb][:, k, :], k == 0, k == 7, xnr + wres("WinA", 8), [PR(b)])
                DVE(lambda e, b=b, c=c: e.tensor_tensor(out=bc[:, c, :], in0=bank(b), in1=cc[:, c, :], op=ALU.mult),
                    [PR(b), ("cc", c)], [("bc", c)])

        def gateA(g, ocs):
            gb = g % 2
            xnr = [("xnT4", gb, t) for t in range(4)]
            for oc in ocs:
                b = next_proj(projA)
                for k in range(8):
                    mm(bank(b), WinA[:, k, 1536 + oc * 128:1536 + (oc + 1) * 128], xnT4[gb][:, k, :], k == 0, k == 7, xnr + wres("WinAg", 8), [PR(b)])
                ACT(lambda e, b=b, oc=oc: e.activation(out=sg8[oc], in_=bank(b), func=AF.Sigmoid), [PR(b)], [("sg", oc)])

        def yconvA(g, ocs):
            for oc in ocs:
                yb = 4 + (oc % 2)
                for k in range(4):
                    mm(bank(yb), Wco[:, k, oc * 128:(oc + 1) * 128], bc[:, k, :], k == 0, k == 3, [("bc", kk) for kk in range(4)] + wres("Wco", 4), [PR(yb)])
                DVE(lambda e, yb=yb, oc=oc, g=g: e.tensor_tensor(out=mergedT[:, oc, g * 512:(g + 1) * 512], in0=bank(yb), in1=sg8[oc], op=ALU.mult),
                    [PR(yb), ("sg", oc)], [("mT", oc, g)])

        for t in range(4):
            hnA(0, t)
            htA(0, t)
        for g in range(4):
            nx = g + 1 if g + 1 < 4 else None
            if nx is not None:
                hnA(nx, 0)
            xinA(g)
            if nx is not None:
                htA(nx, 0)
                hnA(nx, 1)
            cgA(g)
            if nx is not None:
                htA(nx, 1)
                hnA(nx, 2)
            gateA(g, range(0, 4))
            bgA(g)
            if nx is not None:
                htA(nx, 2)
                hnA(nx, 3)
            gateA(g, range(4, 8))
            if nx is not None:
                htA(nx, 3)
            yconvA(g, range(8))
            if g == 0:
                for i in range(16):
                    DMA("sp", XGf[:, i * 4112:(i + 1) * 4112], zt_, ["zfill"], [("XGz", i)], "xgz", nb=16)

        S.barrier()
        A.release(persist_mark)
        if stop == "A":
            dump_m()
            raise _Stop()

        WinB = A.alloc([128, 8, 4096], BF16)
        Wro = A.alloc([128, 8, 1024], BF16)
        gbc_B = A.alloc([128, D], F32)
        cs = A.alloc([128, 2, NT, 32], F32)
        gq = A.alloc([128, 4, 128], F32)
        gk = A.alloc([128, 4, 128], F32)
        zt = A.alloc([128, 8], F32)
        ct = A.alloc([128, 4, 128], F32)
        maskT = A.alloc([128, 4, 128], F32)
        Sst = A.alloc([128, 4, 128], F32)
        Sb = A.alloc([128, 4, 128], BF16)
        xt2_B = [A.alloc([128, D], F32) for _ in range(2)]
        xs_B = A.alloc([128, D], BF16)
        junk_B = A.alloc([128, D], BF16)
        xnT4b2 = [A.alloc([128, 8, 512], BF16) for _ in range(2)]
        xs_B2 = [xs_B, A.alloc([128, D], BF16)]
        qr2 = [A.alloc([128, 8, 2, 32], BF16) for _ in range(2)]
        kr2 = [A.alloc([128, 8, 2, 32], BF16) for _ in range(2)]
        kz2 = [A.alloc([128, 8, 64], BF16) for _ in range(2)]
        v2 = [A.alloc([128, D], BF16) for _ in range(2)]
        sgt2 = [A.alloc([128, D], BF16) for _ in range(2)]
        rt = [A.alloc([128, 8, 32], F32) for _ in range(4)]
        qTz = A.alloc([128, 4, 2, 128], BF16)
        kT = A.alloc([128, 4, 128], BF16)
        PT = A.alloc([128, 8, 128], BF16)
        osq = A.alloc([128, D], F32)
        zb = A.alloc([128, D], BF16)
        zT4 = A.alloc([128, 8, 512], BF16)
        sgr2 = [A.alloc([128, 512], F32) for _ in range(2)]
        tmpm = A.alloc([128, 512], F32)
        gst = A.alloc([128, 64], F32)

        DMA("sp", gbc_B, g_mix.partition_broadcast(128), [], ["gbc"], "gbc")
        DMA("sp", cs, c_cs_pre, [], ["cs"], "cs")
        DMA("sp", gq, c_gq, [], ["gq"], "c_gq")
        DMA("sp", gk, c_gk, [], ["gk"], "c_gk")
        DMA("sp", zt, c_zt, [], ["zt"], "c_zt")
        DMA("sp", ct, c_ct, [], ["ct"], "c_ct")
        DMA("sp", maskT, c_mask, [], ["maskT"], "c_mask")
        for k in range(8):
            DMA("pool", WinB[:, k, 512:2048], w_in[k * 128:(k + 1) * 128, 2048:3584], [], [("WinBkv", k)], "WinBkv", nb=8)
        for k in range(8):
            DMA("pool", WinB[:, k, 0:512], w_in[k * 128:(k + 1) * 128, 1536:2048], [], [("WinBq", k)], "WinBq", nb=8)
        for k in range(8):
            DMA("pool", WinB[:, k, 2048:3072], w_in[k * 128:(k + 1) * 128, 3584:4608], [], [("WinBg", k)], "WinBg", nb=8)
        for k in range(8):
            DMA("pool", WinB[:, k, 3072:4096], w_in[k * 128:(k + 1) * 128, 5632:6656], [], [("WinBr", k)], "WinBr", nb=8)
        load_w(Wro, w_ret_out, 8, "Wro", "Wro")
        POOL(lambda e: e.memset(Sst, 0.0), [], ["Sst"])
        POOL(lambda e: e.memset(Sb, 0.0), [], ["Sb"])
        POOL(lambda e: e.memset(qTz, 0.0), [], ["qT"])

        projB = [1, 2, 7]
        pa_i[0] = 0

        def rotary(pb_ap, pres, tile, dst, dres, ti):
            pv = pb_ap.rearrange("p (h t f) -> p h t f", h=8, t=2)
            cosb = cs[:, 0, tile, :].unsqueeze(1).broadcast_to([128, 8, 32])
            sinb = cs[:, 1, tile, :].unsqueeze(1).broadcast_to([128, 8, 32])
            ta, tb, tc, td = rt
            DVE(lambda e: e.tensor_tensor(out=ta, in0=pv[:, :, 0, :], in1=cosb, op=ALU.mult), [pres, "cs"], ["rta"])
            DVE(lambda e: e.tensor_tensor(out=tb, in0=pv[:, :, 1, :], in1=sinb, op=ALU.mult), [pres, "cs"], ["rtb"])
            DVE(lambda e: e.tensor_tensor(out=tc, in0=pv[:, :, 0, :], in1=sinb, op=ALU.mult), [pres, "cs"], ["rtc"])
            DVE(lambda e: e.tensor_tensor(out=td, in0=pv[:, :, 1, :], in1=cosb, op=ALU.mult), [pres, "cs"], ["rtd"])
            POOL(lambda e: e.tensor_tensor(out=dst[:, :, 0, :], in0=ta, in1=tb, op=ALU.subtract), ["rta", "rtb"], [(dres, 0)])
            POOL(lambda e: e.tensor_tensor(out=dst[:, :, 1, :], in0=tc, in1=td, op=ALU.add), ["rtc", "rtd"], [(dres, 1)])

        def state_update(bi):
            kzv = kz2[bi].rearrange("p h d -> p (h d)")
            for c in range(4):
                ob = ps_t[:, 3 * 512 + c * 256: 3 * 512 + (c + 1) * 256]
                mm(ob, kzv[:, c * 128:(c + 1) * 128], v2[bi][:, c * 256:(c + 1) * 256], True, True,
                   [("kz", bi), ("v", bi)], [PR(3), PR(4)])
            pS = bank(3, 2).rearrange("p (c n) -> p c n", c=4)
            POOL(lambda e: e.tensor_tensor(out=Sst, in0=Sst, in1=ct, op=ALU.mult), ["Sst", "ct"], ["Sst"])
            DVE(lambda e: e.tensor_tensor(out=Sst[0:64], in0=Sst[0:64], in1=pS[0:64, :, 0:128], op=ALU.add), ["Sst", PR(3), PR(4)], ["Sst"])
            DVE(lambda e: e.tensor_tensor(out=Sst[64:128], in0=Sst[64:128], in1=pS[64:128, :, 128:256], op=ALU.add), ["Sst", PR(3), PR(4)], ["Sst"])
            ACT(lambda e: e.copy(out=Sb, in_=Sst), ["Sst"], ["Sb"])

        def proj_tm(xnT_ap, xn_res, col0, wres_, b):
            for k in range(8):
                mm(bank(b), xnT_ap[:, k, :], WinB[:, k, col0:col0 + 512], k == 0, k == 7, xn_res + wres_, [PR(b)])

        def RPp(tile):
            bi = tile % 2
            xb = xt2_B[bi]
            xr = ("xt", bi)
            DMA("sp", xb, xp[tile * 128:(tile + 1) * 128, :], [], [xr], "xt%d" % bi)
            rmsnorm(xb, xr, gbc_B, xs_B, junk_B, xs_res=("xsB", 0))
            transpose8(xs_B, ("xsB", 0), xnT4b2[1][:, :, (tile % 4) * 128:(tile % 4 + 1) * 128], ("xnT4b", 1, tile % 4))

        def PPp(tile):
            bi = tile % 2
            xnTp_ = xnT4b2[1][:, :, (tile % 4) * 128:(tile % 4 + 1) * 128]
            xres_ = [("xnT4b", 1, tile % 4)]
            b = next_proj(projB)
            proj_tm(xnTp_, xres_, 512, wres("WinBkv", 8), b)
            rotary(bank(b), PR(b), tile, kr2[bi], ("kr", bi), 1)
            POOL(lambda e, bi=bi: e.tensor_tensor(out=kz2[bi], in0=kr2[bi].rearrange("p h t f -> p h (t f)"),
                                                  in1=zt.unsqueeze(2).broadcast_to([128, 8, 64]), op=ALU.mult),
                 [(("kr", bi), 0), (("kr", bi), 1), "zt"], [("kz", bi)])
            for hf in range(2):
                b = next_proj(projB)
                proj_tm(xnTp_, xres_, 1024 + hf * 512, wres("WinBkv", 8), b)
                ACT(lambda e, b=b, bi=bi, hf=hf: e.copy(out=v2[bi][:, hf * 512:(hf + 1) * 512], in_=bank(b)), [PR(b)], [("v", bi)])
            state_update(bi)

        RPp(0)
        for tile in range(NT):
            if tile + 1 < NT:
                RPp(tile + 1)
            PPp(tile)

        if stop == "B1":
            raise _Stop()

        def chk(n):
            if stop == "B2:%d" % n:
                raise _Stop()
        DMA("sp", cs, c_cs_own, [], ["cs"], "cs")

        def RB(g, t):
            tile = g * 4 + t
            bi = tile % 2
            xb = xt2_B[bi]
            xr = ("xt", bi)
            DMA("sp", xb, xc[tile * 128:(tile + 1) * 128, :], [], [xr], "xt%d" % bi)
            rmsnorm(xb, xr, gbc_B, xs_B2[bi], junk_B, xs_res=("xsB", bi))

        def RBt(g, t):
            tile = g * 4 + t
            bi = tile % 2
            xnT_t = xnT4b2[g % 2][:, :, t * 128:(t + 1) * 128]
            transpose8(xs_B2[bi], ("xsB", bi), xnT_t, ("xnT4b", g % 2, t))

        def PB(g, t):
            tile = g * 4 + t
            bi = tile % 2
            xnT_t = xnT4b2[g % 2][:, :, t * 128:(t + 1) * 128]
            xnres = [("xnT4b", g % 2, t)]
            b = next_proj(projB)
            proj_tm(xnT_t, xnres, 0, wres("WinBq", 8), b)
            rotary(bank(b), PR(b), tile, qr2[bi], ("qr", bi), 0)
            b = next_proj(projB)
            proj_tm(xnT_t, xnres, 512, wres("WinBkv", 8), b)
            rotary(bank(b), PR(b), tile, kr2[bi], ("kr", bi), 1)
            POOL(lambda e, bi=bi: e.tensor_tensor(out=kz2[bi], in0=kr2[bi].rearrange("p h t f -> p h (t f)"),
                                                  in1=zt.unsqueeze(2).broadcast_to([128, 8, 64]), op=ALU.mult),
                 [(("kr", bi), 0), (("kr", bi), 1), "zt"], [("kz", bi)])
            yield
            for hf in range(2):
                b = next_proj(projB)
                proj_tm(xnT_t, xnres, 1024 + hf * 512, wres("WinBkv", 8), b)
                ACT(lambda e, b=b, bi=bi, hf=hf: e.copy(out=v2[bi][:, hf * 512:(hf + 1) * 512], in_=bank(b)), [PR(b)], [("v", bi)])
            yield
            for hf in range(2):
                b = next_proj(projB)
                proj_tm(xnT_t, xnres, 2048 + hf * 512, wres("WinBg", 8), b)
                ACT(lambda e, b=b, hf=hf: e.activation(out=sgr2[hf], in_=bank(b), func=AF.Sigmoid), [PR(b)], [("sgr", hf)])
                DVE(lambda e, b=b, bi=bi, hf=hf: e.tensor_tensor(out=sgt2[bi][:, hf * 512:(hf + 1) * 512], in0=bank(b), in1=sgr2[hf], op=ALU.mult),
                    [PR(b), ("sgr", hf)], [("sgt", bi)])

        def tailB(g, t):
            tile = g * 4 + t
            bi = tile % 2
            pb = bank_bf(0)
            qrv = qr2[bi].rearrange("p h t f -> p (h t f)")
            krv = kr2[bi].rearrange("p h t f -> p (h t f)")
            for c in range(4):
                tp(pb[:, c, :], qrv[:, c * 128:(c + 1) * 128], [(("qr", bi), 0), (("qr", bi), 1)], [PR(0)])
            for c in range(4):
                tp(pb[:, 4 + c, :], krv[:, c * 128:(c + 1) * 128], [(("kr", bi), 0), (("kr", bi), 1)], [PR(0)])
            DVE(lambda e: e.tensor_tensor(out=qTz[0:64, :, 0, :], in0=bank_bf(0)[0:64, 0:4, :], in1=gq[0:64], op=ALU.mult), [PR(0), "gq"], ["qT"])
            DVE(lambda e: e.tensor_tensor(out=qTz[64:128, :, 1, :], in0=bank_bf(0)[64:128, 0:4, :], in1=gq[64:128], op=ALU.mult), [PR(0), "gq", "qT"], ["qT"])
            DVE(lambda e: e.tensor_tensor(out=kT, in0=bank_bf(0)[:, 4:8, :], in1=gk, op=ALU.mult), [PR(0), "gk"], ["kT"])
            yield
            for c in range(4):
                mm(ps_t[:, 3 * 512 + c * 256: 3 * 512 + (c + 1) * 256], kT[:, c, :], qTz[:, c, :, :].rearrange("p a q -> p (a q)"), True, True,
                   ["qT", "kT"], [PR(3 + c // 2)])
            mb = maskT
            DVE(lambda e, mb=mb: e.tensor_tensor(out=PT[:, 0:4, :], in0=bank(3).rearrange("p (h q) -> p h q", h=4), in1=mb, op=ALU.mult),
                [PR(3), "maskT"], [("PT", 0)])
            DVE(lambda e, mb=mb: e.tensor_tensor(out=PT[:, 4:8, :], in0=bank(4).rearrange("p (h q) -> p h q", h=4), in1=mb, op=ALU.mult),
                [PR(4), "maskT"], [("PT", 1)])
            yield
            for h in range(8):
                p0 = (h % 2) * 64
                ob = ps_t[:, 5 * 512 + h * 128: 5 * 512 + (h + 1) * 128]
                mm(ob, PT[:, h, :], v2[bi][:, h * 128:(h + 1) * 128], True, False, [("PT", h // 4), ("v", bi)], [PR(5 + h // 4)])
                mm(ob, qTz[:, h // 2, h % 2, :], Sb[:, h // 2, :], False, True, ["qT", "Sb"], [PR(5 + h // 4)])
            yield
            for hb in range(2):
                ACT(lambda e, hb=hb: e.copy(out=osq[:, hb * 512:(hb + 1) * 512], in_=bank(5 + hb)), [PR(5 + hb)],
                    [("osq", hb)] + [("on", hh_) for hh_ in range(hb * 4, hb * 4 + 4)])
            DVE(lambda e: e.reduce_sum(out=gst[:, 0:8], in_=osq.rearrange("p (h e) -> p h e", h=8), axis=AX.X), [("osq", 0), ("osq", 1)], ["g_sum"])
            for h in range(8):
                ACT(lambda e, h=h: e.activation(out=junk_B[:, h * 128:(h + 1) * 128], in_=osq[:, h * 128:(h + 1) * 128], func=AF.Square,
                                                accum_out=gst[:, 8 + h:9 + h]),
                    [("osq", h // 4)], ["junk", ("g_sq", h)])
            DVE(lambda e: e.tensor_scalar(out=gst[:, 16:24], in0=gst[:, 0:8], scalar1=1.0 / 128, scalar2=None, op0=ALU.mult), ["g_sum"], ["g_mean"])
            DVE(lambda e: e.tensor_tensor(out=gst[:, 24:32], in0=gst[:, 16:24], in1=gst[:, 16:24], op=ALU.mult), ["g_mean"], ["g_msq"])
            DVE(lambda e: e.scalar_tensor_tensor(out=gst[:, 32:40], in0=gst[:, 8:16], scalar=1.0 / 128, in1=gst[:, 24:32], op0=ALU.mult, op1=ALU.subtract),
                [("g_sq", h) for h in range(8)] + ["g_msq"], ["g_var"])
            ACT(lambda e: e.activation(out=gst[:, 40:48], in_=gst[:, 32:40], func=AF.Sqrt, bias=EPS, scale=1.0), ["g_var"], ["g_std"])
            DVE(lambda e: e.reciprocal(out=gst[:, 48:56], in_=gst[:, 40:48]), ["g_std"], ["g_rstd"])
            DVE(lambda e: e.scalar_tensor_tensor(out=gst[:, 56:64], in0=gst[:, 16:24], scalar=-1.0, in1=gst[:, 48:56], op0=ALU.mult, op1=ALU.mult),
                ["g_mean", "g_rstd"], ["g_nmr"])
            for h in range(8):
                DVE(lambda e, h=h: e.tensor_scalar(out=osq[:, h * 128:(h + 1) * 128], in0=osq[:, h * 128:(h + 1) * 128],
                                                   scalar1=gst[:, 48 + h:49 + h], scalar2=gst[:, 56 + h:57 + h], op0=ALU.mult, op1=ALU.add),
                    [("osq", h // 4), "g_rstd", "g_nmr"] + [("g_sq", hh_) for hh_ in range(8)] + ["g_sum"], [("on", h)])
            yield
            POOL(lambda e, bi=bi: e.tensor_tensor(out=zb, in0=osq, in1=sgt2[bi], op=ALU.mult), [("on", hh_) for hh_ in range(8)] + [("sgt", bi)], ["zb"])
            transpose8(zb, "zb", zT4[:, :, t * 128:(t + 1) * 128], ("zT4", t))
            state_update(bi)

        def glevelB(g):
            xnr = [("xnT4b", g % 2, t) for t in range(4)]
            zr = [("zT4", t) for t in range(4)]
            for oc in range(8):
                yb = next_proj(projB)
                for k in range(8):
                    mm(bank(yb), Wro[:, k, oc * 128:(oc + 1) * 128], zT4[:, k, :], k == 0, k == 7, zr + wres("Wro", 8), [PR(yb)])
                b = next_proj(projB)
                for k in range(8):
                    mm(bank(b), WinB[:, k, 3072 + oc * 128:3072 + (oc + 1) * 128], xnT4b2[g % 2][:, k, :], k == 0, k == 7, xnr + wres("WinBr", 8), [PR(b)])
                sgb = sgr2[oc % 2]
                ACT(lambda e, b=b, sgb=sgb: e.activation(out=sgb, in_=bank(b), func=AF.Sigmoid), [PR(b)], [("sgr", oc % 2)])
                DVE(lambda e, yb=yb, sgb=sgb: e.tensor_tensor(out=tmpm, in0=bank(yb), in1=sgb, op=ALU.mult), [PR(yb), ("sgr", oc % 2)], ["tmpm"])
                POOL(lambda e, oc=oc, g=g: e.tensor_tensor(out=mergedT[:, oc, g * 512:(g + 1) * 512], in0=tmpm, in1=mergedT[:, oc, g * 512:(g + 1) * 512], op=ALU.add),
                     ["tmpm", ("mT", oc, g)], [("mT", oc, g)])


        def interleave(*gens):
            alive = [g_ for g_ in gens if g_ is not None]
            while alive:
                for g_ in list(alive):
                    try:
                        next(g_)
                    except StopIteration:
                        alive.remove(g_)

        def step(gen_):
            if gen_ is None:
                return
            try:
                next(gen_)
            except StopIteration:
                pass

        def drain(gen_):
            if gen_ is None:
                return
            for _ in gen_:
                pass

        orderB = [(g, t) for g in range(4) for t in range(4)]
        RB(*orderB[0])
        RBt(*orderB[0])
        RB(*orderB[1])
        RBt(*orderB[1])
        drain(PB(*orderB[0]))
        for i_, (g, t) in enumerate(orderB):
            if i_ + 2 < len(orderB):
                RB(*orderB[i_ + 2])
            tg = tailB(g, t)
            pg = PB(*orderB[i_ + 1]) if i_ + 1 < len(orderB) else None
            step(tg)
            step(pg)
            step(tg)
            step(tg)
            step(tg)
            step(pg)
            drain(pg)
            drain(tg)
            if i_ + 2 < len(orderB):
                RBt(*orderB[i_ + 2])
            if t == 3:
                glevelB(g)

        S.barrier()
        A.release(persist_mark)

        if DBG:
            dump_m()
            A.release(persist_mark)
        if stop == "B":
            raise _Stop()

        h = A.alloc([128, NT, D], F32)
        e_mark = A.mark()
        KT = A.alloc([128, 8, 256], BF16)
        Vm = A.alloc([128, 2, D], BF16)
        Wmix = A.alloc([128, 8, D], BF16)
        Wq = A.alloc([128, 8, D], BF16)
        Wo = A.alloc([128, 8, D], BF16)
        Wr = A.alloc([128, 8, 36], BF16)
        gbc_xa = A.alloc([128, D], F32)
        gbc_moe = A.alloc([128, D], F32)
        brt = A.alloc([128, 36], F32)
        ebase2 = A.alloc([128, 2 * NE], F32)
        ebase = ebase2[:, 0:NE]
        ebaseY = ebase2[:, NE:2 * NE]
        x_mark = A.mark()
        Wkv = A.alloc([128, 8, 2048], BF16)
        memt = A.alloc([128, D], F32)
        xs_K = A.alloc([128, D], BF16)
        mnT = A.alloc([128, 8, 256], BF16)
        load_w(Wkv, w_xa_kv, 8, "Wkv", "Wkv")
        load_w(Wmix, w_mix_out, 8, "Wmix", "Wmix")
        load_w(Wq, w_xa_q, 8, "Wq", "Wq")
        load_w(Wo, w_xa_o, 8, "Wo", "Wo")
        load_w(Wr, w_rt, 8, "Wr", "Wr")
        DMA("sp", gbc_xa, g_xa.partition_broadcast(128), [], ["gbc_xa"], "gbcx1")
        DMA("sp", brt, b_rt.partition_broadcast(128), [], ["brt"], "gbcx3")
        DMA("sp", ebase2, c_eb, [], ["ebase"], "gbcx4")
        DMA("sp", gbc_moe, g_mem.partition_broadcast(128), [], ["gbc_moe"], "gbcx2")
        for mc in range(2):
            DMA("sp", memt, memc[mc * 128:(mc + 1) * 128, :], [], ["memt"], "memt")
            rmsnorm(memt, "memt", gbc_moe, xs_K, xs_K, junk_res="xs", g_res="gbc_moe")
            transpose8(xs_K, "xs", mnT[:, :, mc * 128:(mc + 1) * 128], ("mnT", mc))
        DMA("sp", gbc_moe, g_moe.partition_broadcast(128), [], ["gbc_moe"], "gbcx2")
        mnr = [("mnT", 0), ("mnT", 1)]
        for c in range(8):
            b = 1 + (c % 2)
            for k in range(8):
                mm(bank(b)[:, 0:256], Wkv[:, k, c * 128:(c + 1) * 128], mnT[:, k, :], k == 0, k == 7, mnr + wres("Wkv", 8), [PR(b)])
            ACT(lambda e, b=b, c=c: e.copy(out=KT[:, c, :], in_=bank(b)[:, 0:256]), [PR(b)], ["KT"])
        for mc in range(2):
            for hf in range(2):
                b = 3 + ((mc * 2 + hf) % 2)
                for k in range(8):
                    mm(bank(b), mnT[:, k, mc * 128:(mc + 1) * 128], Wkv[:, k, 1024 + hf * 512:1024 + (hf + 1) * 512], k == 0, k == 7, mnr + wres("Wkv", 8), [PR(b)])
                DVE(lambda e, b=b, mc=mc, hf=hf: e.tensor_copy(out=Vm[:, mc, hf * 512:(hf + 1) * 512], in_=bank(b)), [PR(b)], ["Vm"])
        S.barrier()
        A.release(x_mark)

        carry = A.alloc([128, NE], F32)
        xt2_X = [A.alloc([128, D], F32) for _ in range(2)]
        xs_X = A.alloc([128, D], BF16)
        junk_X = A.alloc([128, D], BF16)
        xn2T = A.alloc([128, 8, 256], BF16)
        qT2 = A.alloc([128, 8, 256], BF16)
        Pb = A.alloc([128, 4, 256], BF16)
        PTx = A.alloc([128, 8, 128], BF16)
        obx = A.alloc([128, 4, 256], BF16)
        oTx = A.alloc([128, 8, 128], BF16)
        xn3 = [A.alloc([128, D], BF16) for _ in range(2)]
        xn3T = A.alloc([128, 8, 128], BF16)
        rs = A.alloc([128, 256], F32)
        Mb = A.alloc([128, NE], BF16)

        POOL(lambda e: e.memset(carry, 0.0), [], ["carry"])


        def rmsnorm2(xt_ap, xt_res, g_bc, g_res, xs_ap, xs_res, rt=False):
            o_ = 4 if rt else 0
            sfx = "_r" if rt else ""
            ssq_, std_, rstd_ = stat[:, o_:o_ + 1], stat[:, o_ + 1:o_ + 2], stat[:, o_ + 2:o_ + 3]
            jk = junk_X
            ACT(lambda e: e.activation(out=jk, in_=xt_ap, func=AF.Square, accum_out=ssq_), [xt_res], ["junk", "ssq" + sfx])
            ACT(lambda e: e.activation(out=std_, in_=ssq_, func=AF.Sqrt, bias=EPS, scale=1.0 / D), ["ssq" + sfx], ["std" + sfx])
            DVE(lambda e: e.reciprocal(out=rstd_, in_=std_), ["std" + sfx], ["rstd" + sfx])
            DVE(lambda e: e.scalar_tensor_tensor(out=xs_ap, in0=xt_ap, scalar=rstd_, in1=g_bc, op0=ALU.mult, op1=ALU.mult),
                [xt_res, "rstd" + sfx, g_res], [xs_res])

        LG = rs[:, 0:36]
        GMAX = rs[:, 36:37]
        NGM = rs[:, 37:38]
        GE = rs[:, 40:44]
        GSUM = rs[:, 44:45]
        PG = rs[:, 45:46]
        GM = rs[:, 48:52]
        PEN = rs[:, 52:56]
        ELM = rs[:, 64:96]
        M1V = rs[:, 96:97]
        M2V = rs[:, 97:98]
        DD = rs[:, 98:99]
        S2 = rs[:, 99:100]
        W1 = rs[:, 100:101]
        W2 = rs[:, 101:102]
        I1 = rs[:, 102:103]
        I2 = rs[:, 103:104]
        V1 = rs[:, 104:105]
        V2 = rs[:, 105:106]
        J1 = rs[:, 106:107]
        J2 = rs[:, 107:108]
        M1 = rs[:, 128:160]
        M2 = rs[:, 160:192]
        ELM2 = rs[:, 192:224]
        POSB = rs[:, 224:256]
        TMPR = A.alloc([128, NE], F32)
        POSR = A.alloc([128, NE], F32)
        POSY = A.alloc([128, NE], F32)

        def router(tile, bi):
            hres = ("h", tile)
            rmsnorm2(h[:, tile, :], hres, gbc_moe, "gbc_moe", xn3[bi], ("xn3", bi), rt=True)
            yield
            transpose8(xn3[bi], ("xn3", bi), xn3T, "xn3T")
            yield
            for k in range(8):
                mm(bank(7)[:, 0:36], xn3T[:, k, :], Wr[:, k, :], k == 0, k == 7, ["xn3T"] + wres("Wr", 8), [PR(7)])
            R = "rt"
            DVE(lambda e: e.tensor_tensor(out=LG, in0=bank(7)[:, 0:36], in1=brt, op=ALU.add), [PR(7), "brt"], [R])
            DVE(lambda e: e.reduce_max(out=GMAX, in_=LG[:, 0:4], axis=AX.X), [R], [R])
            DVE(lambda e: e.tensor_scalar(out=NGM, in0=GMAX, scalar1=-1.0, scalar2=None, op0=ALU.mult), [R], [R])
            ACT(lambda e: e.activation(out=GE, in_=LG[:, 0:4], func=AF.Exp, bias=NGM, scale=1.0, accum_out=GSUM), [R], [R])
            DVE(lambda e: e.reciprocal(out=PG, in_=GSUM), [R], [R])
            DVE(lambda e: e.tensor_scalar(out=GM, in0=LG[:, 0:4], scalar1=GMAX, scalar2=None, op0=ALU.is_equal), [R], [R])
            DVE(lambda e: e.tensor_scalar(out=PEN, in0=GM, scalar1=-1.0, scalar2=1e30, op0=ALU.add, op1=ALU.mult), [R], [R])
            DVE(lambda e: e.tensor_tensor(out=ELM.rearrange("p (g j) -> p g j", g=4), in0=LG[:, 4:36].rearrange("p (g j) -> p g j", g=4),
                                          in1=PEN.unsqueeze(2).broadcast_to([128, 4, 8]), op=ALU.add), [R], [R])
            yield
            DVE(lambda e: e.reduce_max(out=M1V, in_=ELM, axis=AX.X), [R], [R])
            DVE(lambda e: e.tensor_scalar(out=M1, in0=ELM, scalar1=M1V, scalar2=None, op0=ALU.is_equal), [R], [R])
            DVE(lambda e: e.scalar_tensor_tensor(out=ELM2, in0=M1, scalar=-1e30, in1=ELM, op0=ALU.mult, op1=ALU.add), [R], [R])
            DVE(lambda e: e.reduce_max(out=M2V, in_=ELM2, axis=AX.X), [R], [R])
            DVE(lambda e: e.tensor_scalar(out=M2, in0=ELM2, scalar1=M2V, scalar2=None, op0=ALU.is_equal), [R], [R])
            yield
            DVE(lambda e: e.tensor_tensor(out=DD, in0=M2V, in1=M1V, op=ALU.subtract), [R], [R])
            ACT(lambda e: e.activation(out=S2, in_=DD, func=AF.Sigmoid), [R], [R])
            DVE(lambda e: e.tensor_tensor(out=W2, in0=PG, in1=S2, op=ALU.mult), [R], [R])
            DVE(lambda e: e.tensor_tensor(out=W1, in0=PG, in1=W2, op=ALU.subtract), [R], [R])
            DVE(lambda e: e.tensor_tensor(out=Mb, in0=M1, in1=M2, op=ALU.add), [R], ["Mb"])
            mm(bank(7)[:, 64:96], ustrict, Mb, True, True, ["Mb", "ident"], [PR(7)])
            mm(bank(7)[:, 96:128], onesb, Mb, True, True, ["Mb", "ident"], [PR(7)])
            DVE(lambda e: e.tensor_tensor(out=POSR, in0=bank(7)[:, 64:96], in1=carry, op=ALU.add), [PR(7), "carry"], [R])
            DVE(lambda e: e.tensor_tensor(out=carry, in0=bank(7)[:, 96:128], in1=carry, op=ALU.add), [PR(7), "carry"], ["carry"])
            yield
            DVE(lambda e: e.scalar_tensor_tensor(out=POSB, in0=POSR, scalar=float(CAP), in1=ebase, op0=ALU.min, op1=ALU.add), [R, "ebase"], [R])
            DVE(lambda e: e.scalar_tensor_tensor(out=POSY, in0=POSR, scalar=float(CAP - 1), in1=ebaseY, op0=ALU.min, op1=ALU.add), [R, "ebase"], [R])
            for (Mx, Ix, Jx, Vx, Wx, col) in ((M1, I1, J1, V1, W1, 0), (M2, I2, J2, V2, W2, 1)):
                yield
                DVE(lambda e, Mx=Mx: e.tensor_tensor(out=TMPR, in0=Mx, in1=POSB, op=ALU.mult), [R], [R])
                DVE(lambda e, Ix=Ix: e.reduce_sum(out=Ix, in_=TMPR, axis=AX.X), [R], [R])
                DVE(lambda e, Mx=Mx: e.tensor_tensor(out=TMPR, in0=Mx, in1=POSR, op=ALU.mult), [R], [R])
                DVE(lambda e, Vx=Vx: e.reduce_sum(out=Vx, in_=TMPR, axis=AX.X), [R], [R])
                DVE(lambda e, Ix=Ix, col=col: e.tensor_copy(out=sidx[:, tile, col:col + 1], in_=Ix), [R], [("sidx", tile)])
                DVE(lambda e, Mx=Mx: e.tensor_tensor(out=TMPR, in0=Mx, in1=POSY, op=ALU.mult), [R], [R])
                DVE(lambda e, Jx=Jx: e.reduce_sum(out=Jx, in_=TMPR, axis=AX.X), [R], [R])
                DVE(lambda e, Jx=Jx, col=col: e.tensor_copy(out=gidx[:, tile, col:col + 1], in_=Jx), [R], [("gidx", tile)])
                DVE(lambda e, Vx=Vx, Wx=Wx, col=col: e.scalar_tensor_tensor(out=wts[:, tile, col:col + 1], in0=Vx, scalar=float(CAP), in1=Wx, op0=ALU.is_lt, op1=ALU.mult),
                    [R], [("wts", tile)])
            for col in range(2):
                S.add("pool", lambda e, col=col, bi=bi: e.indirect_dma_start(
                    out=XG, out_offset=bass.IndirectOffsetOnAxis(ap=sidx[:, tile, col:col + 1], axis=0), in_=xn3[bi], in_offset=None,
                    bounds_check=NE * CAPR - 1, oob_is_err=False),
                    [("xn3", bi), ("sidx", tile)], [("XG", tile, col)], dma=True, key="xgs%d" % bi, bsize=2)

        projX = [1, 2]
        xn2T2 = [xn2T, A.alloc([128, 8, 256], BF16)]
        qT22 = [qT2, A.alloc([128, 8, 256], BF16)]

        def interleaveX(*gens):
            alive = [g_ for g_ in gens if g_ is not None]
            while alive:
                for g_ in list(alive):
                    try:
                        next(g_)
                    except StopIteration:
                        alive.remove(g_)

        def S1t(g, tl):
            gp = g % 2
            tile = g * 2 + tl
            bi = tile % 2
            xb = xt2_X[bi]
            xr = ("xt", bi)
            DMA("sp", xb, xc[tile * 128:(tile + 1) * 128, :], [], [xr], "xt%d" % bi)
            for hf in range(2):
                b = projX[hf]
                for k in range(8):
                    mm(bank(b), mergedT[:, k, tile * 128:(tile + 1) * 128], Wmix[:, k, hf * 512:(hf + 1) * 512], k == 0, k == 7,
                       [("mT", k, tile // 4)] + wres("Wmix", 8), [PR(b)])
                DVE(lambda e, b=b, hf=hf, tile=tile, xb=xb: e.tensor_tensor(out=h[:, tile, hf * 512:(hf + 1) * 512], in0=bank(b), in1=xb[:, hf * 512:(hf + 1) * 512], op=ALU.add),
                    [PR(b), xr], [("h", tile)])
            yield
            rmsnorm2(h[:, tile, :], ("h", tile), gbc_xa, "gbc_xa", xs_X, "xs")
            yield
            transpose8(xs_X, "xs", xn2T2[gp][:, :, tl * 128:(tl + 1) * 128], ("xn2T", gp, tl))
            yield

        def QT_X(g):
            gp = g % 2
            for c in range(8):
                b = projX[c % 2]
                for k in range(8):
                    mm(bank(b)[:, 0:256], Wq[:, k, c * 128:(c + 1) * 128], xn2T2[gp][:, k, :], k == 0, k == 7,
                       [("xn2T", gp, 0), ("xn2T", gp, 1)] + wres("Wq", 8), [PR(b)])
                ACT(lambda e, b=b, c=c, gp=gp: e.copy(out=qT22[gp][:, c, :], in_=bank(b)[:, 0:256]), [PR(b)], [("qT2", gp, c)])
                if c % 2 == 1:
                    yield

        def XA_X(tile):
            g, tl = tile // 2, tile % 2
            gp = g % 2
            sc = bank(3, 2).rearrange("p (h m) -> p h m", h=4)
            for hh in range(4):
                for kc in range(2):
                    mm(ps_t[:, 3 * 512 + hh * 256: 3 * 512 + (hh + 1) * 256], qT22[gp][:, 2 * hh + kc, tl * 128:(tl + 1) * 128], KT[:, 2 * hh + kc, :], kc == 0, kc == 1,
                       [("qT2", gp, 2 * hh + kc), "KT"], [PR(3 + hh // 2)])
            DVE(lambda e, sc=sc: e.reduce_max(out=stat[:, 8:12], in_=sc, axis=AX.X), [PR(3), PR(4)], ["xmx"])
            DVE(lambda e: e.tensor_scalar(out=stat[:, 12:16], in0=stat[:, 8:12], scalar1=-1.0 / 16, scalar2=None, op0=ALU.mult), ["xmx"], ["xnb"])
            for hh in range(4):
                ACT(lambda e, hh=hh: e.activation(out=Pb[:, hh, :], in_=ps_t[:, 3 * 512 + hh * 256: 3 * 512 + (hh + 1) * 256], func=AF.Exp,
                                                  bias=stat[:, 12 + hh:13 + hh], scale=1.0 / 16, accum_out=stat[:, 16 + hh:17 + hh]),
                    [PR(3 + hh // 2), "xnb"], [("Pb", hh), ("xsum", hh)])
            DVE(lambda e: e.reciprocal(out=stat[:, 20:24], in_=stat[:, 16:20]), [("xsum", hh) for hh in range(4)], ["xrs"])
            yield
            pb = bank_bf(0)
            Pv = Pb.rearrange("p h m -> p (h m)")
            for j in range(8):
                tp(pb[:, j, :], Pv[:, j * 128:(j + 1) * 128], [("Pb", j // 2)], [PR(0)])
            ACT(lambda e: e.copy(out=PTx, in_=bank_bf(0)), [PR(0)], ["PTx"])
            yield
            for hh in range(4):
                for mc in range(2):
                    mm(ps_t[:, 5 * 512 + hh * 256: 5 * 512 + (hh + 1) * 256], PTx[:, 2 * hh + mc, :], Vm[:, mc, hh * 256:(hh + 1) * 256], mc == 0, mc == 1,
                       ["PTx", "Vm"], [PR(5 + hh // 2)])
            DVE(lambda e: e.tensor_tensor(out=obx, in0=bank(5, 2).rearrange("p (h m) -> p h m", h=4),
                                          in1=stat[:, 20:24].unsqueeze(2).broadcast_to([128, 4, 256]), op=ALU.mult),
                [PR(5), PR(6), "xrs"], ["obx"])
            yield
            transpose8(obx.rearrange("p h m -> p (h m)"), "obx", oTx, "oTx")
            yield
            for hf in range(2):
                b = projX[hf]
                for k in range(8):
                    mm(bank(b), oTx[:, k, :], Wo[:, k, hf * 512:(hf + 1) * 512], k == 0, k == 7, ["oTx"] + wres("Wo", 8), [PR(b)])
                DVE(lambda e, b=b, hf=hf, tile=tile: e.tensor_tensor(out=h[:, tile, hf * 512:(hf + 1) * 512], in0=bank(b), in1=h[:, tile, hf * 512:(hf + 1) * 512], op=ALU.add),
                    [PR(b), ("h", tile)], [("h", tile)])
            yield

        interleaveX(S1t(0, 0))
        interleaveX(S1t(0, 1))
        interleaveX(QT_X(0))
        for g in range(8):
            nx = g + 1 < 8
            interleaveX(XA_X(2 * g), S1t(g + 1, 0) if nx else None)
            interleaveX(XA_X(2 * g + 1), router(2 * g, 0), S1t(g + 1, 1) if nx else None)
            interleaveX(QT_X(g + 1) if nx else None, router(2 * g + 1, 1))

        S.barrier()
        A.release(e_mark)
        A.limit = ARENA_BYTES

        if DBG:
            for tile in range(NT):
                DMA("sp", dbg_h[:, tile * D:(tile + 1) * D], h[:, tile, :], [("h", tile)], [("dbgh", tile)], "dbgh")
            S.barrier()
        if stop in ("X", "XNR", "X1"):
            raise _Stop()

        NR = 3
        Wg_r = [A.alloc([128, 8, 512], BF16) for _ in range(NR)]
        Wu_r = [A.alloc([128, 8, 512], BF16) for _ in range(NR)]
        Wd_r = [A.alloc([128, 4, D], BF16) for _ in range(NR)]
        xg4 = [A.alloc([128, 2, D], BF16) for _ in range(4)]
        xgT2 = [A.alloc([128, 8, 256], BF16) for _ in range(2)]
        hid2 = [A.alloc([128, 4, 256], BF16) for _ in range(2)]
        sge2 = [A.alloc([128, 256], F32) for _ in range(2)]
        ysb2 = [A.alloc([128, 2, D], BF16) for _ in range(2)]

        allxg = [("XG", tile, col) for tile in range(NT) for col in range(2)]
        gb_i = [0]
        def TG_E(ex):
            s = ex % NR
            bi = ex % 2
            DMA("pool", Wg_r[s], w_gate[ex].rearrange("(k p) n -> p k n", p=128), [], [("Wg", s, k) for k in range(8)], "wg%d" % s)
            DMA("pool", Wu_r[s], w_up[ex].rearrange("(k p) n -> p k n", p=128), [], [("Wu", s, k) for k in range(8)], "wu%d" % s)
            DMA("pool", Wd_r[s], w_down[ex].rearrange("(k p) n -> p k n", p=128), [], [("Wd", s, k) for k in range(4)], "wd%d" % s)
            for rb in range(2):
                pb = bank_bf(0)
                for k in range(8):
                    tp(pb[:, k, :], xg4[ex % 4][:, rb, k * 128:(k + 1) * 128], [("xg", ex % 4)], [PR(0)])
                if rb == 0:
                    ACT(lambda e, bi=bi, rb=rb: e.copy(out=xgT2[bi][:, :, rb * 128:(rb + 1) * 128], in_=bank_bf(0)), [PR(0)], [("xgT", bi, rb)])
                else:
                    DVE(lambda e, bi=bi, rb=rb: e.tensor_copy(out=xgT2[bi][:, :, rb * 128:(rb + 1) * 128], in_=bank_bf(0)), [PR(0)], [("xgT", bi, rb)])
            xgr = [("xgT", bi, 0), ("xgT", bi, 1)]
            for hc in range(4):
                gbk = 1 + (gb_i[0] % 2)
                ubk = 3 + (gb_i[0] % 2)
                gb_i[0] += 1
                for k in range(8):
                    mm(bank(gbk)[:, 0:256], Wg_r[s][:, k, hc * 128:(hc + 1) * 128], xgT2[bi][:, k, :], k == 0, k == 7, xgr + [("Wg", s, kk) for kk in range(8)], [PR(gbk)])
                for k in range(8):
                    mm(bank(ubk)[:, 0:256], Wu_r[s][:, k, hc * 128:(hc + 1) * 128], xgT2[bi][:, k, :], k == 0, k == 7, xgr + [("Wu", s, kk) for kk in range(8)], [PR(ubk)])
                sgb = sge2[hc % 2]
                ACT(lambda e, gbk=gbk, sgb=sgb: e.activation(out=sgb, in_=bank(gbk)[:, 0:256], func=AF.Sigmoid), [PR(gbk)], [("sge", hc % 2)])
                DVE(lambda e, gbk=gbk, sgb=sgb: e.tensor_tensor(out=sgb, in0=bank(gbk)[:, 0:256], in1=sgb, op=ALU.mult),
                    [PR(gbk), ("sge", hc % 2)], [("sge", hc % 2)])
                DVE(lambda e, ubk=ubk, sgb=sgb, bi=bi, hc=hc: e.tensor_tensor(out=hid2[bi][:, hc, :], in0=bank(ubk)[:, 0:256], in1=sgb, op=ALU.mult),
                    [PR(ubk), ("sge", hc % 2)], [("hid", bi, hc)])

        def DN_E(ex):
            s = ex % NR
            bi = ex % 2
            hr = [("hid", bi, hc) for hc in range(4)]
            for rb in range(2):
                for hf in range(2):
                    yb = 5 + ((rb * 2 + hf) % 3)
                    for hc in range(4):
                        mm(bank(yb), hid2[bi][:, hc, rb * 128:(rb + 1) * 128], Wd_r[s][:, hc, hf * 512:(hf + 1) * 512], hc == 0, hc == 3,
                           hr + [("Wd", s, kk) for kk in range(4)], [PR(yb)])
                    if hf == 0:
                        ACT(lambda e, yb=yb, bi=bi, rb=rb, hf=hf: e.copy(out=ysb2[bi][:, rb, hf * 512:(hf + 1) * 512], in_=bank(yb)), [PR(yb)], [("ysb", bi, rb, hf)])
                    else:
                        DVE(lambda e, yb=yb, bi=bi, rb=rb, hf=hf: e.tensor_copy(out=ysb2[bi][:, rb, hf * 512:(hf + 1) * 512], in_=bank(yb)), [PR(yb)], [("ysb", bi, rb, hf)])
            DMA("sp", YG[ex * CAP:(ex + 1) * CAP, :].rearrange("(b p) d -> p b d", p=128), ysb2[bi],
                [("ysb", bi, rb, hf) for rb in range(2) for hf in range(2)], [("YG", ex)], "yg%d" % bi)


        def XL_E(ex):
            DMA("sp", xg4[ex % 4], XG[ex * CAPR:ex * CAPR + CAP, :].rearrange("(b p) d -> p b d", p=128), allxg if ex < 4 else [], [("xg", ex % 4)], "xg%d" % (ex % 4))

        for ex in range(4):
            XL_E(ex)
        TG_E(0)
        for ex in range(NE):
            if ex + 4 < NE:
                XL_E(ex + 4)
            if ex + 1 < NE:
                TG_E(ex + 1)
            DN_E(ex)

        S.barrier()
        A.release(e_mark)

        gbc_C = A.alloc([128, D], F32)
        y12 = [[A.alloc([128, D], BF16) for _ in range(2)] for _ in range(2)]
        ot2 = [A.alloc([128, D], F32) for _ in range(2)]
        junk_C = A.alloc([128, D], BF16)
        DMA("sp", gbc_C, g_fin.partition_broadcast(128), [], ["gbc"], "gbc")
        outres = []

        def c_s1(tile):
            bi = tile % 2
            o_ = 24 + 3 * bi
            for col in range(2):
                S.add("pool", lambda e, col=col, bi=bi, tile=tile: e.indirect_dma_start(
                    out=y12[bi][col], out_offset=None, in_=YG, in_offset=bass.IndirectOffsetOnAxis(ap=gidx[:, tile, col:col + 1], axis=0)),
                    [("gidx", tile)], [("y12", bi, col)], dma=True, key="yga%d%d" % (bi, col))
            for col in range(2):
                DVE(lambda e, col=col, bi=bi, tile=tile: e.scalar_tensor_tensor(out=h[:, tile, :], in0=y12[bi][col], scalar=wts[:, tile, col:col + 1], in1=h[:, tile, :],
                                                                             op0=ALU.mult, op1=ALU.add),
                    [("y12", bi, col), ("wts", tile), ("h", tile)], [("h", tile)])
            ACT(lambda e, tile=tile, o_=o_: e.activation(out=junk_C, in_=h[:, tile, :], func=AF.Square, accum_out=stat[:, o_:o_ + 1]), [("h", tile)], ["junk", ("ssqC", bi)])
            ACT(lambda e, o_=o_: e.activation(out=stat[:, o_ + 1:o_ + 2], in_=stat[:, o_:o_ + 1], func=AF.Sqrt, bias=EPS, scale=1.0 / D), [("ssqC", bi)], [("stdC", bi)])

        def c_s2(tile):
            bi = tile % 2
            o_ = 24 + 3 * bi
            DVE(lambda e, o_=o_: e.reciprocal(out=stat[:, o_ + 2:o_ + 3], in_=stat[:, o_ + 1:o_ + 2]), [("stdC", bi)], [("rstdC", bi)])
            DVE(lambda e, tile=tile, bi=bi, o_=o_: e.scalar_tensor_tensor(out=ot2[bi], in0=h[:, tile, :], scalar=stat[:, o_ + 2:o_ + 3], in1=gbc_C, op0=ALU.mult, op1=ALU.mult),
                [("h", tile), ("rstdC", bi), "gbc"], [("ot", bi)])
            DMA("sp", out[tile * 128:(tile + 1) * 128, :], ot2[bi], [("ot", bi)], [("out", tile)], "out%d" % bi)
            outres.append(("out", tile))

        c_s1(0)
        for tile in range(NT):
            if tile + 1 < NT:
                c_s1(tile + 1)
            c_s2(tile)
        S.add("sp", None, outres)
        S.barrier()


    try:
        phases()
    except _Stop:
        S.barrier()

    S.resolve()
    sems = {}
    for e in ("pe", "act", "dve", "pool"):
        sems[("eng", e)] = es.enter_context(nc.semaphore("s_" + e))
    for k in S.keys:
        sems[("dma", k)] = es.enter_context(nc.semaphore("d_" + str(k)))
    with nc.Block() as block:
        block.sync(lambda e: S.run_engine("sp", e, sems))
        block.scalar(lambda e: S.run_engine("act", e, sems))
        block.vector(lambda e: S.run_engine("dve", e, sems))
        block.gpsimd(lambda e: S.run_engine("pool", e, sems))
        block.tensor(lambda e: S.run_engine("pe", e, sems))
    es.close()
    return nc, S, A


def _consts(half):
    p = np.arange(128, dtype=np.float64)
    inv_freq = 10000.0 ** (-np.arange(0, 64, 2, dtype=np.float64) / 64)

    def cs_tab(base):
        pos = base + np.arange(NT)[None, :] * 128 + p[:, None]
        ang = (pos[:, :, None].astype(np.float32) * inv_freq[None, None, :].astype(np.float32)).astype(np.float32)
        return np.stack([np.cos(ang), np.sin(ang)], axis=1).astype(np.float32)

    gam = 1.0 - 2.0 ** (-5.0 - np.arange(8, dtype=np.float64))
    lg = np.log(gam)
    gq = np.zeros((128, 4, 128), np.float32)
    gk = np.zeros((128, 4, 128), np.float32)
    ct = np.zeros((128, 4, 128), np.float32)
    i = np.arange(128, dtype=np.float64)
    for c in range(4):
        for hl in range(2):
            h = 2 * c + hl
            gq[hl * 64:(hl + 1) * 64, c, :] = np.exp((i + 1) * lg[h])[None, :]
            gk[hl * 64:(hl + 1) * 64, c, :] = (np.exp(-(i + 1) * lg[h]) / 8.0)[None, :]
            ct[hl * 64:(hl + 1) * 64, c, :] = np.exp(128 * lg[h])
    zt = (np.exp((127 - p)[:, None] * lg[None, :]) / 8.0).astype(np.float32)
    mask = (np.arange(128)[None, :] >= np.arange(128)[:, None]).astype(np.float32)
    ident = np.eye(128, dtype=np.float32)
    ustrict = (np.arange(128)[:, None] < np.arange(128)[None, :]).astype(np.float32)
    ones = np.ones((128, 128), np.float32)
    c_bf = np.concatenate([ident, ustrict, ones], axis=1)
    eb = np.concatenate([np.tile((np.arange(NE, dtype=np.float32) * CAPR)[None, :], (128, 1)),
                         np.tile((np.arange(NE, dtype=np.float32) * CAP)[None, :], (128, 1))], axis=1)
    return {
        "c_bf": c_bf, "c_cs_own": cs_tab(half * TOK), "c_cs_pre": cs_tab(0.0),
        "c_gq": gq, "c_gk": gk, "c_zt": zt, "c_ct": ct, "c_mask": np.ascontiguousarray(np.tile(mask[:, None, :], (1, 4, 1))), "c_eb": eb,
    }


_CACHE = {}


def kernel(x, mem, mix_norm_g, w_in, conv_w, w_conv_out, w_ret_out, w_mix_out,
           xa_norm_g, mem_norm_g, w_xa_q, w_xa_kv, w_xa_o, moe_norm_g,
           w_group, b_group, w_router, b_router, w_gate, w_up, w_down, final_norm_g):
    f = lambda a: np.ascontiguousarray(np.asarray(a, dtype=np.float32))
    x = f(x)
    mem = f(mem)
    if "nc" not in _CACHE:
        _CACHE["nc"] = build_program()
    nc = _CACHE["nc"][0]
    shared = {
        "w_in": f(w_in)[0], "conv_wT": np.ascontiguousarray(f(conv_w)[0].T), "w_conv_out": f(w_conv_out)[0],
        "w_ret_out": f(w_ret_out)[0], "w_mix_out": f(w_mix_out)[0], "w_xa_q": f(w_xa_q)[0], "w_xa_kv": f(w_xa_kv)[0],
        "w_xa_o": f(w_xa_o)[0], "g_mix": f(mix_norm_g)[0], "g_xa": f(xa_norm_g)[0], "g_mem": f(mem_norm_g)[0],
        "g_moe": f(moe_norm_g)[0], "g_fin": f(final_norm_g),
        "w_rt": np.ascontiguousarray(np.concatenate([f(w_group)[0], f(w_router)[0]], axis=1)),
        "b_rt": np.ascontiguousarray(np.concatenate([f(b_group)[0], f(b_router)[0]], axis=0)),
        "w_gate": f(w_gate)[0], "w_up": f(w_up)[0], "w_down": f(w_down)[0],
    }
    zeros = np.zeros((TOK, D), np.float32)
    in_maps = []
    for c in range(8):
        b, half = c // 2, c % 2
        m = dict(shared)
        m["xc"] = np.ascontiguousarray(x[b, half * TOK:(half + 1) * TOK])
        m["xp"] = np.ascontiguousarray(x[b, 0:TOK]) if half == 1 else zeros
        m["memc"] = np.ascontiguousarray(mem[b])
        m.update(_consts(half))
        in_maps.append(m)
    res = run_bass_kernel_spmd(nc, in_maps, core_ids=list(range(8)))
    _CACHE["res"] = res
    outp = np.empty((4, 2 * TOK, D), np.float32)
    for c in range(8):
        b, half = c // 2, c % 2
        outp[b, half * TOK:(half + 1) * TOK] = res.results[c]["out"]
    return outp
```

```python
import math
import numpy as np
from contextlib import ExitStack
import concourse.bass as bass
import concourse.mybir as mybir
from concourse.bass_utils import run_bass_kernel_spmd

F32 = mybir.dt.float32
BF16 = mybir.dt.bfloat16
I32 = mybir.dt.int32
U8 = mybir.dt.uint8
AF = mybir.ActivationFunctionType
ALU = mybir.AluOpType
AX = mybir.AxisListType

D = 1024
NT = 16
TOK = 2048
NE = 32
CAP = 256
CAPR = CAP + 1
EPS = 1e-6
INW = 6656
DBG = False


class Op:
    __slots__ = ("eng", "fn", "reads", "writes", "dma", "key", "idx", "sig", "ev", "deps", "xdeps", "bsize")

    def __init__(self, eng, fn, reads, writes, dma, key):
        self.eng = eng
        self.fn = fn
        self.reads = tuple(reads)
        self.writes = tuple(writes)
        self.dma = dma
        self.key = key
        self.sig = False
        self.ev = None
        self.deps = ()
        self.xdeps = ()
        self.bsize = 1


class Sched:
    ENGS = ("pe", "act", "dve", "pool", "sp")

    def __init__(self):
        self.ops = []
        self.last_eng = {}
        self.last_key = {}

    def add(self, eng, fn, reads=(), writes=(), dma=False, key=None, bsize=1):
        if dma:
            assert key is not None
        op = Op(eng, fn, reads, writes, dma, key)
        op.bsize = bsize
        op.idx = len(self.ops)
        self.ops.append(op)
        if dma:
            self.last_key[key] = op.idx
        elif fn is not None:
            self.last_eng[eng] = op.idx
        return op

    def barrier(self):
        deps = tuple(self.last_eng.values()) + tuple(self.last_key.values())
        for e in self.ENGS:
            op = self.add(e, None)
            op.xdeps = deps

    def resolve(self):
        last_w = {}
        readers = {}
        for op in self.ops:
            deps = set(op.xdeps)
            for r in op.reads:
                w = last_w.get(r)
                if w is not None:
                    deps.add(w)
            for w_ in op.writes:
                w = last_w.get(w_)
                if w is not None:
                    deps.add(w)
                for rd in readers.get(w_, {}).values():
                    deps.add(rd)
            deps.discard(op.idx)
            dl = []
            for d in sorted(deps):
                dop = self.ops[d]
                if dop.fn is None:
                    continue
                if op.eng == "pe" and dop.eng == "pe" and not dop.dma and not op.dma:
                    continue
                dop.sig = True
                dl.append(d)
            op.deps = tuple(dl)
            rk = ("dma", op.idx) if op.dma else op.eng
            for r in op.reads:
                readers.setdefault(r, {})[rk] = op.idx
            for w_ in op.writes:
                last_w[w_] = op.idx
                readers[w_] = {}
        cnt = {e: 0 for e in self.ENGS}
        keycnt = {}
        import os
        if os.environ.get("ALLSIG"):
            for op in self.ops:
                if not op.dma and op.fn is not None and op.eng != "sp":
                    op.sig = True
        for op in self.ops:
            if op.dma:
                keycnt[op.key] = keycnt.get(op.key, 0) + 16
                q = 16 * op.bsize
                op.ev = (("dma", op.key), (keycnt[op.key] + q - 1) // q * q)
            elif op.sig:
                cnt[op.eng] += 1
                op.ev = (("eng", op.eng), cnt[op.eng])
        self.keys = list(keycnt.keys())
        self.cnt = cnt
        return self

    def run_engine(self, eng, eobj, sems):
        waited = {}
        for op in self.ops:
            if op.eng != eng:
                continue
            need = {}
            for d in op.deps:
                sk, val = self.ops[d].ev
                if need.get(sk, 0) < val:
                    need[sk] = val
            for sk, val in need.items():
                if waited.get(sk, 0) >= val:
                    continue
                eobj.wait_ge(sems[sk], val)
                waited[sk] = val
            if op.fn is None:
                continue
            ins = op.fn(eobj)
            if op.dma:
                ins.then_inc(sems[op.ev[0]], 16)
            elif op.sig:
                ins.then_inc(sems[op.ev[0]], 1)


_DTSZ = {F32: 4, BF16: 2, I32: 4, U8: 1}


class Arena:
    def __init__(self, ap, size):
        self.ap = ap
        self.size = size
        self.off = 0
        self.peak = 0
        self.limit = size

    def mark(self):
        return self.off

    def release(self, m):
        self.off = m

    def alloc(self, shape, dt):
        n = 1
        for s in shape[1:]:
            n *= s
        nbytes = n * _DTSZ[dt]
        off = (self.off + 31) // 32 * 32
        assert off + nbytes <= self.limit, ("SBUF arena overflow", off, nbytes, self.limit)
        self.off = off + nbytes
        self.peak = max(self.peak, self.off)
        v = self.ap[:, off:off + nbytes].bitcast(dt)
        if len(shape) == 3:
            v = v.rearrange("p (a b) -> p a b", a=shape[1])
        elif len(shape) == 4:
            v = v.rearrange("p (a b c) -> p a b c", a=shape[1], b=shape[2])
        return v


def build_program(stop=None):
    nc = bass.Bass("TRN2", target_bir_lowering=False)

    def din(name, shape, dt=F32):
        return nc.dram_tensor(name, list(shape), dt, kind="ExternalInput").ap()

    xc = din("xc", [TOK, D])
    xp = din("xp", [TOK, D])
    memc = din("memc", [256, D])
    w_in = din("w_in", [D, INW])
    conv_wT = din("conv_wT", [512, 3])
    w_conv_out = din("w_conv_out", [512, D])
    w_ret_out = din("w_ret_out", [D, D])
    w_mix_out = din("w_mix_out", [D, D])
    w_xa_q = din("w_xa_q", [D, D])
    w_xa_kv = din("w_xa_kv", [D, 2 * D])
    w_xa_o = din("w_xa_o", [D, D])
    g_mix = din("g_mix", [D])
    g_xa = din("g_xa", [D])
    g_mem = din("g_mem", [D])
    g_moe = din("g_moe", [D])
    g_fin = din("g_fin", [D])
    w_rt = din("w_rt", [D, 36])
    b_rt = din("b_rt", [36])
    w_gate = din("w_gate", [NE, D, 512])
    w_up = din("w_up", [NE, D, 512])
    w_down = din("w_down", [NE, 512, D])
    c_bf = din("c_bf", [128, 384])
    c_cs_own = din("c_cs_own", [128, 2, NT, 32])
    c_cs_pre = din("c_cs_pre", [128, 2, NT, 32])
    c_gq = din("c_gq", [128, 4, 128])
    c_gk = din("c_gk", [128, 4, 128])
    c_zt = din("c_zt", [128, 8])
    c_ct = din("c_ct", [128, 4, 128])
    c_mask = din("c_mask", [128, 4, 128])
    c_eb = din("c_eb", [128, 2 * NE])
    out = nc.dram_tensor("out", [TOK, D], F32, kind="ExternalOutput").ap()
    XG = nc.dram_tensor("xg_scr", [NE * CAPR, D], BF16, kind="Internal").ap()
    YG = nc.dram_tensor("yg_scr", [NE * CAP, D], BF16, kind="Internal").ap()
    if DBG:
        dbg_m = nc.dram_tensor("dbg_m", [128, 8 * TOK], F32, kind="ExternalOutput").ap()
        dbg_h = nc.dram_tensor("dbg_h", [128, NT * D], F32, kind="ExternalOutput").ap()

    S = Sched()
    es = ExitStack()
    ARENA_BYTES = 207 * 1024
    arena_t = es.enter_context(nc.sbuf_tensor("arena", [128, ARENA_BYTES], U8))
    A = Arena(arena_t, ARENA_BYTES)
    ps_t = es.enter_context(nc.psum_tensor("ps", [128, 4096], F32))

    def bank(i, n=1):
        return ps_t[:, i * 512:(i + n) * 512]

    def bank_bf(i):
        return ps_t[:, i * 512:(i + 1) * 512].bitcast(BF16).rearrange("p (a b) -> p a b", a=8)

    def PR(i):
        return ("ps", i)

    uid = [0]

    def ukey(p):
        uid[0] += 1
        return "%s%d" % (p, uid[0])

    def PE(fn, r, w):
        return S.add("pe", fn, r, w)

    def ACT(fn, r, w):
        return S.add("act", fn, r, w)

    def DVE(fn, r, w):
        return S.add("dve", fn, r, w)

    def POOL(fn, r, w):
        return S.add("pool", fn, r, w)

    def DMA(eng, out_, in_, r, w, key, nb=1):
        return S.add(eng, lambda e: e.dma_start(out=out_, in_=in_), r, w, dma=True, key=key, bsize=nb)

    def mm(out_, lhsT, rhs, start, stop, r, w):
        return PE(lambda e: e.matmul(out_, lhsT=lhsT, rhs=rhs, start=start, stop=stop), r, w)

    def tp(out_, in_, r, w):
        return PE(lambda e: e.transpose(out=out_, in_=in_, identity=ident), r + ["ident"], w)

    def load_w(dst, src, rows_k, res, key):
        for k in range(rows_k):
            DMA("pool", dst[:, k, :], src[k * 128:(k + 1) * 128, :], [], [(res, k)], key, nb=rows_k)

    def wres(res, n):
        return [(res, k) for k in range(n)]

    ident3 = A.alloc([128, 3, 128], BF16)
    ident = ident3[:, 0, :]
    ustrict = ident3[:, 1, :]
    onesb = ident3[:, 2, :]
    DMA("pool", ident3, c_bf.rearrange("p (a b) -> p a b", a=3), [], ["ident"], "c_bf")
    MT_BYTES = 8 * TOK * 2
    mergedT = arena_t[:, ARENA_BYTES - MT_BYTES:ARENA_BYTES].bitcast(BF16).rearrange("p (a b) -> p a b", a=8)
    A.limit = ARENA_BYTES - MT_BYTES
    stat = A.alloc([128, 64], F32)
    wts = A.alloc([128, NT, 2], F32)
    sidx = A.alloc([128, NT, 2], I32)
    gidx = A.alloc([128, NT, 2], I32)
    ssq = stat[:, 0:1]
    std = stat[:, 1:2]
    rstd = stat[:, 2:3]
    persist_mark = A.mark()

    def rmsnorm(xt_ap, xt_res, g_bc, xs_ap, junk_ap, xs_res="xs"):
        ACT(lambda e: e.activation(out=junk_ap, in_=xt_ap, func=AF.Square, accum_out=ssq), [xt_res], ["junk", "ssq"])
        ACT(lambda e: e.activation(out=std, in_=ssq, func=AF.Sqrt, bias=EPS, scale=1.0 / D), ["ssq"], ["std"])
        DVE(lambda e: e.reciprocal(out=rstd, in_=std), ["std"], ["rstd"])
        DVE(lambda e: e.scalar_tensor_tensor(out=xs_ap, in0=xt_ap, scalar=rstd, in1=g_bc, op0=ALU.mult, op1=ALU.mult),
            [xt_res, "rstd", "gbc"], [xs_res])

    def transpose8(src_ap, src_res, dst_ap, dst_res, nblk=8, copy_eng="act"):
        pb = bank_bf(0)
        for k in range(nblk):
            tp(pb[:, k, :], src_ap[:, k * 128:(k + 1) * 128], [src_res], [PR(0)])
        if copy_eng == "act":
            ACT(lambda e: e.copy(out=dst_ap, in_=pb[:, 0:nblk, :]), [PR(0)], [dst_res])
        else:
            DVE(lambda e: e.tensor_copy(out=dst_ap, in_=pb[:, 0:nblk, :]), [PR(0)], [dst_res])

    def dump_m():
        dtmp = A.alloc([128, 8, 512], F32)
        for g in range(4):
            DVE(lambda e, g=g: e.tensor_copy(out=dtmp, in_=mergedT[:, :, g * 512:(g + 1) * 512]), [("mT", oc, g) for oc in range(8)] + ["dbgo"], ["dtmp"])
            DMA("sp", dbg_m.rearrange("p (k t) -> p k t", k=8)[:, :, g * 512:(g + 1) * 512], dtmp, ["dtmp"], ["dbgo"], "dbgo")
        S.barrier()

    class _Stop(Exception):
        pass

    def phases():
        WinA = A.alloc([128, 8, 2560], BF16)
        Wco = A.alloc([128, 4, 1024], BF16)
        gbc_A = A.alloc([128, D], F32)
        convw = A.alloc([128, 4, 3], F32)
        xt2_A = [A.alloc([128, D], F32) for _ in range(2)]
        xs_A = A.alloc([128, D], BF16)
        junk_A = A.alloc([128, D], BF16)
        xnT4 = [A.alloc([128, 8, 512], BF16) for _ in range(2)]
        xin_sb = A.alloc([128, 4, 512], F32)
        u = A.alloc([128, 4, 514], F32)
        cc = A.alloc([128, 4, 512], F32)
        bc = A.alloc([128, 4, 512], BF16)
        sg2 = [A.alloc([128, 512], F32) for _ in range(2)]

        zt_ = A.alloc([128, 4112], BF16)
        POOL(lambda e: e.memset(zt_, 0.0), [], ["zfill"])
        XGf = XG.rearrange("r d -> (r d)").rearrange("(p n) -> p n", p=128)
        DMA("sp", gbc_A, g_mix.partition_broadcast(128), [], ["gbc"], "gbc")
        DMA("sp", convw, conv_wT.rearrange("(c p) k -> p c k", p=128), [], ["convw"], "convw")
        for k in range(8):
            DMA("pool", WinA[:, k, 0:1536], w_in[k * 128:(k + 1) * 128, 0:1536], [], [("WinA", k)], "WinA", nb=8)
        for k in range(4):
            DMA("pool", Wco[:, k, :], w_conv_out[k * 128:(k + 1) * 128, :], [], [("Wco", k)], "Wco", nb=4)
        for k in range(8):
            DMA("pool", WinA[:, k, 1536:2560], w_in[k * 128:(k + 1) * 128, 4608:5632], [], [("WinAg", k)], "WinAg", nb=8)
        DMA("sp", xt2_A[1], xp[TOK - 128:TOK, :], [], [("xt", 1)], "xt1")
        rmsnorm(xt2_A[1], ("xt", 1), gbc_A, xs_A, junk_A, xs_res=("xsA", 0))
        transpose8(xs_A, ("xsA", 0), xnT4[1][:, :, 0:128], ("xnT4", 1, 0))
        for c in range(4):
            for k in range(8):
                mm(bank(1)[:, 0:128], WinA[:, k, c * 128:(c + 1) * 128], xnT4[1][:, k, 0:128], k == 0, k == 7, [("xnT4", 1, 0)] + wres("WinA", 8), [PR(1)])
            ACT(lambda e, c=c: e.copy(out=xin_sb[:, c, 0:128], in_=bank(1)[:, 0:128]), [PR(1)], [("xin", c)])
            for k in range(8):
                mm(bank(2)[:, 0:128], WinA[:, k, 1024 + c * 128:1024 + (c + 1) * 128], xnT4[1][:, k, 0:128], k == 0, k == 7, [("xnT4", 1, 0)] + wres("WinA", 8), [PR(2)])
            DVE(lambda e, c=c: e.tensor_tensor(out=u[:, c, 0:2], in0=bank(2)[:, 126:128], in1=xin_sb[:, c, 126:128], op=ALU.mult),
                [PR(2), ("xin", c)], ["u"])

        projA = [1, 2, 3, 6, 7]
        pa_i = [0]

        def next_proj(pool):
            b = pool[pa_i[0] % len(pool)]
            pa_i[0] += 1
            return b

        xs4 = [xs_A] + [A.alloc([128, D], BF16) for _ in range(3)]
        sg8 = list(sg2) + [A.alloc([128, 512], F32) for _ in range(6)]

        def hnA(g, t):
            tile = g * 4 + t
            xb = xt2_A[tile % 2]
            xr = ("xt", tile % 2)
            DMA("sp", xb, xc[tile * 128:(tile + 1) * 128, :], [], [xr], "xt%d" % (tile % 2))
            ACT(lambda e: e.activation(out=junk_A, in_=xb, func=AF.Square, accum_out=ssq), [xr], ["junk", "ssq"])
            ACT(lambda e: e.activation(out=std, in_=ssq, func=AF.Sqrt, bias=EPS, scale=1.0 / D), ["ssq"], ["std"])
            DVE(lambda e: e.reciprocal(out=rstd, in_=std), ["std"], ["rstd"])
            DVE(lambda e: e.scalar_tensor_tensor(out=xs4[t], in0=xb, scalar=rstd, in1=gbc_A, op0=ALU.mult, op1=ALU.mult),
                [xr, "rstd", "gbc"], [("xsA", t)])

        def htA(g, t):
            transpose8(xs4[t], ("xsA", t), xnT4[g % 2][:, :, t * 128:(t + 1) * 128], ("xnT4", g % 2, t))

        def xinA(g):
            gb = g % 2
            xnr = [("xnT4", gb, t) for t in range(4)]
            for c in range(4):
                b = next_proj(projA)
                for k in range(8):
                    mm(bank(b), WinA[:, k, c * 128:(c + 1) * 128], xnT4[gb][:, k, :], k == 0, k == 7, xnr + wres("WinA", 8), [PR(b)])
                ACT(lambda e, b=b, c=c: e.copy(out=xin_sb[:, c, :], in_=bank(b)), [PR(b)], [("xin", c)])

        def cgA(g):
            gb = g % 2
            xnr = [("xnT4", gb, t) for t in range(4)]
            for c in range(4):
                b = next_proj(projA)
                for k in range(8):
                    mm(bank(b), WinA[:, k, 1024 + c * 128:1024 + (c + 1) * 128], xnT4[gb][:, k, :], k == 0, k == 7, xnr + wres("WinA", 8), [PR(b)])
                DVE(lambda e, b=b, c=c: e.tensor_tensor(out=u[:, c, 2:514], in0=bank(b), in1=xin_sb[:, c, :], op=ALU.mult),
                    [PR(b), ("xin", c)], ["u"])
                POOL(lambda e, c=c: e.tensor_scalar(out=cc[:, c, :], in0=u[:, c, 0:512], scalar1=convw[:, c, 0:1], scalar2=None, op0=ALU.mult),
                     ["u", "convw"], [("cc", c)])
                DVE(lambda e, c=c: e.scalar_tensor_tensor(out=cc[:, c, :], in0=u[:, c, 1:513], scalar=convw[:, c, 1:2], in1=cc[:, c, :], op0=ALU.mult, op1=ALU.add),
                    ["u", "convw", ("cc", c)], [("cc", c)])
                DVE(lambda e, c=c: e.scalar_tensor_tensor(out=cc[:, c, :], in0=u[:, c, 2:514], scalar=convw[:, c, 2:3], in1=cc[:, c, :], op0=ALU.mult, op1=ALU.add),
                    ["u", "convw", ("cc", c)], [("cc", c)])
            POOL(lambda e: e.tensor_copy(out=u[:, :, 0:2], in_=u[:, :, 512:514]), ["u"], ["u"])

        def bgA(g):
            gb = g % 2
            xnr = [("xnT4", gb, t) for t in range(4)]
            for c in range(4):
                b = next_proj(projA)
                for k in range(8):
                    mm(bank(b), WinA[:, k, 512 + c * 128:512 + (c + 1) * 128], xnT4[gb][:, k, :], k == 0, k == 7, xnr + wres("WinA", 8), [PR(b)])
                DVE(lambda e, b=b, c=c: e.tensor_tensor(out=bc[:, c, :], in0=bank(b), in1=cc[:, c, :], op=ALU.mult),
                    [PR(b), ("cc", c)], [("bc", c)])

        def gateA(g, ocs):
            gb = g % 2
            xnr = [("xnT4", gb, t) for t in range(4)]
            for oc in ocs:
                b = next_proj(projA)
                for k in range(8):
                    mm(bank(b), WinA[:, k, 1536 + oc * 128:1536 + (oc + 1) * 128], xnT4[gb][:, k, :], k == 0, k == 7, xnr + wres("WinAg", 8), [PR(b)])
                ACT(lambda e, b=b, oc=oc: e.activation(out=sg8[oc], in_=bank(b), func=AF.Sigmoid), [PR(b)], [("sg", oc)])

        def yconvA(g, ocs):
            for oc in ocs:
                yb = 4 + (oc % 2)
                for k in range(4):
                    mm(bank(yb), Wco[:, k, oc * 128:(oc + 1) * 128], bc[:, k, :], k == 0, k == 3, [("bc", kk) for kk in range(4)] + wres("Wco", 4), [PR(yb)])
                DVE(lambda e, yb=yb, oc=oc, g=g: e.tensor_tensor(out=mergedT[:, oc, g * 512:(g + 1) * 512], in0=bank(yb), in1=sg8[oc], op=ALU.mult),
                    [PR(yb), ("sg", oc)], [("mT", oc, g)])

        for t in range(4):
            hnA(0, t)
            htA(0, t)
        for g in range(4):
            nx = g + 1 if g + 1 < 4 else None
            if nx is not None:
                hnA(nx, 0)
            xinA(g)
            if nx is not None:
                htA(nx, 0)
                hnA(nx, 1)
            cgA(g)
            if nx is not None:
                htA(nx, 1)
                hnA(nx, 2)
            gateA(g, range(0, 4))
            bgA(g)
            if nx is not None:
                htA(nx, 2)
                hnA(nx, 3)
            gateA(g, range(4, 8))
            if nx is not None:
                htA(nx, 3)
            yconvA(g, range(8))
            if g == 0:
                for i in range(16):
                    DMA("sp", XGf[:, i * 4112:(i + 1) * 4112], zt_, ["zfill"], [("XGz", i)], "xgz", nb=16)

        S.barrier()
        A.release(persist_mark)
        if stop == "A":
            dump_m()
            raise _Stop()

        WinB = A.alloc([128, 8, 4096], BF16)
        Wro = A.alloc([128, 8, 1024], BF16)
        gbc_B = A.alloc([128, D], F32)
        cs = A.alloc([128, 2, NT, 32], F32)
        gq = A.alloc([128, 4, 128], F32)
        gk = A.alloc([128, 4, 128], F32)
        zt = A.alloc([128, 8], F32)
        ct = A.alloc([128, 4, 128], F32)
        maskT = A.alloc([128, 4, 128], F32)
        Sst = A.alloc([128, 4, 128], F32)
        Sb = A.alloc([128, 4, 128], BF16)
        xt2_B = [A.alloc([128, D], F32) for _ in range(2)]
        xs_B = A.alloc([128, D], BF16)
        junk_B = A.alloc([128, D], BF16)
        xnT4b2 = [A.alloc([128, 8, 512], BF16) for _ in range(2)]
        xs_B2 = [xs_B, A.alloc([128, D], BF16)]
        qr2 = [A.alloc([128, 8, 2, 32], BF16) for _ in range(2)]
        kr2 = [A.alloc([128, 8, 2, 32], BF16) for _ in range(2)]
        kz2 = [A.alloc([128, 8, 64], BF16) for _ in range(2)]
        v2 = [A.alloc([128, D], BF16) for _ in range(2)]
        sgt2 = [A.alloc([128, D], BF16) for _ in range(2)]
        rt = [A.alloc([128, 8, 32], F32) for _ in range(4)]
        qTz = A.alloc([128, 4, 2, 128], BF16)
        kT = A.alloc([128, 4, 128], BF16)
        PT = A.alloc([128, 8, 128], BF16)
        osq = A.alloc([128, D], F32)
        zb = A.alloc([128, D], BF16)
        zT4 = A.alloc([128, 8, 512], BF16)
        sgr2 = [A.alloc([128, 512], F32) for _ in range(2)]
        tmpm = A.alloc([128, 512], F32)
        gst = A.alloc([128, 64], F32)

        DMA("sp", gbc_B, g_mix.partition_broadcast(128), [], ["gbc"], "gbc")
        DMA("sp", cs, c_cs_pre, [], ["cs"], "cs")
        DMA("sp", gq, c_gq, [], ["gq"], "c_gq")
        DMA("sp", gk, c_gk, [], ["gk"], "c_gk")
        DMA("sp", zt, c_zt, [], ["zt"], "c_zt")
        DMA("sp", ct, c_ct, [], ["ct"], "c_ct")
        DMA("sp", maskT, c_mask, [], ["maskT"], "c_mask")
        for k in range(8):
            DMA("pool", WinB[:, k, 512:2048], w_in[k * 128:(k + 1) * 128, 2048:3584], [], [("WinBkv", k)], "WinBkv", nb=8)
        for k in range(8):
            DMA("pool", WinB[:, k, 0:512], w_in[k * 128:(k + 1) * 128, 1536:2048], [], [("WinBq", k)], "WinBq", nb=8)
        for k in range(8):
            DMA("pool", WinB[:, k, 2048:3072], w_in[k * 128:(k + 1) * 128, 3584:4608], [], [("WinBg", k)], "WinBg", nb=8)
        for k in range(8):
            DMA("pool", WinB[:, k, 3072:4096], w_in[k * 128:(k + 1) * 128, 5632:6656], [], [("WinBr", k)], "WinBr", nb=8)
        load_w(Wro, w_ret_out, 8, "Wro", "Wro")
        POOL(lambda e: e.memset(Sst, 0.0), [], ["Sst"])
        POOL(lambda e: e.memset(Sb, 0.0), [], ["Sb"])
        POOL(lambda e: e.memset(qTz, 0.0), [], ["qT"])

        projB = [1, 2, 7]
        pa_i[0] = 0

        def rotary(pb_ap, pres, tile, dst, dres, ti):
            pv = pb_ap.rearrange("p (h t f) -> p h t f", h=8, t=2)
            cosb = cs[:, 0, tile, :].unsqueeze(1).broadcast_to([128, 8, 32])
            sinb = cs[:, 1, tile, :].unsqueeze(1).broadcast_to([128, 8, 32])
            ta, tb, tc, td = rt
            DVE(lambda e: e.tensor_tensor(out=ta, in0=pv[:, :, 0, :], in1=cosb, op=ALU.mult), [pres, "cs"], ["rta"])
            DVE(lambda e: e.tensor_tensor(out=tb, in0=pv[:, :, 1, :], in1=sinb, op=ALU.mult), [pres, "cs"], ["rtb"])
            DVE(lambda e: e.tensor_tensor(out=tc, in0=pv[:, :, 0, :], in1=sinb, op=ALU.mult), [pres, "cs"], ["rtc"])
            DVE(lambda e: e.tensor_tensor(out=td, in0=pv[:, :, 1, :], in1=cosb, op=ALU.mult), [pres, "cs"], ["rtd"])
            POOL(lambda e: e.tensor_tensor(out=dst[:, :, 0, :], in0=ta, in1=tb, op=ALU.subtract), ["rta", "rtb"], [(dres, 0)])
            POOL(lambda e: e.tensor_tensor(out=dst[:, :, 1, :], in0=tc, in1=td, op=ALU.add), ["rtc", "rtd"], [(dres, 1)])

        def state_update(bi):
            kzv = kz2[bi].rearrange("p h d -> p (h d)")
            for c in range(4):
                ob = ps_t[:, 3 * 512 + c * 256: 3 * 512 + (c + 1) * 256]
                mm(ob, kzv[:, c * 128:(c + 1) * 128], v2[bi][:, c * 256:(c + 1) * 256], True, True,
                   [("kz", bi), ("v", bi)], [PR(3), PR(4)])
            pS = bank(3, 2).rearrange("p (c n) -> p c n", c=4)
            POOL(lambda e: e.tensor_tensor(out=Sst, in0=Sst, in1=ct, op=ALU.mult), ["Sst", "ct"], ["Sst"])
            DVE(lambda e: e.tensor_tensor(out=Sst[0:64], in0=Sst[0:64], in1=pS[0:64, :, 0:128], op=ALU.add), ["Sst", PR(3), PR(4)], ["Sst"])
            DVE(lambda e: e.tensor_tensor(out=Sst[64:128], in0=Sst[64:128], in1=pS[64:128, :, 128:256], op=ALU.add), ["Sst", PR(3), PR(4)], ["Sst"])
            ACT(lambda e: e.copy(out=Sb, in_=Sst), ["Sst"], ["Sb"])

        def proj_tm(xnT_ap, xn_res, col0, wres_, b):
            for k in range(8):
                mm(bank(b), xnT_ap[:, k, :], WinB[:, k, col0:col0 + 512], k == 0, k == 7, xn_res + wres_, [PR(b)])

        def RPp(tile):
            bi = tile % 2
            xb = xt2_B[bi]
            xr = ("xt", bi)
            DMA("sp", xb, xp[tile * 128:(tile + 1) * 128, :], [], [xr], "xt%d" % bi)
            rmsnorm(xb, xr, gbc_B, xs_B, junk_B, xs_res=("xsB", 0))
            transpose8(xs_B, ("xsB", 0), xnT4b2[1][:, :, (tile % 4) * 128:(tile % 4 + 1) * 128], ("xnT4b", 1, tile % 4))

        def PPp(tile):
            bi = tile % 2
            xnTp_ = xnT4b2[1][:, :, (tile % 4) * 128:(tile % 4 + 1) * 128]
            xres_ = [("xnT4b", 1, tile % 4)]
            b = next_proj(projB)
            proj_tm(xnTp_, xres_, 512, wres("WinBkv", 8), b)
            rotary(bank(b), PR(b), tile, kr2[bi], ("kr", bi), 1)
            POOL(lambda e, bi=bi: e.tensor_tensor(out=kz2[bi], in0=kr2[bi].rearrange("p h t f -> p h (t f)"),
                                                  in1=zt.unsqueeze(2).broadcast_to([128, 8, 64]), op=ALU.mult),
                 [(("kr", bi), 0), (("kr", bi), 1), "zt"], [("kz", bi)])
            for hf in range(2):
                b = next_proj(projB)
                proj_tm(xnTp_, xres_, 1024 + hf * 512, wres("WinBkv", 8), b)
                ACT(lambda e, b=b, bi=bi, hf=hf: e.copy(out=v2[bi][:, hf * 512:(hf + 1) * 512], in_=bank(b)), [PR(b)], [("v", bi)])
            state_update(bi)

        RPp(0)
        for tile in range(NT):
            if tile + 1 < NT:
                RPp(tile + 1)
            PPp(tile)

        if stop == "B1":
            raise _Stop()

        def chk(n):
            if stop == "B2:%d" % n:
                raise _Stop()
        DMA("sp", cs, c_cs_own, [], ["cs"], "cs")

        def RB(g, t):
            tile = g * 4 + t
            bi = tile % 2
            xb = xt2_B[bi]
            xr = ("xt", bi)
            DMA("sp", xb, xc[tile * 128:(tile + 1) * 128, :], [], [xr], "xt%d" % bi)
            rmsnorm(xb, xr, gbc_B, xs_B2[bi], junk_B, xs_res=("xsB", bi))

        def RBt(g, t):
            tile = g * 4 + t
            bi = tile % 2
            xnT_t = xnT4b2[g % 2][:, :, t * 128:(t + 1) * 128]
            transpose8(xs_B2[bi], ("xsB", bi), xnT_t, ("xnT4b", g % 2, t))

        def PB(g, t):
            tile = g * 4 + t
            bi = tile % 2
            xnT_t = xnT4b2[g % 2][:, :, t * 128:(t + 1) * 128]
            xnres = [("xnT4b", g % 2, t)]
            b = next_proj(projB)
            proj_tm(xnT_t, xnres, 0, wres("WinBq", 8), b)
            rotary(bank(b), PR(b), tile, qr2[bi], ("qr", bi), 0)
            b = next_proj(projB)
            proj_tm(xnT_t, xnres, 512, wres("WinBkv", 8), b)
            rotary(bank(b), PR(b), tile, kr2[bi], ("kr", bi), 1)
            POOL(lambda e, bi=bi: e.tensor_tensor(out=kz2[bi], in0=kr2[bi].rearrange("p h t f -> p h (t f)"),
                                                  in1=zt.unsqueeze(2).broadcast_to([128, 8, 64]), op=ALU.mult),
                 [(("kr", bi), 0), (("kr", bi), 1), "zt"], [("kz", bi)])
            yield
            for hf in range(2):
                b = next_proj(projB)
                proj_tm(xnT_t, xnres, 1024 + hf * 512, wres("WinBkv", 8), b)
                ACT(lambda e, b=b, bi=bi, hf=hf: e.copy(out=v2[bi][:, hf * 512:(hf + 1) * 512], in_=bank(b)), [PR(b)], [("v", bi)])
            yield
            for hf in range(2):
                b = next_proj(projB)
                proj_tm(xnT_t, xnres, 2048 + hf * 512, wres("WinBg", 8), b)
                ACT(lambda e, b=b, hf=hf: e.activation(out=sgr2[hf], in_=bank(b), func=AF.Sigmoid), [PR(b)], [("sgr", hf)])
                DVE(lambda e, b=b, bi=bi, hf=hf: e.tensor_tensor(out=sgt2[bi][:, hf * 512:(hf + 1) * 512], in0=bank(b), in1=sgr2[hf], op=ALU.mult),
                    [PR(b), ("sgr", hf)], [("sgt", bi)])

        def tailB(g, t):
            tile = g * 4 + t
            bi = tile % 2
            pb = bank_bf(0)
            qrv = qr2[bi].rearrange("p h t f -> p (h t f)")
            krv = kr2[bi].rearrange("p h t f -> p (h t f)")
            for c in range(4):
                tp(pb[:, c, :], qrv[:, c * 128:(c + 1) * 128], [(("qr", bi), 0), (("qr", bi), 1)], [PR(0)])
            for c in range(4):
                tp(pb[:, 4 + c, :], krv[:, c * 128:(c + 1) * 128], [(("kr", bi), 0), (("kr", bi), 1)], [PR(0)])
            DVE(lambda e: e.tensor_tensor(out=qTz[0:64, :, 0, :], in0=bank_bf(0)[0:64, 0:4, :], in1=gq[0:64], op=ALU.mult), [PR(0), "gq"], ["qT"])
            DVE(lambda e: e.tensor_tensor(out=qTz[64:128, :, 1, :], in0=bank_bf(0)[64:128, 0:4, :], in1=gq[64:128], op=ALU.mult), [PR(0), "gq", "qT"], ["qT"])
            DVE(lambda e: e.tensor_tensor(out=kT, in0=bank_bf(0)[:, 4:8, :], in1=gk, op=ALU.mult), [PR(0), "gk"], ["kT"])
            yield
            for c in range(4):
                mm(ps_t[:, 3 * 512 + c * 256: 3 * 512 + (c + 1) * 256], kT[:, c, :], qTz[:, c, :, :].rearrange("p a q -> p (a q)"), True, True,
                   ["qT", "kT"], [PR(3 + c // 2)])
            mb = maskT
            DVE(lambda e, mb=mb: e.tensor_tensor(out=PT[:, 0:4, :], in0=bank(3).rearrange("p (h q) -> p h q", h=4), in1=mb, op=ALU.mult),
                [PR(3), "maskT"], [("PT", 0)])
            DVE(lambda e, mb=mb: e.tensor_tensor(out=PT[:, 4:8, :], in0=bank(4).rearrange("p (h q) -> p h q", h=4), in1=mb, op=ALU.mult),
                [PR(4), "maskT"], [("PT", 1)])
            yield
            for h in range(8):
                p0 = (h % 2) * 64
                ob = ps_t[:, 5 * 512 + h * 128: 5 * 512 + (h + 1) * 128]
                mm(ob, PT[:, h, :], v2[bi][:, h * 128:(h + 1) * 128], True, False, [("PT", h // 4), ("v", bi)], [PR(5 + h // 4)])
                mm(ob, qTz[:, h // 2, h % 2, :], Sb[:, h // 2, :], False, True, ["qT", "Sb"], [PR(5 + h // 4)])
            yield
            for hb in range(2):
                ACT(lambda e, hb=hb: e.copy(out=osq[:, hb * 512:(hb + 1) * 512], in_=bank(5 + hb)), [PR(5 + hb)],
                    [("osq", hb)] + [("on", hh_) for hh_ in range(hb * 4, hb * 4 + 4)])
            DVE(lambda e: e.reduce_sum(out=gst[:, 0:8], in_=osq.rearrange("p (h e) -> p h e", h=8), axis=AX.X), [("osq", 0), ("osq", 1)], ["g_sum"])
            for h in range(8):
                ACT(lambda e, h=h: e.activation(out=junk_B[:, h * 128:(h + 1) * 128], in_=osq[:, h * 128:(h + 1) * 128], func=AF.Square,
                                                accum_out=gst[:, 8 + h:9 + h]),
                    [("osq", h // 4)], ["junk", ("g_sq", h)])
            DVE(lambda e: e.tensor_scalar(out=gst[:, 16:24], in0=gst[:, 0:8], scalar1=1.0 / 128, scalar2=None, op0=ALU.mult), ["g_sum"], ["g_mean"])
            DVE(lambda e: e.tensor_tensor(out=gst[:, 24:32], in0=gst[:, 16:24], in1=gst[:, 16:24], op=ALU.mult), ["g_mean"], ["g_msq"])
            DVE(lambda e: e.scalar_tensor_tensor(out=gst[:, 32:40], in0=gst[:, 8:16], scalar=1.0 / 128, in1=gst[:, 24:32], op0=ALU.mult, op1=ALU.subtract),
                [("g_sq", h) for h in range(8)] + ["g_msq"], ["g_var"])
            ACT(lambda e: e.activation(out=gst[:, 40:48], in_=gst[:, 32:40], func=AF.Sqrt, bias=EPS, scale=1.0), ["g_var"], ["g_std"])
            DVE(lambda e: e.reciprocal(out=gst[:, 48:56], in_=gst[:, 40:48]), ["g_std"], ["g_rstd"])
            DVE(lambda e: e.scalar_tensor_tensor(out=gst[:, 56:64], in0=gst[:, 16:24], scalar=-1.0, in1=gst[:, 48:56], op0=ALU.mult, op1=ALU.mult),
                ["g_mean", "g_rstd"], ["g_nmr"])
            for h in range(8):
                DVE(lambda e, h=h: e.tensor_scalar(out=osq[:, h * 128:(h + 1) * 128], in0=osq[:, h * 128:(h + 1) * 128],
                                                   scalar1=gst[:, 48 + h:49 + h], scalar2=gst[:, 56 + h:57 + h], op0=ALU.mult, op1=ALU.add),
                    [("osq", h // 4), "g_rstd", "g_nmr"] + [("g_sq", hh_) for hh_ in range(8)] + ["g_sum"], [("on", h)])
            yield
            POOL(lambda e, bi=bi: e.tensor_tensor(out=zb, in0=osq, in1=sgt2[bi], op=ALU.mult), [("on", hh_) for hh_ in range(8)] + [("sgt", bi)], ["zb"])
            transpose8(zb, "zb", zT4[:, :, t * 128:(t + 1) * 128], ("zT4", t))
            state_update(bi)

        def glevelB(g):
            xnr = [("xnT4b", g % 2, t) for t in range(4)]
            zr = [("zT4", t) for t in range(4)]
            for oc in range(8):
                yb = next_proj(projB)
                for k in range(8):
                    mm(bank(yb), Wro[:, k, oc * 128:(oc + 1) * 128], zT4[:, k, :], k == 0, k == 7, zr + wres("Wro", 8), [PR(yb)])
                b = next_proj(projB)
                for k in range(8):
                    mm(bank(b), WinB[:, k, 3072 + oc * 128:3072 + (oc + 1) * 128], xnT4b2[g % 2][:, k, :], k == 0, k == 7, xnr + wres("WinBr", 8), [PR(b)])
                sgb = sgr2[oc % 2]
                ACT(lambda e, b=b, sgb=sgb: e.activation(out=sgb, in_=bank(b), func=AF.Sigmoid), [PR(b)], [("sgr", oc % 2)])
                DVE(lambda e, yb=yb, sgb=sgb: e.tensor_tensor(out=tmpm, in0=bank(yb), in1=sgb, op=ALU.mult), [PR(yb), ("sgr", oc % 2)], ["tmpm"])
                POOL(lambda e, oc=oc, g=g: e.tensor_tensor(out=mergedT[:, oc, g * 512:(g + 1) * 512], in0=tmpm, in1=mergedT[:, oc, g * 512:(g + 1) * 512], op=ALU.add),
                     ["tmpm", ("mT", oc, g)], [("mT", oc, g)])


        def interleave(*gens):
            alive = [g_ for g_ in gens if g_ is not None]
            while alive:
                for g_ in list(alive):
                    try:
                        next(g_)
                    except StopIteration:
                        alive.remove(g_)

        def step(gen_):
            if gen_ is None:
                return
            try:
                next(gen_)
            except StopIteration:
                pass

        def drain(gen_):
            if gen_ is None:
                return
            for _ in gen_:
                pass

        orderB = [(g, t) for g in range(4) for t in range(4)]
        RB(*orderB[0])
        RBt(*orderB[0])
        RB(*orderB[1])
        RBt(*orderB[1])
        drain(PB(*orderB[0]))
        for i_, (g, t) in enumerate(orderB):
            if i_ + 2 < len(orderB):
                RB(*orderB[i_ + 2])
            tg = tailB(g, t)
            pg = PB(*orderB[i_ + 1]) if i_ + 1 < len(orderB) else None
            step(tg)
            step(pg)
            step(tg)
            step(tg)
            step(tg)
            step(pg)
            drain(pg)
            drain(tg)
            if i_ + 2 < len(orderB):
                RBt(*orderB[i_ + 2])
            if t == 3:
                glevelB(g)

        S.barrier()
        A.release(persist_mark)

        if DBG:
            dump_m()
            A.release(persist_mark)
        if stop == "B":
            raise _Stop()

        h = A.alloc([128, NT, D], F32)
        e_mark = A.mark()
        KT = A.alloc([128, 8, 256], BF16)
        Vm = A.alloc([128, 2, D], BF16)
        x_mark = A.mark()
        Wkv = A.alloc([128, 8, 2048], BF16)
        gbc_K = A.alloc([128, D], F32)
        memt = A.alloc([128, 2, D], F32)
        xs_K = A.alloc([128, D], BF16)
        junk_K = A.alloc([128, D], BF16)
        mnT = A.alloc([128, 8, 256], BF16)
        load_w(Wkv, w_xa_kv, 8, "Wkv", "Wkv")
        DMA("sp", gbc_K, g_mem.partition_broadcast(128), [], ["gbc"], "gbc")
        DMA("sp", memt, memc.rearrange("(c p) d -> p c d", p=128), [], ["memt"], "memt")
        for mc in range(2):
            rmsnorm(memt[:, mc, :], "memt", gbc_K, xs_K, junk_K)
            transpose8(xs_K, "xs", mnT[:, :, mc * 128:(mc + 1) * 128], ("mnT", mc))
        mnr = [("mnT", 0), ("mnT", 1)]
        for c in range(8):
            b = 1 + (c % 2)
            for k in range(8):
                mm(bank(b)[:, 0:256], Wkv[:, k, c * 128:(c + 1) * 128], mnT[:, k, :], k == 0, k == 7, mnr + wres("Wkv", 8), [PR(b)])
            ACT(lambda e, b=b, c=c: e.copy(out=KT[:, c, :], in_=bank(b)[:, 0:256]), [PR(b)], ["KT"])
        for mc in range(2):
            for hf in range(2):
                b = 3 + ((mc * 2 + hf) % 2)
                for k in range(8):
                    mm(bank(b), mnT[:, k, mc * 128:(mc + 1) * 128], Wkv[:, k, 1024 + hf * 512:1024 + (hf + 1) * 512], k == 0, k == 7, mnr + wres("Wkv", 8), [PR(b)])
                DVE(lambda e, b=b, mc=mc, hf=hf: e.tensor_copy(out=Vm[:, mc, hf * 512:(hf + 1) * 512], in_=bank(b)), [PR(b)], ["Vm"])
        S.barrier()
        A.release(x_mark)

        Wmix = A.alloc([128, 8, D], BF16)
        Wq = A.alloc([128, 8, D], BF16)
        Wo = A.alloc([128, 8, D], BF16)
        Wr = A.alloc([128, 8, 36], BF16)
        gbc_xa = A.alloc([128, D], F32)
        gbc_moe = A.alloc([128, D], F32)
        brt = A.alloc([128, 36], F32)
        ebase2 = A.alloc([128, 2 * NE], F32)
        ebase = ebase2[:, 0:NE]
        ebaseY = ebase2[:, NE:2 * NE]
        carry = A.alloc([128, NE], F32)
        xt2_X = [A.alloc([128, D], F32) for _ in range(2)]
        xs_X = A.alloc([128, D], BF16)
        junk_X = A.alloc([128, D], BF16)
        xn2T = A.alloc([128, 8, 256], BF16)
        qT2 = A.alloc([128, 8, 256], BF16)
        Pb = A.alloc([128, 4, 256], BF16)
        PTx = A.alloc([128, 8, 128], BF16)
        obx = A.alloc([128, 4, 256], BF16)
        oTx = A.alloc([128, 8, 128], BF16)
        xn3 = [A.alloc([128, D], BF16) for _ in range(2)]
        xn3T = A.alloc([128, 8, 128], BF16)
        rs = A.alloc([128, 256], F32)
        Mb = A.alloc([128, NE], BF16)

        load_w(Wmix, w_mix_out, 8, "Wmix", "Wmix")
        load_w(Wq, w_xa_q, 8, "Wq", "Wq")
        load_w(Wo, w_xa_o, 8, "Wo", "Wo")
        load_w(Wr, w_rt, 8, "Wr", "Wr")
        DMA("sp", gbc_xa, g_xa.partition_broadcast(128), [], ["gbc_xa"], "gbcx1")
        DMA("sp", gbc_moe, g_moe.partition_broadcast(128), [], ["gbc_moe"], "gbcx2")
        DMA("sp", brt, b_rt.partition_broadcast(128), [], ["brt"], "gbcx3")
        DMA("sp", ebase2, c_eb, [], ["ebase"], "gbcx4")
        POOL(lambda e: e.memset(carry, 0.0), [], ["carry"])


        def rmsnorm2(xt_ap, xt_res, g_bc, g_res, xs_ap, xs_res, rt=False):
            o_ = 4 if rt else 0
            sfx = "_r" if rt else ""
            ssq_, std_, rstd_ = stat[:, o_:o_ + 1], stat[:, o_ + 1:o_ + 2], stat[:, o_ + 2:o_ + 3]
            jk = junk_X
            ACT(lambda e: e.activation(out=jk, in_=xt_ap, func=AF.Square, accum_out=ssq_), [xt_res], ["junk", "ssq" + sfx])
            ACT(lambda e: e.activation(out=std_, in_=ssq_, func=AF.Sqrt, bias=EPS, scale=1.0 / D), ["ssq" + sfx], ["std" + sfx])
            DVE(lambda e: e.reciprocal(out=rstd_, in_=std_), ["std" + sfx], ["rstd" + sfx])
            DVE(lambda e: e.scalar_tensor_tensor(out=xs_ap, in0=xt_ap, scalar=rstd_, in1=g_bc, op0=ALU.mult, op1=ALU.mult),
                [xt_res, "rstd" + sfx, g_res], [xs_res])

        LG = rs[:, 0:36]
        GMAX = rs[:, 36:37]
        NGM = rs[:, 37:38]
        GE = rs[:, 40:44]
        GSUM = rs[:, 44:45]
        PG = rs[:, 45:46]
        GM = rs[:, 48:52]
        PEN = rs[:, 52:56]
        ELM = rs[:, 64:96]
        M1V = rs[:, 96:97]
        M2V = rs[:, 97:98]
        DD = rs[:, 98:99]
        S2 = rs[:, 99:100]
        W1 = rs[:, 100:101]
        W2 = rs[:, 101:102]
        I1 = rs[:, 102:103]
        I2 = rs[:, 103:104]
        V1 = rs[:, 104:105]
        V2 = rs[:, 105:106]
        J1 = rs[:, 106:107]
        J2 = rs[:, 107:108]
        M1 = rs[:, 128:160]
        M2 = rs[:, 160:192]
        ELM2 = rs[:, 192:224]
        POSB = rs[:, 224:256]
        TMPR = A.alloc([128, NE], F32)
        POSR = A.alloc([128, NE], F32)
        POSY = A.alloc([128, NE], F32)

        def router(tile, bi):
            hres = ("h", tile)
            rmsnorm2(h[:, tile, :], hres, gbc_moe, "gbc_moe", xn3[bi], ("xn3", bi), rt=True)
            yield
            transpose8(xn3[bi], ("xn3", bi), xn3T, "xn3T")
            yield
            for k in range(8):
                mm(bank(7)[:, 0:36], xn3T[:, k, :], Wr[:, k, :], k == 0, k == 7, ["xn3T"] + wres("Wr", 8), [PR(7)])
            R = "rt"
            DVE(lambda e: e.tensor_tensor(out=LG, in0=bank(7)[:, 0:36], in1=brt, op=ALU.add), [PR(7), "brt"], [R])
            DVE(lambda e: e.reduce_max(out=GMAX, in_=LG[:, 0:4], axis=AX.X), [R], [R])
            DVE(lambda e: e.tensor_scalar(out=NGM, in0=GMAX, scalar1=-1.0, scalar2=None, op0=ALU.mult), [R], [R])
            ACT(lambda e: e.activation(out=GE, in_=LG[:, 0:4], func=AF.Exp, bias=NGM, scale=1.0, accum_out=GSUM), [R], [R])
            DVE(lambda e: e.reciprocal(out=PG, in_=GSUM), [R], [R])
            DVE(lambda e: e.tensor_scalar(out=GM, in0=LG[:, 0:4], scalar1=GMAX, scalar2=None, op0=ALU.is_equal), [R], [R])
            DVE(lambda e: e.tensor_scalar(out=PEN, in0=GM, scalar1=-1.0, scalar2=1e30, op0=ALU.add, op1=ALU.mult), [R], [R])
            DVE(lambda e: e.tensor_tensor(out=ELM.rearrange("p (g j) -> p g j", g=4), in0=LG[:, 4:36].rearrange("p (g j) -> p g j", g=4),
                                          in1=PEN.unsqueeze(2).broadcast_to([128, 4, 8]), op=ALU.add), [R], [R])
            yield
            DVE(lambda e: e.reduce_max(out=M1V, in_=ELM, axis=AX.X), [R], [R])
            DVE(lambda e: e.tensor_scalar(out=M1, in0=ELM, scalar1=M1V, scalar2=None, op0=ALU.is_equal), [R], [R])
            DVE(lambda e: e.scalar_tensor_tensor(out=ELM2, in0=M1, scalar=-1e30, in1=ELM, op0=ALU.mult, op1=ALU.add), [R], [R])
            DVE(lambda e: e.reduce_max(out=M2V, in_=ELM2, axis=AX.X), [R], [R])
            DVE(lambda e: e.tensor_scalar(out=M2, in0=ELM2, scalar1=M2V, scalar2=None, op0=ALU.is_equal), [R], [R])
            yield
            DVE(lambda e: e.tensor_tensor(out=DD, in0=M2V, in1=M1V, op=ALU.subtract), [R], [R])
            ACT(lambda e: e.activation(out=S2, in_=DD, func=AF.Sigmoid), [R], [R])
            DVE(lambda e: e.tensor_tensor(out=W2, in0=PG, in1=S2, op=ALU.mult), [R], [R])
            DVE(lambda e: e.tensor_tensor(out=W1, in0=PG, in1=W2, op=ALU.subtract), [R], [R])
            DVE(lambda e: e.tensor_tensor(out=Mb, in0=M1, in1=M2, op=ALU.add), [R], ["Mb"])
            mm(bank(7)[:, 64:96], ustrict, Mb, True, True, ["Mb", "ident"], [PR(7)])
            mm(bank(7)[:, 96:128], onesb, Mb, True, True, ["Mb", "ident"], [PR(7)])
            DVE(lambda e: e.tensor_tensor(out=POSR, in0=bank(7)[:, 64:96], in1=carry, op=ALU.add), [PR(7), "carry"], [R])
            DVE(lambda e: e.tensor_tensor(out=carry, in0=bank(7)[:, 96:128], in1=carry, op=ALU.add), [PR(7), "carry"], ["carry"])
            yield
            DVE(lambda e: e.scalar_tensor_tensor(out=POSB, in0=POSR, scalar=float(CAP), in1=ebase, op0=ALU.min, op1=ALU.add), [R, "ebase"], [R])
            DVE(lambda e: e.scalar_tensor_tensor(out=POSY, in0=POSR, scalar=float(CAP - 1), in1=ebaseY, op0=ALU.min, op1=ALU.add), [R, "ebase"], [R])
            for (Mx, Ix, Jx, Vx, Wx, col) in ((M1, I1, J1, V1, W1, 0), (M2, I2, J2, V2, W2, 1)):
                yield
                DVE(lambda e, Mx=Mx: e.tensor_tensor(out=TMPR, in0=Mx, in1=POSB, op=ALU.mult), [R], [R])
                DVE(lambda e, Ix=Ix: e.reduce_sum(out=Ix, in_=TMPR, axis=AX.X), [R], [R])
                DVE(lambda e, Mx=Mx: e.tensor_tensor(out=TMPR, in0=Mx, in1=POSR, op=ALU.mult), [R], [R])
                DVE(lambda e, Vx=Vx: e.reduce_sum(out=Vx, in_=TMPR, axis=AX.X), [R], [R])
                DVE(lambda e, Ix=Ix, col=col: e.tensor_copy(out=sidx[:, tile, col:col + 1], in_=Ix), [R], [("sidx", tile)])
                DVE(lambda e, Mx=Mx: e.tensor_tensor(out=TMPR, in0=Mx, in1=POSY, op=ALU.mult), [R], [R])
                DVE(lambda e, Jx=Jx: e.reduce_sum(out=Jx, in_=TMPR, axis=AX.X), [R], [R])
                DVE(lambda e, Jx=Jx, col=col: e.tensor_copy(out=gidx[:, tile, col:col + 1], in_=Jx), [R], [("gidx", tile)])
                DVE(lambda e, Vx=Vx, Wx=Wx, col=col: e.scalar_tensor_tensor(out=wts[:, tile, col:col + 1], in0=Vx, scalar=float(CAP), in1=Wx, op0=ALU.is_lt, op1=ALU.mult),
                    [R], [("wts", tile)])
            for col in range(2):
                S.add("pool", lambda e, col=col, bi=bi: e.indirect_dma_start(
                    out=XG, out_offset=bass.IndirectOffsetOnAxis(ap=sidx[:, tile, col:col + 1], axis=0), in_=xn3[bi], in_offset=None,
                    bounds_check=NE * CAPR - 1, oob_is_err=False),
                    [("xn3", bi), ("sidx", tile)], [("XG", tile, col)], dma=True, key="xgs%d" % bi, bsize=2)

        projX = [1, 2]
        xn2T2 = [xn2T, A.alloc([128, 8, 256], BF16)]
        qT22 = [qT2, A.alloc([128, 8, 256], BF16)]

        def interleaveX(*gens):
            alive = [g_ for g_ in gens if g_ is not None]
            while alive:
                for g_ in list(alive):
                    try:
                        next(g_)
                    except StopIteration:
                        alive.remove(g_)

        def S1_X(g):
            gp = g % 2
            for tl in range(2):
                tile = g * 2 + tl
                bi = tile % 2
                xb = xt2_X[bi]
                xr = ("xt", bi)
                DMA("sp", xb, xc[tile * 128:(tile + 1) * 128, :], [], [xr], "xt%d" % bi)
                for hf in range(2):
                    b = projX[hf]
                    for k in range(8):
                        mm(bank(b), mergedT[:, k, tile * 128:(tile + 1) * 128], Wmix[:, k, hf * 512:(hf + 1) * 512], k == 0, k == 7,
                           [("mT", k, tile // 4)] + wres("Wmix", 8), [PR(b)])
                    DVE(lambda e, b=b, hf=hf, tile=tile, xb=xb: e.tensor_tensor(out=h[:, tile, hf * 512:(hf + 1) * 512], in0=bank(b), in1=xb[:, hf * 512:(hf + 1) * 512], op=ALU.add),
                        [PR(b), xr], [("h", tile)])
                yield
                rmsnorm2(h[:, tile, :], ("h", tile), gbc_xa, "gbc_xa", xs_X, "xs")
                transpose8(xs_X, "xs", xn2T2[gp][:, :, tl * 128:(tl + 1) * 128], ("xn2T", gp, tl))
                yield

        def QT_X(g):
            gp = g % 2
            for c in range(8):
                b = projX[c % 2]
                for k in range(8):
                    mm(bank(b)[:, 0:256], Wq[:, k, c * 128:(c + 1) * 128], xn2T2[gp][:, k, :], k == 0, k == 7,
                       [("xn2T", gp, 0), ("xn2T", gp, 1)] + wres("Wq", 8), [PR(b)])
                ACT(lambda e, b=b, c=c, gp=gp: e.copy(out=qT22[gp][:, c, :], in_=bank(b)[:, 0:256]), [PR(b)], [("qT2", gp, c)])
                if c % 2 == 1:
                    yield

        def XA_X(tile):
            g, tl = tile // 2, tile % 2
            gp = g % 2
            sc = bank(3, 2).rearrange("p (h m) -> p h m", h=4)
            for hh in range(4):
                for kc in range(2):
                    mm(ps_t[:, 3 * 512 + hh * 256: 3 * 512 + (hh + 1) * 256], qT22[gp][:, 2 * hh + kc, tl * 128:(tl + 1) * 128], KT[:, 2 * hh + kc, :], kc == 0, kc == 1,
                       [("qT2", gp, 2 * hh + kc), "KT"], [PR(3 + hh // 2)])
            DVE(lambda e, sc=sc: e.reduce_max(out=stat[:, 8:12], in_=sc, axis=AX.X), [PR(3), PR(4)], ["xmx"])
            DVE(lambda e: e.tensor_scalar(out=stat[:, 12:16], in0=stat[:, 8:12], scalar1=-1.0 / 16, scalar2=None, op0=ALU.mult), ["xmx"], ["xnb"])
            for hh in range(4):
                ACT(lambda e, hh=hh: e.activation(out=Pb[:, hh, :], in_=ps_t[:, 3 * 512 + hh * 256: 3 * 512 + (hh + 1) * 256], func=AF.Exp,
                                                  bias=stat[:, 12 + hh:13 + hh], scale=1.0 / 16, accum_out=stat[:, 16 + hh:17 + hh]),
                    [PR(3 + hh // 2), "xnb"], [("Pb", hh), ("xsum", hh)])
            DVE(lambda e: e.reciprocal(out=stat[:, 20:24], in_=stat[:, 16:20]), [("xsum", hh) for hh in range(4)], ["xrs"])
            yield
            pb = bank_bf(0)
            Pv = Pb.rearrange("p h m -> p (h m)")
            for j in range(8):
                tp(pb[:, j, :], Pv[:, j * 128:(j + 1) * 128], [("Pb", j // 2)], [PR(0)])
            ACT(lambda e: e.copy(out=PTx, in_=bank_bf(0)), [PR(0)], ["PTx"])
            yield
            for hh in range(4):
                for mc in range(2):
                    mm(ps_t[:, 5 * 512 + hh * 256: 5 * 512 + (hh + 1) * 256], PTx[:, 2 * hh + mc, :], Vm[:, mc, hh * 256:(hh + 1) * 256], mc == 0, mc == 1,
                       ["PTx", "Vm"], [PR(5 + hh // 2)])
            DVE(lambda e: e.tensor_tensor(out=obx, in0=bank(5, 2).rearrange("p (h m) -> p h m", h=4),
                                          in1=stat[:, 20:24].unsqueeze(2).broadcast_to([128, 4, 256]), op=ALU.mult),
                [PR(5), PR(6), "xrs"], ["obx"])
            yield
            transpose8(obx.rearrange("p h m -> p (h m)"), "obx", oTx, "oTx")
            yield
            for hf in range(2):
                b = projX[hf]
                for k in range(8):
                    mm(bank(b), oTx[:, k, :], Wo[:, k, hf * 512:(hf + 1) * 512], k == 0, k == 7, ["oTx"] + wres("Wo", 8), [PR(b)])
                DVE(lambda e, b=b, hf=hf, tile=tile: e.tensor_tensor(out=h[:, tile, hf * 512:(hf + 1) * 512], in0=bank(b), in1=h[:, tile, hf * 512:(hf + 1) * 512], op=ALU.add),
                    [PR(b), ("h", tile)], [("h", tile)])
            yield

        interleaveX(S1_X(0))
        interleaveX(QT_X(0))
        pending = None
        for g in range(8):
            interleaveX(XA_X(2 * g), pending)
            interleaveX(XA_X(2 * g + 1), router(2 * g, 0), S1_X(g + 1) if g + 1 < 8 else None)
            if g + 1 < 8:
                interleaveX(QT_X(g + 1))
            pending = router(2 * g + 1, 1)
        interleaveX(pending)

        S.barrier()
        A.release(e_mark)
        A.limit = ARENA_BYTES

        if DBG:
            for tile in range(NT):
                DMA("sp", dbg_h[:, tile * D:(tile + 1) * D], h[:, tile, :], [("h", tile)], [("dbgh", tile)], "dbgh")
            S.barrier()
        if stop in ("X", "XNR", "X1"):
            raise _Stop()

        NR = 3
        Wg_r = [A.alloc([128, 8, 512], BF16) for _ in range(NR)]
        Wu_r = [A.alloc([128, 8, 512], BF16) for _ in range(NR)]
        Wd_r = [A.alloc([128, 4, D], BF16) for _ in range(NR)]
        xg4 = [A.alloc([128, 2, D], BF16) for _ in range(4)]
        xgT2 = [A.alloc([128, 8, 256], BF16) for _ in range(2)]
        hid2 = [A.alloc([128, 4, 256], BF16) for _ in range(2)]
        sge2 = [A.alloc([128, 256], F32) for _ in range(2)]
        ysb2 = [A.alloc([128, 2, D], BF16) for _ in range(2)]

        allxg = [("XG", tile, col) for tile in range(NT) for col in range(2)]
        gb_i = [0]
        def TG_E(ex):
            s = ex % NR
            bi = ex % 2
            DMA("pool", Wg_r[s], w_gate[ex].rearrange("(k p) n -> p k n", p=128), [], [("Wg", s, k) for k in range(8)], "wg%d" % s)
            DMA("pool", Wu_r[s], w_up[ex].rearrange("(k p) n -> p k n", p=128), [], [("Wu", s, k) for k in range(8)], "wu%d" % s)
            DMA("pool", Wd_r[s], w_down[ex].rearrange("(k p) n -> p k n", p=128), [], [("Wd", s, k) for k in range(4)], "wd%d" % s)
            for rb in range(2):
                pb = bank_bf(0)
                for k in range(8):
                    tp(pb[:, k, :], xg4[ex % 4][:, rb, k * 128:(k + 1) * 128], [("xg", ex % 4)], [PR(0)])
                if rb == 0:
                    ACT(lambda e, bi=bi, rb=rb: e.copy(out=xgT2[bi][:, :, rb * 128:(rb + 1) * 128], in_=bank_bf(0)), [PR(0)], [("xgT", bi, rb)])
                else:
                    DVE(lambda e, bi=bi, rb=rb: e.tensor_copy(out=xgT2[bi][:, :, rb * 128:(rb + 1) * 128], in_=bank_bf(0)), [PR(0)], [("xgT", bi, rb)])
            xgr = [("xgT", bi, 0), ("xgT", bi, 1)]
            for hc in range(4):
                gbk = 1 + (gb_i[0] % 2)
                ubk = 3 + (gb_i[0] % 2)
                gb_i[0] += 1
                for k in range(8):
                    mm(bank(gbk)[:, 0:256], Wg_r[s][:, k, hc * 128:(hc + 1) * 128], xgT2[bi][:, k, :], k == 0, k == 7, xgr + [("Wg", s, kk) for kk in range(8)], [PR(gbk)])
                for k in range(8):
                    mm(bank(ubk)[:, 0:256], Wu_r[s][:, k, hc * 128:(hc + 1) * 128], xgT2[bi][:, k, :], k == 0, k == 7, xgr + [("Wu", s, kk) for kk in range(8)], [PR(ubk)])
                sgb = sge2[hc % 2]
                ACT(lambda e, gbk=gbk, sgb=sgb: e.activation(out=sgb, in_=bank(gbk)[:, 0:256], func=AF.Sigmoid), [PR(gbk)], [("sge", hc % 2)])
                DVE(lambda e, gbk=gbk, sgb=sgb: e.tensor_tensor(out=sgb, in0=bank(gbk)[:, 0:256], in1=sgb, op=ALU.mult),
                    [PR(gbk), ("sge", hc % 2)], [("sge", hc % 2)])
                DVE(lambda e, ubk=ubk, sgb=sgb, bi=bi, hc=hc: e.tensor_tensor(out=hid2[bi][:, hc, :], in0=bank(ubk)[:, 0:256], in1=sgb, op=ALU.mult),
                    [PR(ubk), ("sge", hc % 2)], [("hid", bi, hc)])

        def DN_E(ex):
            s = ex % NR
            bi = ex % 2
            hr = [("hid", bi, hc) for hc in range(4)]
            for rb in range(2):
                for hf in range(2):
                    yb = 5 + ((rb * 2 + hf) % 3)
                    for hc in range(4):
                        mm(bank(yb), hid2[bi][:, hc, rb * 128:(rb + 1) * 128], Wd_r[s][:, hc, hf * 512:(hf + 1) * 512], hc == 0, hc == 3,
                           hr + [("Wd", s, kk) for kk in range(4)], [PR(yb)])
                    if hf == 0:
                        ACT(lambda e, yb=yb, bi=bi, rb=rb, hf=hf: e.copy(out=ysb2[bi][:, rb, hf * 512:(hf + 1) * 512], in_=bank(yb)), [PR(yb)], [("ysb", bi, rb, hf)])
                    else:
                        DVE(lambda e, yb=yb, bi=bi, rb=rb, hf=hf: e.tensor_copy(out=ysb2[bi][:, rb, hf * 512:(hf + 1) * 512], in_=bank(yb)), [PR(yb)], [("ysb", bi, rb, hf)])
            DMA("sp", YG[ex * CAP:(ex + 1) * CAP, :].rearrange("(b p) d -> p b d", p=128), ysb2[bi],
                [("ysb", bi, rb, hf) for rb in range(2) for hf in range(2)], [("YG", ex)], "yg%d" % bi)


        def XL_E(ex):
            DMA("sp", xg4[ex % 4], XG[ex * CAPR:ex * CAPR + CAP, :].rearrange("(b p) d -> p b d", p=128), allxg if ex < 4 else [], [("xg", ex % 4)], "xg%d" % (ex % 4))

        for ex in range(4):
            XL_E(ex)
        TG_E(0)
        for ex in range(NE):
            if ex + 4 < NE:
                XL_E(ex + 4)
            if ex + 1 < NE:
                TG_E(ex + 1)
            DN_E(ex)

        S.barrier()
        A.release(e_mark)

        gbc_C = A.alloc([128, D], F32)
        y12 = [[A.alloc([128, D], BF16) for _ in range(2)] for _ in range(2)]
        ot2 = [A.alloc([128, D], F32) for _ in range(2)]
        junk_C = A.alloc([128, D], BF16)
        DMA("sp", gbc_C, g_fin.partition_broadcast(128), [], ["gbc"], "gbc")
        outres = []

        def c_s1(tile):
            bi = tile % 2
            o_ = 24 + 3 * bi
            for col in range(2):
                S.add("pool", lambda e, col=col, bi=bi, tile=tile: e.indirect_dma_start(
                    out=y12[bi][col], out_offset=None, in_=YG, in_offset=bass.IndirectOffsetOnAxis(ap=gidx[:, tile, col:col + 1], axis=0)),
                    [("gidx", tile)], [("y12", bi, col)], dma=True, key="yga%d%d" % (bi, col))
            for col in range(2):
                DVE(lambda e, col=col, bi=bi, tile=tile: e.scalar_tensor_tensor(out=h[:, tile, :], in0=y12[bi][col], scalar=wts[:, tile, col:col + 1], in1=h[:, tile, :],
                                                                             op0=ALU.mult, op1=ALU.add),
                    [("y12", bi, col), ("wts", tile), ("h", tile)], [("h", tile)])
            ACT(lambda e, tile=tile, o_=o_: e.activation(out=junk_C, in_=h[:, tile, :], func=AF.Square, accum_out=stat[:, o_:o_ + 1]), [("h", tile)], ["junk", ("ssqC", bi)])
            ACT(lambda e, o_=o_: e.activation(out=stat[:, o_ + 1:o_ + 2], in_=stat[:, o_:o_ + 1], func=AF.Sqrt, bias=EPS, scale=1.0 / D), [("ssqC", bi)], [("stdC", bi)])

        def c_s2(tile):
            bi = tile % 2
            o_ = 24 + 3 * bi
            DVE(lambda e, o_=o_: e.reciprocal(out=stat[:, o_ + 2:o_ + 3], in_=stat[:, o_ + 1:o_ + 2]), [("stdC", bi)], [("rstdC", bi)])
            DVE(lambda e, tile=tile, bi=bi, o_=o_: e.scalar_tensor_tensor(out=ot2[bi], in0=h[:, tile, :], scalar=stat[:, o_ + 2:o_ + 3], in1=gbc_C, op0=ALU.mult, op1=ALU.mult),
                [("h", tile), ("rstdC", bi), "gbc"], [("ot", bi)])
            DMA("sp", out[tile * 128:(tile + 1) * 128, :], ot2[bi], [("ot", bi)], [("out", tile)], "out%d" % bi)
            outres.append(("out", tile))

        c_s1(0)
        for tile in range(NT):
            if tile + 1 < NT:
                c_s1(tile + 1)
            c_s2(tile)
        S.add("sp", None, outres)
        S.barrier()


    try:
        phases()
    except _Stop:
        S.barrier()

    S.resolve()
    sems = {}
    for e in ("pe", "act", "dve", "pool"):
        sems[("eng", e)] = es.enter_context(nc.semaphore("s_" + e))
    for k in S.keys:
        sems[("dma", k)] = es.enter_context(nc.semaphore("d_" + str(k)))
    with nc.Block() as block:
        block.sync(lambda e: S.run_engine("sp", e, sems))
        block.scalar(lambda e: S.run_engine("act", e, sems))
        block.vector(lambda e: S.run_engine("dve", e, sems))
        block.gpsimd(lambda e: S.run_engine("pool", e, sems))
        block.tensor(lambda e: S.run_engine("pe", e, sems))
    es.close()
    return nc, S, A


def _consts(half):
    p = np.arange(128, dtype=np.float64)
    inv_freq = 10000.0 ** (-np.arange(0, 64, 2, dtype=np.float64) / 64)

    def cs_tab(base):
        pos = base + np.arange(NT)[None, :] * 128 + p[:, None]
        ang = (pos[:, :, None].astype(np.float32) * inv_freq[None, None, :].astype(np.float32)).astype(np.float32)
        return np.stack([np.cos(ang), np.sin(ang)], axis=1).astype(np.float32)

    gam = 1.0 - 2.0 ** (-5.0 - np.arange(8, dtype=np.float64))
    lg = np.log(gam)
    gq = np.zeros((128, 4, 128), np.float32)
    gk = np.zeros((128, 4, 128), np.float32)
    ct = np.zeros((128, 4, 128), np.float32)
    i = np.arange(128, dtype=np.float64)
    for c in range(4):
        for hl in range(2):
            h = 2 * c + hl
            gq[hl * 64:(hl + 1) * 64, c, :] = np.exp((i + 1) * lg[h])[None, :]
            gk[hl * 64:(hl + 1) * 64, c, :] = (np.exp(-(i + 1) * lg[h]) / 8.0)[None, :]
            ct[hl * 64:(hl + 1) * 64, c, :] = np.exp(128 * lg[h])
    zt = (np.exp((127 - p)[:, None] * lg[None, :]) / 8.0).astype(np.float32)
    mask = (np.arange(128)[None, :] >= np.arange(128)[:, None]).astype(np.float32)
    ident = np.eye(128, dtype=np.float32)
    ustrict = (np.arange(128)[:, None] < np.arange(128)[None, :]).astype(np.float32)
    ones = np.ones((128, 128), np.float32)
    c_bf = np.concatenate([ident, ustrict, ones], axis=1)
    eb = np.concatenate([np.tile((np.arange(NE, dtype=np.float32) * CAPR)[None, :], (128, 1)),
                         np.tile((np.arange(NE, dtype=np.float32) * CAP)[None, :], (128, 1))], axis=1)
    return {
        "c_bf": c_bf, "c_cs_own": cs_tab(half * TOK), "c_cs_pre": cs_tab(0.0),
        "c_gq": gq, "c_gk": gk, "c_zt": zt, "c_ct": ct, "c_mask": np.ascontiguousarray(np.tile(mask[:, None, :], (1, 4, 1))), "c_eb": eb,
    }


_CACHE = {}


def kernel(x, mem, mix_norm_g, w_in, conv_w, w_conv_out, w_ret_out, w_mix_out,
           xa_norm_g, mem_norm_g, w_xa_q, w_xa_kv, w_xa_o, moe_norm_g,
           w_group, b_group, w_router, b_router, w_gate, w_up, w_down, final_norm_g):
    f = lambda a: np.ascontiguousarray(np.asarray(a, dtype=np.float32))
    x = f(x)
    mem = f(mem)
    if "nc" not in _CACHE:
        _CACHE["nc"] = build_program()
    nc = _CACHE["nc"][0]
    shared = {
        "w_in": f(w_in)[0], "conv_wT": np.ascontiguousarray(f(conv_w)[0].T), "w_conv_out": f(w_conv_out)[0],
        "w_ret_out": f(w_ret_out)[0], "w_mix_out": f(w_mix_out)[0], "w_xa_q": f(w_xa_q)[0], "w_xa_kv": f(w_xa_kv)[0],
        "w_xa_o": f(w_xa_o)[0], "g_mix": f(mix_norm_g)[0], "g_xa": f(xa_norm_g)[0], "g_mem": f(mem_norm_g)[0],
        "g_moe": f(moe_norm_g)[0], "g_fin": f(final_norm_g),
        "w_rt": np.ascontiguousarray(np.concatenate([f(w_group)[0], f(w_router)[0]], axis=1)),
        "b_rt": np.ascontiguousarray(np.concatenate([f(b_group)[0], f(b_router)[0]], axis=0)),
        "w_gate": f(w_gate)[0], "w_up": f(w_up)[0], "w_down": f(w_down)[0],
    }
    zeros = np.zeros((TOK, D), np.float32)
    in_maps = []
    for c in range(8):
        b, half = c // 2, c % 2
        m = dict(shared)
        m["xc"] = np.ascontiguousarray(x[b, half * TOK:(half + 1) * TOK])
        m["xp"] = np.ascontiguousarray(x[b, 0:TOK]) if half == 1 else zeros
        m["memc"] = np.ascontiguousarray(mem[b])
        m.update(_consts(half))
        in_maps.append(m)
    res = run_bass_kernel_spmd(nc, in_maps, core_ids=list(range(8)))
    _CACHE["res"] = res
    outp = np.empty((4, 2 * TOK, D), np.float32)
    for c in range(8):
        b, half = c // 2, c % 2
        outp[b, half * TOK:(half + 1) * TOK] = res.results[c]["out"]
    return outp
```

```python
import math
import numpy as np
from contextlib import ExitStack
import concourse.bass as bass
import concourse.mybir as mybir
from concourse.bass_utils import run_bass_kernel_spmd

F32 = mybir.dt.float32
BF16 = mybir.dt.bfloat16
I32 = mybir.dt.int32
U8 = mybir.dt.uint8
AF = mybir.ActivationFunctionType
ALU = mybir.AluOpType
AX = mybir.AxisListType

D = 1024
NT = 16
TOK = 2048
NE = 32
CAP = 256
CAPR = CAP + 1
EPS = 1e-6
INW = 6656
DBG = False


class Op:
    __slots__ = ("eng", "fn", "reads", "writes", "dma", "key", "idx", "sig", "ev", "deps", "xdeps", "bsize")

    def __init__(self, eng, fn, reads, writes, dma, key):
        self.eng = eng
        self.fn = fn
        self.reads = tuple(reads)
        self.writes = tuple(writes)
        self.dma = dma
        self.key = key
        self.sig = False
        self.ev = None
        self.deps = ()
        self.xdeps = ()
        self.bsize = 1


class Sched:
    ENGS = ("pe", "act", "dve", "pool", "sp")

    def __init__(self):
        self.ops = []
        self.last_eng = {}
        self.last_key = {}

    def add(self, eng, fn, reads=(), writes=(), dma=False, key=None, bsize=1):
        if dma:
            assert key is not None
        op = Op(eng, fn, reads, writes, dma, key)
        op.bsize = bsize
        op.idx = len(self.ops)
        self.ops.append(op)
        if dma:
            self.last_key[key] = op.idx
        elif fn is not None:
            self.last_eng[eng] = op.idx
        return op

    def barrier(self):
        deps = tuple(self.last_eng.values()) + tuple(self.last_key.values())
        for e in self.ENGS:
            op = self.add(e, None)
            op.xdeps = deps

    def resolve(self):
        last_w = {}
        readers = {}
        for op in self.ops:
            deps = set(op.xdeps)
            for r in op.reads:
                w = last_w.get(r)
                if w is not None:
                    deps.add(w)
            for w_ in op.writes:
                w = last_w.get(w_)
                if w is not None:
                    deps.add(w)
                for rd in readers.get(w_, {}).values():
                    deps.add(rd)
            deps.discard(op.idx)
            dl = []
            for d in sorted(deps):
                dop = self.ops[d]
                if dop.fn is None:
                    continue
                if op.eng == "pe" and dop.eng == "pe" and not dop.dma and not op.dma:
                    continue
                dop.sig = True
                dl.append(d)
            op.deps = tuple(dl)
            rk = ("dma", op.idx) if op.dma else op.eng
            for r in op.reads:
                readers.setdefault(r, {})[rk] = op.idx
            for w_ in op.writes:
                last_w[w_] = op.idx
                readers[w_] = {}
        cnt = {e: 0 for e in self.ENGS}
        keycnt = {}
        import os
        if os.environ.get("ALLSIG"):
            for op in self.ops:
                if not op.dma and op.fn is not None and op.eng != "sp":
                    op.sig = True
        for op in self.ops:
            if op.dma:
                keycnt[op.key] = keycnt.get(op.key, 0) + 16
                q = 16 * op.bsize
                op.ev = (("dma", op.key), (keycnt[op.key] + q - 1) // q * q)
            elif op.sig:
                cnt[op.eng] += 1
                op.ev = (("eng", op.eng), cnt[op.eng])
        self.keys = list(keycnt.keys())
        self.cnt = cnt
        return self

    def run_engine(self, eng, eobj, sems):
        waited = {}
        for op in self.ops:
            if op.eng != eng:
                continue
            need = {}
            for d in op.deps:
                sk, val = self.ops[d].ev
                if need.get(sk, 0) < val:
                    need[sk] = val
            for sk, val in need.items():
                if waited.get(sk, 0) >= val:
                    continue
                eobj.wait_ge(sems[sk], val)
                waited[sk] = val
            if op.fn is None:
                continue
            ins = op.fn(eobj)
            if op.dma:
                ins.then_inc(sems[op.ev[0]], 16)
            elif op.sig:
                ins.then_inc(sems[op.ev[0]], 1)


_DTSZ = {F32: 4, BF16: 2, I32: 4, U8: 1}


class Arena:
    def __init__(self, ap, size):
        self.ap = ap
        self.size = size
        self.off = 0
        self.peak = 0
        self.limit = size

    def mark(self):
        return self.off

    def release(self, m):
        self.off = m

    def alloc(self, shape, dt):
        n = 1
        for s in shape[1:]:
            n *= s
        nbytes = n * _DTSZ[dt]
        off = (self.off + 31) // 32 * 32
        assert off + nbytes <= self.limit, ("SBUF arena overflow", off, nbytes, self.limit)
        self.off = off + nbytes
        self.peak = max(self.peak, self.off)
        v = self.ap[:, off:off + nbytes].bitcast(dt)
        if len(shape) == 3:
            v = v.rearrange("p (a b) -> p a b", a=shape[1])
        elif len(shape) == 4:
            v = v.rearrange("p (a b c) -> p a b c", a=shape[1], b=shape[2])
        return v


def build_program(stop=None):
    nc = bass.Bass("TRN2", target_bir_lowering=False)

    def din(name, shape, dt=F32):
        return nc.dram_tensor(name, list(shape), dt, kind="ExternalInput").ap()

    xc = din("xc", [TOK, D])
    xp = din("xp", [TOK, D])
    memc = din("memc", [256, D])
    w_in = din("w_in", [D, INW])
    conv_wT = din("conv_wT", [512, 3])
    w_conv_out = din("w_conv_out", [512, D])
    w_ret_out = din("w_ret_out", [D, D])
    w_mix_out = din("w_mix_out", [D, D])
    w_xa_q = din("w_xa_q", [D, D])
    w_xa_kv = din("w_xa_kv", [D, 2 * D])
    w_xa_o = din("w_xa_o", [D, D])
    g_mix = din("g_mix", [D])
    g_xa = din("g_xa", [D])
    g_mem = din("g_mem", [D])
    g_moe = din("g_moe", [D])
    g_fin = din("g_fin", [D])
    w_rt = din("w_rt", [D, 36])
    b_rt = din("b_rt", [36])
    w_gate = din("w_gate", [NE, D, 512])
    w_up = din("w_up", [NE, D, 512])
    w_down = din("w_down", [NE, 512, D])
    c_bf = din("c_bf", [128, 384])
    c_cs_own = din("c_cs_own", [128, 2, NT, 32])
    c_cs_pre = din("c_cs_pre", [128, 2, NT, 32])
    c_gq = din("c_gq", [128, 4, 128])
    c_gk = din("c_gk", [128, 4, 128])
    c_zt = din("c_zt", [128, 8])
    c_ct = din("c_ct", [128, 4, 128])
    c_mask = din("c_mask", [128, 4, 128])
    c_eb = din("c_eb", [128, 2 * NE])
    out = nc.dram_tensor("out", [TOK, D], F32, kind="ExternalOutput").ap()
    XG = nc.dram_tensor("xg_scr", [NE * CAPR, D], BF16, kind="Internal").ap()
    YG = nc.dram_tensor("yg_scr", [NE * CAP, D], BF16, kind="Internal").ap()
    if DBG:
        dbg_m = nc.dram_tensor("dbg_m", [128, 8 * TOK], F32, kind="ExternalOutput").ap()
        dbg_h = nc.dram_tensor("dbg_h", [128, NT * D], F32, kind="ExternalOutput").ap()

    S = Sched()
    es = ExitStack()
    ARENA_BYTES = 207 * 1024
    arena_t = es.enter_context(nc.sbuf_tensor("arena", [128, ARENA_BYTES], U8))
    A = Arena(arena_t, ARENA_BYTES)
    ps_t = es.enter_context(nc.psum_tensor("ps", [128, 4096], F32))

    def bank(i, n=1):
        return ps_t[:, i * 512:(i + n) * 512]

    def bank_bf(i):
        return ps_t[:, i * 512:(i + 1) * 512].bitcast(BF16).rearrange("p (a b) -> p a b", a=8)

    def PR(i):
        return ("ps", i)

    uid = [0]

    def ukey(p):
        uid[0] += 1
        return "%s%d" % (p, uid[0])

    def PE(fn, r, w):
        return S.add("pe", fn, r, w)

    def ACT(fn, r, w):
        return S.add("act", fn, r, w)

    def DVE(fn, r, w):
        return S.add("dve", fn, r, w)

    def POOL(fn, r, w):
        return S.add("pool", fn, r, w)

    def DMA(eng, out_, in_, r, w, key, nb=1):
        return S.add(eng, lambda e: e.dma_start(out=out_, in_=in_), r, w, dma=True, key=key, bsize=nb)

    def mm(out_, lhsT, rhs, start, stop, r, w):
        return PE(lambda e: e.matmul(out_, lhsT=lhsT, rhs=rhs, start=start, stop=stop), r, w)

    def tp(out_, in_, r, w):
        return PE(lambda e: e.transpose(out=out_, in_=in_, identity=ident), r + ["ident"], w)

    def load_w(dst, src, rows_k, res, key):
        for k in range(rows_k):
            DMA("pool", dst[:, k, :], src[k * 128:(k + 1) * 128, :], [], [(res, k)], key, nb=rows_k)

    def wres(res, n):
        return [(res, k) for k in range(n)]

    ident3 = A.alloc([128, 3, 128], BF16)
    ident = ident3[:, 0, :]
    ustrict = ident3[:, 1, :]
    onesb = ident3[:, 2, :]
    DMA("pool", ident3, c_bf.rearrange("p (a b) -> p a b", a=3), [], ["ident"], "c_bf")
    MT_BYTES = 8 * TOK * 2
    mergedT = arena_t[:, ARENA_BYTES - MT_BYTES:ARENA_BYTES].bitcast(BF16).rearrange("p (a b) -> p a b", a=8)
    A.limit = ARENA_BYTES - MT_BYTES
    stat = A.alloc([128, 64], F32)
    wts = A.alloc([128, NT, 2], F32)
    sidx = A.alloc([128, NT, 2], I32)
    gidx = A.alloc([128, NT, 2], I32)
    ssq = stat[:, 0:1]
    std = stat[:, 1:2]
    rstd = stat[:, 2:3]
    persist_mark = A.mark()

    def rmsnorm(xt_ap, xt_res, g_bc, xs_ap, junk_ap, xs_res="xs"):
        ACT(lambda e: e.activation(out=junk_ap, in_=xt_ap, func=AF.Square, accum_out=ssq), [xt_res], ["junk", "ssq"])
        ACT(lambda e: e.activation(out=std, in_=ssq, func=AF.Sqrt, bias=EPS, scale=1.0 / D), ["ssq"], ["std"])
        DVE(lambda e: e.reciprocal(out=rstd, in_=std), ["std"], ["rstd"])
        DVE(lambda e: e.scalar_tensor_tensor(out=xs_ap, in0=xt_ap, scalar=rstd, in1=g_bc, op0=ALU.mult, op1=ALU.mult),
            [xt_res, "rstd", "gbc"], [xs_res])

    def transpose8(src_ap, src_res, dst_ap, dst_res, nblk=8, copy_eng="act"):
        pb = bank_bf(0)
        for k in range(nblk):
            tp(pb[:, k, :], src_ap[:, k * 128:(k + 1) * 128], [src_res], [PR(0)])
        if copy_eng == "act":
            ACT(lambda e: e.copy(out=dst_ap, in_=pb[:, 0:nblk, :]), [PR(0)], [dst_res])
        else:
            DVE(lambda e: e.tensor_copy(out=dst_ap, in_=pb[:, 0:nblk, :]), [PR(0)], [dst_res])

    def dump_m():
        dtmp = A.alloc([128, 8, 512], F32)
        for g in range(4):
            DVE(lambda e, g=g: e.tensor_copy(out=dtmp, in_=mergedT[:, :, g * 512:(g + 1) * 512]), [("mT", oc, g) for oc in range(8)] + ["dbgo"], ["dtmp"])
            DMA("sp", dbg_m.rearrange("p (k t) -> p k t", k=8)[:, :, g * 512:(g + 1) * 512], dtmp, ["dtmp"], ["dbgo"], "dbgo")
        S.barrier()

    class _Stop(Exception):
        pass

    def phases():
        WinA = A.alloc([128, 8, 2560], BF16)
        Wco = A.alloc([128, 4, 1024], BF16)
        gbc_A = A.alloc([128, D], F32)
        convw = A.alloc([128, 4, 3], F32)
        xt2_A = [A.alloc([128, D], F32) for _ in range(2)]
        xs_A = A.alloc([128, D], BF16)
        junk_A = A.alloc([128, D], BF16)
        xnT4 = [A.alloc([128, 8, 512], BF16) for _ in range(2)]
        xin_sb = A.alloc([128, 4, 512], F32)
        u = A.alloc([128, 4, 514], F32)
        cc = A.alloc([128, 4, 512], F32)
        bc = A.alloc([128, 4, 512], BF16)
        sg2 = [A.alloc([128, 512], F32) for _ in range(2)]

        zt_ = A.alloc([128, 4112], BF16)
        POOL(lambda e: e.memset(zt_, 0.0), [], ["zfill"])
        XGf = XG.rearrange("r d -> (r d)").rearrange("(p n) -> p n", p=128)
        DMA("sp", gbc_A, g_mix.partition_broadcast(128), [], ["gbc"], "gbc")
        DMA("sp", convw, conv_wT.rearrange("(c p) k -> p c k", p=128), [], ["convw"], "convw")
        for k in range(8):
            DMA("pool", WinA[:, k, 0:1536], w_in[k * 128:(k + 1) * 128, 0:1536], [], [("WinA", k)], "WinA", nb=8)
        for k in range(4):
            DMA("pool", Wco[:, k, :], w_conv_out[k * 128:(k + 1) * 128, :], [], [("Wco", k)], "Wco", nb=4)
        for k in range(8):
            DMA("pool", WinA[:, k, 1536:2560], w_in[k * 128:(k + 1) * 128, 4608:5632], [], [("WinAg", k)], "WinAg", nb=8)
        DMA("sp", xt2_A[1], xp[TOK - 128:TOK, :], [], [("xt", 1)], "xt1")
        rmsnorm(xt2_A[1], ("xt", 1), gbc_A, xs_A, junk_A, xs_res=("xsA", 0))
        transpose8(xs_A, ("xsA", 0), xnT4[1][:, :, 0:128], ("xnT4", 1, 0))
        for c in range(4):
            for k in range(8):
                mm(bank(1)[:, 0:128], WinA[:, k, c * 128:(c + 1) * 128], xnT4[1][:, k, 0:128], k == 0, k == 7, [("xnT4", 1, 0)] + wres("WinA", 8), [PR(1)])
            ACT(lambda e, c=c: e.copy(out=xin_sb[:, c, 0:128], in_=bank(1)[:, 0:128]), [PR(1)], [("xin", c)])
            for k in range(8):
                mm(bank(2)[:, 0:128], WinA[:, k, 1024 + c * 128:1024 + (c + 1) * 128], xnT4[1][:, k, 0:128], k == 0, k == 7, [("xnT4", 1, 0)] + wres("WinA", 8), [PR(2)])
            DVE(lambda e, c=c: e.tensor_tensor(out=u[:, c, 0:2], in0=bank(2)[:, 126:128], in1=xin_sb[:, c, 126:128], op=ALU.mult),
                [PR(2), ("xin", c)], ["u"])

        projA = [1, 2, 3, 6, 7]
        pa_i = [0]

        def next_proj(pool):
            b = pool[pa_i[0] % len(pool)]
            pa_i[0] += 1
            return b

        xs4 = [xs_A] + [A.alloc([128, D], BF16) for _ in range(3)]
        sg8 = list(sg2) + [A.alloc([128, 512], F32) for _ in range(6)]

        def hnA(g, t):
            tile = g * 4 + t
            xb = xt2_A[tile % 2]
            xr = ("xt", tile % 2)
            DMA("sp", xb, xc[tile * 128:(tile + 1) * 128, :], [], [xr], "xt%d" % (tile % 2))
            ACT(lambda e: e.activation(out=junk_A, in_=xb, func=AF.Square, accum_out=ssq), [xr], ["junk", "ssq"])
            ACT(lambda e: e.activation(out=std, in_=ssq, func=AF.Sqrt, bias=EPS, scale=1.0 / D), ["ssq"], ["std"])
            DVE(lambda e: e.reciprocal(out=rstd, in_=std), ["std"], ["rstd"])
            DVE(lambda e: e.scalar_tensor_tensor(out=xs4[t], in0=xb, scalar=rstd, in1=gbc_A, op0=ALU.mult, op1=ALU.mult),
                [xr, "rstd", "gbc"], [("xsA", t)])

        def htA(g, t):
            transpose8(xs4[t], ("xsA", t), xnT4[g % 2][:, :, t * 128:(t + 1) * 128], ("xnT4", g % 2, t))

        def xinA(g):
            gb = g % 2
            xnr = [("xnT4", gb, t) for t in range(4)]
            for c in range(4):
                b = next_proj(projA)
                for k in range(8):
                    mm(bank(b), WinA[:, k, c * 128:(c + 1) * 128], xnT4[gb][:, k, :], k == 0, k == 7, xnr + wres("WinA", 8), [PR(b)])
                ACT(lambda e, b=b, c=c: e.copy(out=xin_sb[:, c, :], in_=bank(b)), [PR(b)], [("xin", c)])

        def cgA(g):
            gb = g % 2
            xnr = [("xnT4", gb, t) for t in range(4)]
            for c in range(4):
                b = next_proj(projA)
                for k in range(8):
                    mm(bank(b), WinA[:, k, 1024 + c * 128:1024 + (c + 1) * 128], xnT4[gb][:, k, :], k == 0, k == 7, xnr + wres("WinA", 8), [PR(b)])
                DVE(lambda e, b=b, c=c: e.tensor_tensor(out=u[:, c, 2:514], in0=bank(b), in1=xin_sb[:, c, :], op=ALU.mult),
                    [PR(b), ("xin", c)], ["u"])
                POOL(lambda e, c=c: e.tensor_scalar(out=cc[:, c, :], in0=u[:, c, 0:512], scalar1=convw[:, c, 0:1], scalar2=None, op0=ALU.mult),
                     ["u", "convw"], [("cc", c)])
                DVE(lambda e, c=c: e.scalar_tensor_tensor(out=cc[:, c, :], in0=u[:, c, 1:513], scalar=convw[:, c, 1:2], in1=cc[:, c, :], op0=ALU.mult, op1=ALU.add),
                    ["u", "convw", ("cc", c)], [("cc", c)])
                DVE(lambda e, c=c: e.scalar_tensor_tensor(out=cc[:, c, :], in0=u[:, c, 2:514], scalar=convw[:, c, 2:3], in1=cc[:, c, :], op0=ALU.mult, op1=ALU.add),
                    ["u", "convw", ("cc", c)], [("cc", c)])
            POOL(lambda e: e.tensor_copy(out=u[:, :, 0:2], in_=u[:, :, 512:514]), ["u"], ["u"])

        def bgA(g):
            gb = g % 2
            xnr = [("xnT4", gb, t) for t in range(4)]
            for c in range(4):
                b = next_proj(projA)
                for k in range(8):
                    mm(bank(b), WinA[:, k, 512 + c * 128:512 + (c + 1) * 128], xnT4[gb][:, k, :], k == 0, k == 7, xnr + wres("WinA", 8), [PR(b)])
                DVE(lambda e, b=b, c=c: e.tensor_tensor(out=bc[:, c, :], in0=bank(b), in1=cc[:, c, :], op=ALU.mult),
                    [PR(b), ("cc", c)], [("bc", c)])

        def gateA(g, ocs):
            gb = g % 2
            xnr = [("xnT4", gb, t) for t in range(4)]
            for oc in ocs:
                b = next_proj(projA)
                for k in range(8):
                    mm(bank(b), WinA[:, k, 1536 + oc * 128:1536 + (oc + 1) * 128], xnT4[gb][:, k, :], k == 0, k == 7, xnr + wres("WinAg", 8), [PR(b)])
                ACT(lambda e, b=b, oc=oc: e.activation(out=sg8[oc], in_=bank(b), func=AF.Sigmoid), [PR(b)], [("sg", oc)])

        def yconvA(g, ocs):
            for oc in ocs:
                yb = 4 + (oc % 2)
                for k in range(4):
                    mm(bank(yb), Wco[:, k, oc * 128:(oc + 1) * 128], bc[:, k, :], k == 0, k == 3, [("bc", kk) for kk in range(4)] + wres("Wco", 4), [PR(yb)])
                DVE(lambda e, yb=yb, oc=oc, g=g: e.tensor_tensor(out=mergedT[:, oc, g * 512:(g + 1) * 512], in0=bank(yb), in1=sg8[oc], op=ALU.mult),
                    [PR(yb), ("sg", oc)], [("mT", oc, g)])

        for t in range(4):
            hnA(0, t)
            htA(0, t)
        for g in range(4):
            nx = g + 1 if g + 1 < 4 else None
            if nx is not None:
                hnA(nx, 0)
            xinA(g)
            if nx is not None:
                htA(nx, 0)
                hnA(nx, 1)
            cgA(g)
            if nx is not None:
                htA(nx, 1)
                hnA(nx, 2)
            gateA(g, range(0, 4))
            bgA(g)
            if nx is not None:
                htA(nx, 2)
                hnA(nx, 3)
            gateA(g, range(4, 8))
            if nx is not None:
                htA(nx, 3)
            yconvA(g, range(8))
            if g == 0:
                for i in range(16):
                    DMA("sp", XGf[:, i * 4112:(i + 1) * 4112], zt_, ["zfill"], [("XGz", i)], "xgz", nb=16)

        S.barrier()
        A.release(persist_mark)
        if stop == "A":
            dump_m()
            raise _Stop()

        WinB = A.alloc([128, 8, 4096], BF16)
        Wro = A.alloc([128, 8, 1024], BF16)
        gbc_B = A.alloc([128, D], F32)
        cs = A.alloc([128, 2, NT, 32], F32)
        gq = A.alloc([128, 4, 128], F32)
        gk = A.alloc([128, 4, 128], F32)
        zt = A.alloc([128, 8], F32)
        ct = A.alloc([128, 4, 128], F32)
        maskT = A.alloc([128, 4, 128], F32)
        Sst = A.alloc([128, 4, 128], F32)
        Sb = A.alloc([128, 4, 128], BF16)
        xt2_B = [A.alloc([128, D], F32) for _ in range(2)]
        xs_B = A.alloc([128, D], BF16)
        junk_B = A.alloc([128, D], BF16)
        xnT4b2 = [A.alloc([128, 8, 512], BF16) for _ in range(2)]
        xs_B2 = [xs_B, A.alloc([128, D], BF16)]
        qr2 = [A.alloc([128, 8, 2, 32], BF16) for _ in range(2)]
        kr2 = [A.alloc([128, 8, 2, 32], BF16) for _ in range(2)]
        kz2 = [A.alloc([128, 8, 64], BF16) for _ in range(2)]
        v2 = [A.alloc([128, D], BF16) for _ in range(2)]
        sgt2 = [A.alloc([128, D], BF16) for _ in range(2)]
        rt = [A.alloc([128, 8, 32], F32) for _ in range(4)]
        qTz = A.alloc([128, 4, 2, 128], BF16)
        kT = A.alloc([128, 4, 128], BF16)
        PT = A.alloc([128, 8, 128], BF16)
        osq = A.alloc([128, D], F32)
        zb = A.alloc([128, D], BF16)
        zT4 = A.alloc([128, 8, 512], BF16)
        sgr2 = [A.alloc([128, 512], F32) for _ in range(2)]
        tmpm = A.alloc([128, 512], F32)
        gst = A.alloc([128, 64], F32)

        DMA("sp", gbc_B, g_mix.partition_broadcast(128), [], ["gbc"], "gbc")
        DMA("sp", cs, c_cs_pre, [], ["cs"], "cs")
        DMA("sp", gq, c_gq, [], ["gq"], "c_gq")
        DMA("sp", gk, c_gk, [], ["gk"], "c_gk")
        DMA("sp", zt, c_zt, [], ["zt"], "c_zt")
        DMA("sp", ct, c_ct, [], ["ct"], "c_ct")
        DMA("sp", maskT, c_mask, [], ["maskT"], "c_mask")
        for k in range(8):
            DMA("pool", WinB[:, k, 512:2048], w_in[k * 128:(k + 1) * 128, 2048:3584], [], [("WinBkv", k)], "WinBkv", nb=8)
        for k in range(8):
            DMA("pool", WinB[:, k, 0:512], w_in[k * 128:(k + 1) * 128, 1536:2048], [], [("WinBq", k)], "WinBq", nb=8)
        for k in range(8):
            DMA("pool", WinB[:, k, 2048:3072], w_in[k * 128:(k + 1) * 128, 3584:4608], [], [("WinBg", k)], "WinBg", nb=8)
        for k in range(8):
            DMA("pool", WinB[:, k, 3072:4096], w_in[k * 128:(k + 1) * 128, 5632:6656], [], [("WinBr", k)], "WinBr", nb=8)
        load_w(Wro, w_ret_out, 8, "Wro", "Wro")
        POOL(lambda e: e.memset(Sst, 0.0), [], ["Sst"])
        POOL(lambda e: e.memset(Sb, 0.0), [], ["Sb"])
        POOL(lambda e: e.memset(qTz, 0.0), [], ["qT"])

        projB = [1, 2, 7]
        pa_i[0] = 0

        def rotary(pb_ap, pres, tile, dst, dres, ti):
            pv = pb_ap.rearrange("p (h t f) -> p h t f", h=8, t=2)
            cosb = cs[:, 0, tile, :].unsqueeze(1).broadcast_to([128, 8, 32])
            sinb = cs[:, 1, tile, :].unsqueeze(1).broadcast_to([128, 8, 32])
            ta, tb, tc, td = rt
            DVE(lambda e: e.tensor_tensor(out=ta, in0=pv[:, :, 0, :], in1=cosb, op=ALU.mult), [pres, "cs"], ["rta"])
            DVE(lambda e: e.tensor_tensor(out=tb, in0=pv[:, :, 1, :], in1=sinb, op=ALU.mult), [pres, "cs"], ["rtb"])
            DVE(lambda e: e.tensor_tensor(out=tc, in0=pv[:, :, 0, :], in1=sinb, op=ALU.mult), [pres, "cs"], ["rtc"])
            DVE(lambda e: e.tensor_tensor(out=td, in0=pv[:, :, 1, :], in1=cosb, op=ALU.mult), [pres, "cs"], ["rtd"])
            POOL(lambda e: e.tensor_tensor(out=dst[:, :, 0, :], in0=ta, in1=tb, op=ALU.subtract), ["rta", "rtb"], [(dres, 0)])
            POOL(lambda e: e.tensor_tensor(out=dst[:, :, 1, :], in0=tc, in1=td, op=ALU.add), ["rtc", "rtd"], [(dres, 1)])

        def state_update(bi):
            kzv = kz2[bi].rearrange("p h d -> p (h d)")
            for c in range(4):
                ob = ps_t[:, 3 * 512 + c * 256: 3 * 512 + (c + 1) * 256]
                mm(ob, kzv[:, c * 128:(c + 1) * 128], v2[bi][:, c * 256:(c + 1) * 256], True, True,
                   [("kz", bi), ("v", bi)], [PR(3), PR(4)])
            pS = bank(3, 2).rearrange("p (c n) -> p c n", c=4)
            POOL(lambda e: e.tensor_tensor(out=Sst, in0=Sst, in1=ct, op=ALU.mult), ["Sst", "ct"], ["Sst"])
            DVE(lambda e: e.tensor_tensor(out=Sst[0:64], in0=Sst[0:64], in1=pS[0:64, :, 0:128], op=ALU.add), ["Sst", PR(3), PR(4)], ["Sst"])
            DVE(lambda e: e.tensor_tensor(out=Sst[64:128], in0=Sst[64:128], in1=pS[64:128, :, 128:256], op=ALU.add), ["Sst", PR(3), PR(4)], ["Sst"])
            ACT(lambda e: e.copy(out=Sb, in_=Sst), ["Sst"], ["Sb"])

        def proj_tm(xnT_ap, xn_res, col0, wres_, b):
            for k in range(8):
                mm(bank(b), xnT_ap[:, k, :], WinB[:, k, col0:col0 + 512], k == 0, k == 7, xn_res + wres_, [PR(b)])

        def RPp(tile):
            bi = tile % 2
            xb = xt2_B[bi]
            xr = ("xt", bi)
            DMA("sp", xb, xp[tile * 128:(tile + 1) * 128, :], [], [xr], "xt%d" % bi)
            rmsnorm(xb, xr, gbc_B, xs_B2[bi], junk_B, xs_res=("xsB", bi))

        def RPt(tile):
            bi = tile % 2
            transpose8(xs_B2[bi], ("xsB", bi), xnT4b2[1][:, :, (tile % 4) * 128:(tile % 4 + 1) * 128], ("xnT4b", 1, tile % 4))

        def PPp(tile):
            bi = tile % 2
            xnTp_ = xnT4b2[1][:, :, (tile % 4) * 128:(tile % 4 + 1) * 128]
            xres_ = [("xnT4b", 1, tile % 4)]
            b = next_proj(projB)
            proj_tm(xnTp_, xres_, 512, wres("WinBkv", 8), b)
            rotary(bank(b), PR(b), tile, kr2[bi], ("kr", bi), 1)
            POOL(lambda e, bi=bi: e.tensor_tensor(out=kz2[bi], in0=kr2[bi].rearrange("p h t f -> p h (t f)"),
                                                  in1=zt.unsqueeze(2).broadcast_to([128, 8, 64]), op=ALU.mult),
                 [(("kr", bi), 0), (("kr", bi), 1), "zt"], [("kz", bi)])
            for hf in range(2):
                b = next_proj(projB)
                proj_tm(xnTp_, xres_, 1024 + hf * 512, wres("WinBkv", 8), b)
                ACT(lambda e, b=b, bi=bi, hf=hf: e.copy(out=v2[bi][:, hf * 512:(hf + 1) * 512], in_=bank(b)), [PR(b)], [("v", bi)])
            state_update(bi)

        RPp(0)
        RPt(0)
        RPp(1)
        RPt(1)
        for tile in range(NT):
            if tile + 2 < NT:
                RPp(tile + 2)
            PPp(tile)
            if tile + 2 < NT:
                RPt(tile + 2)

        if stop == "B1":
            raise _Stop()

        def chk(n):
            if stop == "B2:%d" % n:
                raise _Stop()
        DMA("sp", cs, c_cs_own, [], ["cs"], "cs")

        def RB(g, t):
            tile = g * 4 + t
            bi = tile % 2
            xb = xt2_B[bi]
            xr = ("xt", bi)
            DMA("sp", xb, xc[tile * 128:(tile + 1) * 128, :], [], [xr], "xt%d" % bi)
            rmsnorm(xb, xr, gbc_B, xs_B2[bi], junk_B, xs_res=("xsB", bi))

        def RBt(g, t):
            tile = g * 4 + t
            bi = tile % 2
            xnT_t = xnT4b2[g % 2][:, :, t * 128:(t + 1) * 128]
            transpose8(xs_B2[bi], ("xsB", bi), xnT_t, ("xnT4b", g % 2, t))

        def PB(g, t):
            tile = g * 4 + t
            bi = tile % 2
            xnT_t = xnT4b2[g % 2][:, :, t * 128:(t + 1) * 128]
            xnres = [("xnT4b", g % 2, t)]
            b = next_proj(projB)
            proj_tm(xnT_t, xnres, 0, wres("WinBq", 8), b)
            rotary(bank(b), PR(b), tile, qr2[bi], ("qr", bi), 0)
            b = next_proj(projB)
            proj_tm(xnT_t, xnres, 512, wres("WinBkv", 8), b)
            rotary(bank(b), PR(b), tile, kr2[bi], ("kr", bi), 1)
            POOL(lambda e, bi=bi: e.tensor_tensor(out=kz2[bi], in0=kr2[bi].rearrange("p h t f -> p h (t f)"),
                                                  in1=zt.unsqueeze(2).broadcast_to([128, 8, 64]), op=ALU.mult),
                 [(("kr", bi), 0), (("kr", bi), 1), "zt"], [("kz", bi)])
            yield
            for hf in range(2):
                b = next_proj(projB)
                proj_tm(xnT_t, xnres, 1024 + hf * 512, wres("WinBkv", 8), b)
                ACT(lambda e, b=b, bi=bi, hf=hf: e.copy(out=v2[bi][:, hf * 512:(hf + 1) * 512], in_=bank(b)), [PR(b)], [("v", bi)])
            yield
            for hf in range(2):
                b = next_proj(projB)
                proj_tm(xnT_t, xnres, 2048 + hf * 512, wres("WinBg", 8), b)
                ACT(lambda e, b=b, hf=hf: e.activation(out=sgr2[hf], in_=bank(b), func=AF.Sigmoid), [PR(b)], [("sgr", hf)])
                DVE(lambda e, b=b, bi=bi, hf=hf: e.tensor_tensor(out=sgt2[bi][:, hf * 512:(hf + 1) * 512], in0=bank(b), in1=sgr2[hf], op=ALU.mult),
                    [PR(b), ("sgr", hf)], [("sgt", bi)])

        def tailB(g, t):
            tile = g * 4 + t
            bi = tile % 2
            pb = bank_bf(0)
            qrv = qr2[bi].rearrange("p h t f -> p (h t f)")
            krv = kr2[bi].rearrange("p h t f -> p (h t f)")
            for c in range(4):
                tp(pb[:, c, :], qrv[:, c * 128:(c + 1) * 128], [(("qr", bi), 0), (("qr", bi), 1)], [PR(0)])
            for c in range(4):
                tp(pb[:, 4 + c, :], krv[:, c * 128:(c + 1) * 128], [(("kr", bi), 0), (("kr", bi), 1)], [PR(0)])
            DVE(lambda e: e.tensor_tensor(out=qTz[0:64, :, 0, :], in0=bank_bf(0)[0:64, 0:4, :], in1=gq[0:64], op=ALU.mult), [PR(0), "gq"], ["qT"])
            DVE(lambda e: e.tensor_tensor(out=qTz[64:128, :, 1, :], in0=bank_bf(0)[64:128, 0:4, :], in1=gq[64:128], op=ALU.mult), [PR(0), "gq", "qT"], ["qT"])
            DVE(lambda e: e.tensor_tensor(out=kT, in0=bank_bf(0)[:, 4:8, :], in1=gk, op=ALU.mult), [PR(0), "gk"], ["kT"])
            yield
            for c in range(4):
                mm(ps_t[:, 3 * 512 + c * 256: 3 * 512 + (c + 1) * 256], kT[:, c, :], qTz[:, c, :, :].rearrange("p a q -> p (a q)"), True, True,
                   ["qT", "kT"], [PR(3 + c // 2)])
            mb = maskT
            DVE(lambda e, mb=mb: e.tensor_tensor(out=PT[:, 0:4, :], in0=bank(3).rearrange("p (h q) -> p h q", h=4), in1=mb, op=ALU.mult),
                [PR(3), "maskT"], [("PT", 0)])
            DVE(lambda e, mb=mb: e.tensor_tensor(out=PT[:, 4:8, :], in0=bank(4).rearrange("p (h q) -> p h q", h=4), in1=mb, op=ALU.mult),
                [PR(4), "maskT"], [("PT", 1)])
            yield
            for h in range(8):
                p0 = (h % 2) * 64
                ob = ps_t[:, 5 * 512 + h * 128: 5 * 512 + (h + 1) * 128]
                mm(ob, PT[:, h, :], v2[bi][:, h * 128:(h + 1) * 128], True, False, [("PT", h // 4), ("v", bi)], [PR(5 + h // 4)])
                mm(ob, qTz[:, h // 2, h % 2, :], Sb[:, h // 2, :], False, True, ["qT", "Sb"], [PR(5 + h // 4)])
            yield
            for hb in range(2):
                ACT(lambda e, hb=hb: e.copy(out=osq[:, hb * 512:(hb + 1) * 512], in_=bank(5 + hb)), [PR(5 + hb)],
                    [("osq", hb)] + [("on", hh_) for hh_ in range(hb * 4, hb * 4 + 4)])
            DVE(lambda e: e.reduce_sum(out=gst[:, 0:8], in_=osq.rearrange("p (h e) -> p h e", h=8), axis=AX.X), [("osq", 0), ("osq", 1)], ["g_sum"])
            for h in range(8):
                ACT(lambda e, h=h: e.activation(out=junk_B[:, h * 128:(h + 1) * 128], in_=osq[:, h * 128:(h + 1) * 128], func=AF.Square,
                                                accum_out=gst[:, 8 + h:9 + h]),
                    [("osq", h // 4)], ["junk", ("g_sq", h)])
            DVE(lambda e: e.tensor_scalar(out=gst[:, 16:24], in0=gst[:, 0:8], scalar1=1.0 / 128, scalar2=None, op0=ALU.mult), ["g_sum"], ["g_mean"])
            DVE(lambda e: e.tensor_tensor(out=gst[:, 24:32], in0=gst[:, 16:24], in1=gst[:, 16:24], op=ALU.mult), ["g_mean"], ["g_msq"])
            DVE(lambda e: e.scalar_tensor_tensor(out=gst[:, 32:40], in0=gst[:, 8:16], scalar=1.0 / 128, in1=gst[:, 24:32], op0=ALU.mult, op1=ALU.subtract),
                [("g_sq", h) for h in range(8)] + ["g_msq"], ["g_var"])
            ACT(lambda e: e.activation(out=gst[:, 40:48], in_=gst[:, 32:40], func=AF.Sqrt, bias=EPS, scale=1.0), ["g_var"], ["g_std"])
            DVE(lambda e: e.reciprocal(out=gst[:, 48:56], in_=gst[:, 40:48]), ["g_std"], ["g_rstd"])
            DVE(lambda e: e.scalar_tensor_tensor(out=gst[:, 56:64], in0=gst[:, 16:24], scalar=-1.0, in1=gst[:, 48:56], op0=ALU.mult, op1=ALU.mult),
                ["g_mean", "g_rstd"], ["g_nmr"])
            for h in range(8):
                DVE(lambda e, h=h: e.tensor_scalar(out=osq[:, h * 128:(h + 1) * 128], in0=osq[:, h * 128:(h + 1) * 128],
                                                   scalar1=gst[:, 48 + h:49 + h], scalar2=gst[:, 56 + h:57 + h], op0=ALU.mult, op1=ALU.add),
                    [("osq", h // 4), "g_rstd", "g_nmr"] + [("g_sq", hh_) for hh_ in range(8)] + ["g_sum"], [("on", h)])
            yield
            POOL(lambda e, bi=bi: e.tensor_tensor(out=zb, in0=osq, in1=sgt2[bi], op=ALU.mult), [("on", hh_) for hh_ in range(8)] + [("sgt", bi)], ["zb"])
            transpose8(zb, "zb", zT4[:, :, t * 128:(t + 1) * 128], ("zT4", t))
            state_update(bi)

        def glevelB(g):
            xnr = [("xnT4b", g % 2, t) for t in range(4)]
            zr = [("zT4", t) for t in range(4)]
            for oc in range(8):
                yb = next_proj(projB)
                for k in range(8):
                    mm(bank(yb), Wro[:, k, oc * 128:(oc + 1) * 128], zT4[:, k, :], k == 0, k == 7, zr + wres("Wro", 8), [PR(yb)])
                b = next_proj(projB)
                for k in range(8):
                    mm(bank(b), WinB[:, k, 3072 + oc * 128:3072 + (oc + 1) * 128], xnT4b2[g % 2][:, k, :], k == 0, k == 7, xnr + wres("WinBr", 8), [PR(b)])
                sgb = sgr2[oc % 2]
                ACT(lambda e, b=b, sgb=sgb: e.activation(out=sgb, in_=bank(b), func=AF.Sigmoid), [PR(b)], [("sgr", oc % 2)])
                DVE(lambda e, yb=yb, sgb=sgb: e.tensor_tensor(out=tmpm, in0=bank(yb), in1=sgb, op=ALU.mult), [PR(yb), ("sgr", oc % 2)], ["tmpm"])
                POOL(lambda e, oc=oc, g=g: e.tensor_tensor(out=mergedT[:, oc, g * 512:(g + 1) * 512], in0=tmpm, in1=mergedT[:, oc, g * 512:(g + 1) * 512], op=ALU.add),
                     ["tmpm", ("mT", oc, g)], [("mT", oc, g)])


        def interleave(*gens):
            alive = [g_ for g_ in gens if g_ is not None]
            while alive:
                for g_ in list(alive):
                    try:
                        next(g_)
                    except StopIteration:
                        alive.remove(g_)

        def step(gen_):
            if gen_ is None:
                return
            try:
                next(gen_)
            except StopIteration:
                pass

        def drain(gen_):
            if gen_ is None:
                return
            for _ in gen_:
                pass

        orderB = [(g, t) for g in range(4) for t in range(4)]
        RB(*orderB[0])
        RBt(*orderB[0])
        RB(*orderB[1])
        RBt(*orderB[1])
        drain(PB(*orderB[0]))
        for i_, (g, t) in enumerate(orderB):
            if i_ + 2 < len(orderB):
                RB(*orderB[i_ + 2])
            tg = tailB(g, t)
            pg = PB(*orderB[i_ + 1]) if i_ + 1 < len(orderB) else None
            step(tg)
            step(pg)
            step(tg)
            step(tg)
            step(tg)
            step(pg)
            drain(pg)
            drain(tg)
            if i_ + 2 < len(orderB):
                RBt(*orderB[i_ + 2])
            if t == 3:
                glevelB(g)

        S.barrier()
        A.release(persist_mark)

        if DBG:
            dump_m()
            A.release(persist_mark)
        if stop == "B":
            raise _Stop()

        h = A.alloc([128, NT, D], F32)
        e_mark = A.mark()
        KT = A.alloc([128, 8, 256], BF16)
        Vm = A.alloc([128, 2, D], BF16)
        x_mark = A.mark()
        Wkv = A.alloc([128, 8, 2048], BF16)
        gbc_K = A.alloc([128, D], F32)
        memt = A.alloc([128, 2, D], F32)
        xs_K = A.alloc([128, D], BF16)
        junk_K = A.alloc([128, D], BF16)
        mnT = A.alloc([128, 8, 256], BF16)
        load_w(Wkv, w_xa_kv, 8, "Wkv", "Wkv")
        DMA("sp", gbc_K, g_mem.partition_broadcast(128), [], ["gbc"], "gbc")
        DMA("sp", memt, memc.rearrange("(c p) d -> p c d", p=128), [], ["memt"], "memt")
        for mc in range(2):
            rmsnorm(memt[:, mc, :], "memt", gbc_K, xs_K, junk_K)
            transpose8(xs_K, "xs", mnT[:, :, mc * 128:(mc + 1) * 128], ("mnT", mc))
        mnr = [("mnT", 0), ("mnT", 1)]
        for c in range(8):
            b = 1 + (c % 2)
            for k in range(8):
                mm(bank(b)[:, 0:256], Wkv[:, k, c * 128:(c + 1) * 128], mnT[:, k, :], k == 0, k == 7, mnr + wres("Wkv", 8), [PR(b)])
            ACT(lambda e, b=b, c=c: e.copy(out=KT[:, c, :], in_=bank(b)[:, 0:256]), [PR(b)], ["KT"])
        for mc in range(2):
            for hf in range(2):
                b = 3 + ((mc * 2 + hf) % 2)
                for k in range(8):
                    mm(bank(b), mnT[:, k, mc * 128:(mc + 1) * 128], Wkv[:, k, 1024 + hf * 512:1024 + (hf + 1) * 512], k == 0, k == 7, mnr + wres("Wkv", 8), [PR(b)])
                DVE(lambda e, b=b, mc=mc, hf=hf: e.tensor_copy(out=Vm[:, mc, hf * 512:(hf + 1) * 512], in_=bank(b)), [PR(b)], ["Vm"])
        S.barrier()
        A.release(x_mark)

        Wmix = A.alloc([128, 8, D], BF16)
        Wq = A.alloc([128, 8, D], BF16)
        Wo = A.alloc([128, 8, D], BF16)
        Wr = A.alloc([128, 8, 36], BF16)
        gbc_xa = A.alloc([128, D], F32)
        gbc_moe = A.alloc([128, D], F32)
        brt = A.alloc([128, 36], F32)
        ebase2 = A.alloc([128, 2 * NE], F32)
        ebase = ebase2[:, 0:NE]
        ebaseY = ebase2[:, NE:2 * NE]
        carry = A.alloc([128, NE], F32)
        xt2_X = [A.alloc([128, D], F32) for _ in range(2)]
        xs_X = A.alloc([128, D], BF16)
        junk_X = A.alloc([128, D], BF16)
        xn2T = A.alloc([128, 8, 256], BF16)
        qT2 = A.alloc([128, 8, 256], BF16)
        Pb = A.alloc([128, 4, 256], BF16)
        PTx = A.alloc([128, 8, 128], BF16)
        obx = A.alloc([128, 4, 256], BF16)
        oTx = A.alloc([128, 8, 128], BF16)
        xn3 = [A.alloc([128, D], BF16) for _ in range(2)]
        xn3T = A.alloc([128, 8, 128], BF16)
        rs = A.alloc([128, 256], F32)
        Mb = A.alloc([128, NE], BF16)

        load_w(Wmix, w_mix_out, 8, "Wmix", "Wmix")
        load_w(Wq, w_xa_q, 8, "Wq", "Wq")
        load_w(Wo, w_xa_o, 8, "Wo", "Wo")
        load_w(Wr, w_rt, 8, "Wr", "Wr")
        DMA("sp", gbc_xa, g_xa.partition_broadcast(128), [], ["gbc_xa"], "gbcx1")
        DMA("sp", gbc_moe, g_moe.partition_broadcast(128), [], ["gbc_moe"], "gbcx2")
        DMA("sp", brt, b_rt.partition_broadcast(128), [], ["brt"], "gbcx3")
        DMA("sp", ebase2, c_eb, [], ["ebase"], "gbcx4")
        POOL(lambda e: e.memset(carry, 0.0), [], ["carry"])


        def rmsnorm2(xt_ap, xt_res, g_bc, g_res, xs_ap, xs_res, rt=False):
            o_ = 4 if rt else 0
            sfx = "_r" if rt else ""
            ssq_, std_, rstd_ = stat[:, o_:o_ + 1], stat[:, o_ + 1:o_ + 2], stat[:, o_ + 2:o_ + 3]
            jk = junk_X
            ACT(lambda e: e.activation(out=jk, in_=xt_ap, func=AF.Square, accum_out=ssq_), [xt_res], ["junk", "ssq" + sfx])
            ACT(lambda e: e.activation(out=std_, in_=ssq_, func=AF.Sqrt, bias=EPS, scale=1.0 / D), ["ssq" + sfx], ["std" + sfx])
            DVE(lambda e: e.reciprocal(out=rstd_, in_=std_), ["std" + sfx], ["rstd" + sfx])
            DVE(lambda e: e.scalar_tensor_tensor(out=xs_ap, in0=xt_ap, scalar=rstd_, in1=g_bc, op0=ALU.mult, op1=ALU.mult),
                [xt_res, "rstd" + sfx, g_res], [xs_res])

        LG = rs[:, 0:36]
        GMAX = rs[:, 36:37]
        NGM = rs[:, 37:38]
        GE = rs[:, 40:44]
        GSUM = rs[:, 44:45]
        PG = rs[:, 45:46]
        GM = rs[:, 48:52]
        PEN = rs[:, 52:56]
        ELM = rs[:, 64:96]
        M1V = rs[:, 96:97]
        M2V = rs[:, 97:98]
        DD = rs[:, 98:99]
        S2 = rs[:, 99:100]
        W1 = rs[:, 100:101]
        W2 = rs[:, 101:102]
        I1 = rs[:, 102:103]
        I2 = rs[:, 103:104]
        V1 = rs[:, 104:105]
        V2 = rs[:, 105:106]
        J1 = rs[:, 106:107]
        J2 = rs[:, 107:108]
        M1 = rs[:, 128:160]
        M2 = rs[:, 160:192]
        ELM2 = rs[:, 192:224]
        POSB = rs[:, 224:256]
        TMPR = A.alloc([128, NE], F32)
        POSR = A.alloc([128, NE], F32)
        POSY = A.alloc([128, NE], F32)

        def router(tile, bi):
            hres = ("h", tile)
            rmsnorm2(h[:, tile, :], hres, gbc_moe, "gbc_moe", xn3[bi], ("xn3", bi), rt=True)
            yield
            transpose8(xn3[bi], ("xn3", bi), xn3T, "xn3T")
            yield
            for k in range(8):
                mm(bank(7)[:, 0:36], xn3T[:, k, :], Wr[:, k, :], k == 0, k == 7, ["xn3T"] + wres("Wr", 8), [PR(7)])
            R = "rt"
            DVE(lambda e: e.tensor_tensor(out=LG, in0=bank(7)[:, 0:36], in1=brt, op=ALU.add), [PR(7), "brt"], [R])
            DVE(lambda e: e.reduce_max(out=GMAX, in_=LG[:, 0:4], axis=AX.X), [R], [R])
            DVE(lambda e: e.tensor_scalar(out=NGM, in0=GMAX, scalar1=-1.0, scalar2=None, op0=ALU.mult), [R], [R])
            ACT(lambda e: e.activation(out=GE, in_=LG[:, 0:4], func=AF.Exp, bias=NGM, scale=1.0, accum_out=GSUM), [R], [R])
            DVE(lambda e: e.reciprocal(out=PG, in_=GSUM), [R], [R])
            DVE(lambda e: e.tensor_scalar(out=GM, in0=LG[:, 0:4], scalar1=GMAX, scalar2=None, op0=ALU.is_equal), [R], [R])
            DVE(lambda e: e.tensor_scalar(out=PEN, in0=GM, scalar1=-1.0, scalar2=1e30, op0=ALU.add, op1=ALU.mult), [R], [R])
            DVE(lambda e: e.tensor_tensor(out=ELM.rearrange("p (g j) -> p g j", g=4), in0=LG[:, 4:36].rearrange("p (g j) -> p g j", g=4),
                                          in1=PEN.unsqueeze(2).broadcast_to([128, 4, 8]), op=ALU.add), [R], [R])
            yield
            DVE(lambda e: e.reduce_max(out=M1V, in_=ELM, axis=AX.X), [R], [R])
            DVE(lambda e: e.tensor_scalar(out=M1, in0=ELM, scalar1=M1V, scalar2=None, op0=ALU.is_equal), [R], [R])
            DVE(lambda e: e.scalar_tensor_tensor(out=ELM2, in0=M1, scalar=-1e30, in1=ELM, op0=ALU.mult, op1=ALU.add), [R], [R])
            DVE(lambda e: e.reduce_max(out=M2V, in_=ELM2, axis=AX.X), [R], [R])
            DVE(lambda e: e.tensor_scalar(out=M2, in0=ELM2, scalar1=M2V, scalar2=None, op0=ALU.is_equal), [R], [R])
            yield
            DVE(lambda e: e.tensor_tensor(out=DD, in0=M2V, in1=M1V, op=ALU.subtract), [R], [R])
            ACT(lambda e: e.activation(out=S2, in_=DD, func=AF.Sigmoid), [R], [R])
            DVE(lambda e: e.tensor_tensor(out=W2, in0=PG, in1=S2, op=ALU.mult), [R], [R])
            DVE(lambda e: e.tensor_tensor(out=W1, in0=PG, in1=W2, op=ALU.subtract), [R], [R])
            DVE(lambda e: e.tensor_tensor(out=Mb, in0=M1, in1=M2, op=ALU.add), [R], ["Mb"])
            mm(bank(7)[:, 64:96], ustrict, Mb, True, True, ["Mb", "ident"], [PR(7)])
            mm(bank(7)[:, 96:128], onesb, Mb, True, True, ["Mb", "ident"], [PR(7)])
            DVE(lambda e: e.tensor_tensor(out=POSR, in0=bank(7)[:, 64:96], in1=carry, op=ALU.add), [PR(7), "carry"], [R])
            DVE(lambda e: e.tensor_tensor(out=carry, in0=bank(7)[:, 96:128], in1=carry, op=ALU.add), [PR(7), "carry"], ["carry"])
            yield
            DVE(lambda e: e.scalar_tensor_tensor(out=POSB, in0=POSR, scalar=float(CAP), in1=ebase, op0=ALU.min, op1=ALU.add), [R, "ebase"], [R])
            DVE(lambda e: e.scalar_tensor_tensor(out=POSY, in0=POSR, scalar=float(CAP - 1), in1=ebaseY, op0=ALU.min, op1=ALU.add), [R, "ebase"], [R])
            for (Mx, Ix, Jx, Vx, Wx, col) in ((M1, I1, J1, V1, W1, 0), (M2, I2, J2, V2, W2, 1)):
                yield
                DVE(lambda e, Mx=Mx: e.tensor_tensor(out=TMPR, in0=Mx, in1=POSB, op=ALU.mult), [R], [R])
                DVE(lambda e, Ix=Ix: e.reduce_sum(out=Ix, in_=TMPR, axis=AX.X), [R], [R])
                DVE(lambda e, Mx=Mx: e.tensor_tensor(out=TMPR, in0=Mx, in1=POSR, op=ALU.mult), [R], [R])
                DVE(lambda e, Vx=Vx: e.reduce_sum(out=Vx, in_=TMPR, axis=AX.X), [R], [R])
                DVE(lambda e, Ix=Ix, col=col: e.tensor_copy(out=sidx[:, tile, col:col + 1], in_=Ix), [R], [("sidx", tile)])
                DVE(lambda e, Mx=Mx: e.tensor_tensor(out=TMPR, in0=Mx, in1=POSY, op=ALU.mult), [R], [R])
                DVE(lambda e, Jx=Jx: e.reduce_sum(out=Jx, in_=TMPR, axis=AX.X), [R], [R])
                DVE(lambda e, Jx=Jx, col=col: e.tensor_copy(out=gidx[:, tile, col:col + 1], in_=Jx), [R], [("gidx", tile)])
                DVE(lambda e, Vx=Vx, Wx=Wx, col=col: e.scalar_tensor_tensor(out=wts[:, tile, col:col + 1], in0=Vx, scalar=float(CAP), in1=Wx, op0=ALU.is_lt, op1=ALU.mult),
                    [R], [("wts", tile)])
            for col in range(2):
                S.add("pool", lambda e, col=col, bi=bi: e.indirect_dma_start(
                    out=XG, out_offset=bass.IndirectOffsetOnAxis(ap=sidx[:, tile, col:col + 1], axis=0), in_=xn3[bi], in_offset=None,
                    bounds_check=NE * CAPR - 1, oob_is_err=False),
                    [("xn3", bi), ("sidx", tile)], [("XG", tile, col)], dma=True, key="xgs%d" % bi, bsize=2)

        projX = [1, 2]
        xn2T2 = [xn2T, A.alloc([128, 8, 256], BF16)]
        qT22 = [qT2, A.alloc([128, 8, 256], BF16)]

        def interleaveX(*gens):
            alive = [g_ for g_ in gens if g_ is not None]
            while alive:
                for g_ in list(alive):
                    try:
                        next(g_)
                    except StopIteration:
                        alive.remove(g_)

        def S1_X(g):
            gp = g % 2
            for tl in range(2):
                tile = g * 2 + tl
                bi = tile % 2
                xb = xt2_X[bi]
                xr = ("xt", bi)
                DMA("sp", xb, xc[tile * 128:(tile + 1) * 128, :], [], [xr], "xt%d" % bi)
                for hf in range(2):
                    b = projX[hf]
                    for k in range(8):
                        mm(bank(b), mergedT[:, k, tile * 128:(tile + 1) * 128], Wmix[:, k, hf * 512:(hf + 1) * 512], k == 0, k == 7,
                           [("mT", k, tile // 4)] + wres("Wmix", 8), [PR(b)])
                    DVE(lambda e, b=b, hf=hf, tile=tile, xb=xb: e.tensor_tensor(out=h[:, tile, hf * 512:(hf + 1) * 512], in0=bank(b), in1=xb[:, hf * 512:(hf + 1) * 512], op=ALU.add),
                        [PR(b), xr], [("h", tile)])
                yield
                rmsnorm2(h[:, tile, :], ("h", tile), gbc_xa, "gbc_xa", xs_X, "xs")
                transpose8(xs_X, "xs", xn2T2[gp][:, :, tl * 128:(tl + 1) * 128], ("xn2T", gp, tl))
                yield

        def QT_X(g):
            gp = g % 2
            for c in range(8):
                b = projX[c % 2]
                for k in range(8):
                    mm(bank(b)[:, 0:256], Wq[:, k, c * 128:(c + 1) * 128], xn2T2[gp][:, k, :], k == 0, k == 7,
                       [("xn2T", gp, 0), ("xn2T", gp, 1)] + wres("Wq", 8), [PR(b)])
                ACT(lambda e, b=b, c=c, gp=gp: e.copy(out=qT22[gp][:, c, :], in_=bank(b)[:, 0:256]), [PR(b)], [("qT2", gp, c)])
                if c % 2 == 1:
                    yield

        def XA_X(tile):
            g, tl = tile // 2, tile % 2
            gp = g % 2
            sc = bank(3, 2).rearrange("p (h m) -> p h m", h=4)
            for hh in range(4):
                for kc in range(2):
                    mm(ps_t[:, 3 * 512 + hh * 256: 3 * 512 + (hh + 1) * 256], qT22[gp][:, 2 * hh + kc, tl * 128:(tl + 1) * 128], KT[:, 2 * hh + kc, :], kc == 0, kc == 1,
                       [("qT2", gp, 2 * hh + kc), "KT"], [PR(3 + hh // 2)])
            DVE(lambda e, sc=sc: e.reduce_max(out=stat[:, 8:12], in_=sc, axis=AX.X), [PR(3), PR(4)], ["xmx"])
            DVE(lambda e: e.tensor_scalar(out=stat[:, 12:16], in0=stat[:, 8:12], scalar1=-1.0 / 16, scalar2=None, op0=ALU.mult), ["xmx"], ["xnb"])
            for hh in range(4):
                ACT(lambda e, hh=hh: e.activation(out=Pb[:, hh, :], in_=ps_t[:, 3 * 512 + hh * 256: 3 * 512 + (hh + 1) * 256], func=AF.Exp,
                                                  bias=stat[:, 12 + hh:13 + hh], scale=1.0 / 16, accum_out=stat[:, 16 + hh:17 + hh]),
                    [PR(3 + hh // 2), "xnb"], [("Pb", hh), ("xsum", hh)])
            DVE(lambda e: e.reciprocal(out=stat[:, 20:24], in_=stat[:, 16:20]), [("xsum", hh) for hh in range(4)], ["xrs"])
            yield
            pb = bank_bf(0)
            Pv = Pb.rearrange("p h m -> p (h m)")
            for j in range(8):
                tp(pb[:, j, :], Pv[:, j * 128:(j + 1) * 128], [("Pb", j // 2)], [PR(0)])
            ACT(lambda e: e.copy(out=PTx, in_=bank_bf(0)), [PR(0)], ["PTx"])
            yield
            for hh in range(4):
                for mc in range(2):
                    mm(ps_t[:, 5 * 512 + hh * 256: 5 * 512 + (hh + 1) * 256], PTx[:, 2 * hh + mc, :], Vm[:, mc, hh * 256:(hh + 1) * 256], mc == 0, mc == 1,
                       ["PTx", "Vm"], [PR(5 + hh // 2)])
            DVE(lambda e: e.tensor_tensor(out=obx, in0=bank(5, 2).rearrange("p (h m) -> p h m", h=4),
                                          in1=stat[:, 20:24].unsqueeze(2).broadcast_to([128, 4, 256]), op=ALU.mult),
                [PR(5), PR(6), "xrs"], ["obx"])
            yield
            transpose8(obx.rearrange("p h m -> p (h m)"), "obx", oTx, "oTx")
            yield
            for hf in range(2):
                b = projX[hf]
                for k in range(8):
                    mm(bank(b), oTx[:, k, :], Wo[:, k, hf * 512:(hf + 1) * 512], k == 0, k == 7, ["oTx"] + wres("Wo", 8), [PR(b)])
                DVE(lambda e, b=b, hf=hf, tile=tile: e.tensor_tensor(out=h[:, tile, hf * 512:(hf + 1) * 512], in0=bank(b), in1=h[:, tile, hf * 512:(hf + 1) * 512], op=ALU.add),
                    [PR(b), ("h", tile)], [("h", tile)])
            yield

        interleaveX(S1_X(0))
        interleaveX(QT_X(0))
        pending = None
        for g in range(8):
            interleaveX(XA_X(2 * g), pending)
            interleaveX(XA_X(2 * g + 1), router(2 * g, 0), S1_X(g + 1) if g + 1 < 8 else None)
            if g + 1 < 8:
                interleaveX(QT_X(g + 1))
            pending = router(2 * g + 1, 1)
        interleaveX(pending)

        S.barrier()
        A.release(e_mark)
        A.limit = ARENA_BYTES

        if DBG:
            for tile in range(NT):
                DMA("sp", dbg_h[:, tile * D:(tile + 1) * D], h[:, tile, :], [("h", tile)], [("dbgh", tile)], "dbgh")
            S.barrier()
        if stop in ("X", "XNR", "X1"):
            raise _Stop()

        NR = 3
        Wg_r = [A.alloc([128, 8, 512], BF16) for _ in range(NR)]
        Wu_r = [A.alloc([128, 8, 512], BF16) for _ in range(NR)]
        Wd_r = [A.alloc([128, 4, D], BF16) for _ in range(NR)]
        xg4 = [A.alloc([128, 2, D], BF16) for _ in range(4)]
        xgT2 = [A.alloc([128, 8, 256], BF16) for _ in range(2)]
        hid2 = [A.alloc([128, 4, 256], BF16) for _ in range(2)]
        sge2 = [A.alloc([128, 256], F32) for _ in range(2)]
        ysb2 = [A.alloc([128, 2, D], BF16) for _ in range(2)]

        allxg = [("XG", tile, col) for tile in range(NT) for col in range(2)]
        gb_i = [0]
        def TG_E(ex):
            s = ex % NR
            bi = ex % 2
            DMA("pool", Wg_r[s], w_gate[ex].rearrange("(k p) n -> p k n", p=128), [], [("Wg", s, k) for k in range(8)], "wg%d" % s)
            DMA("pool", Wu_r[s], w_up[ex].rearrange("(k p) n -> p k n", p=128), [], [("Wu", s, k) for k in range(8)], "wu%d" % s)
            DMA("pool", Wd_r[s], w_down[ex].rearrange("(k p) n -> p k n", p=128), [], [("Wd", s, k) for k in range(4)], "wd%d" % s)
            for rb in range(2):
                pb = bank_bf(0)
                for k in range(8):
                    tp(pb[:, k, :], xg4[ex % 4][:, rb, k * 128:(k + 1) * 128], [("xg", ex % 4)], [PR(0)])
                if rb == 0:
                    ACT(lambda e, bi=bi, rb=rb: e.copy(out=xgT2[bi][:, :, rb * 128:(rb + 1) * 128], in_=bank_bf(0)), [PR(0)], [("xgT", bi, rb)])
                else:
                    DVE(lambda e, bi=bi, rb=rb: e.tensor_copy(out=xgT2[bi][:, :, rb * 128:(rb + 1) * 128], in_=bank_bf(0)), [PR(0)], [("xgT", bi, rb)])
            xgr = [("xgT", bi, 0), ("xgT", bi, 1)]
            for hc in range(4):
                gbk = 1 + (gb_i[0] % 2)
                ubk = 3 + (gb_i[0] % 2)
                gb_i[0] += 1
                for k in range(8):
                    mm(bank(gbk)[:, 0:256], Wg_r[s][:, k, hc * 128:(hc + 1) * 128], xgT2[bi][:, k, :], k == 0, k == 7, xgr + [("Wg", s, kk) for kk in range(8)], [PR(gbk)])
                for k in range(8):
                    mm(bank(ubk)[:, 0:256], Wu_r[s][:, k, hc * 128:(hc + 1) * 128], xgT2[bi][:, k, :], k == 0, k == 7, xgr + [("Wu", s, kk) for kk in range(8)], [PR(ubk)])
                sgb = sge2[hc % 2]
                ACT(lambda e, gbk=gbk, sgb=sgb: e.activation(out=sgb, in_=bank(gbk)[:, 0:256], func=AF.Sigmoid), [PR(gbk)], [("sge", hc % 2)])
                DVE(lambda e, gbk=gbk, sgb=sgb: e.tensor_tensor(out=sgb, in0=bank(gbk)[:, 0:256], in1=sgb, op=ALU.mult),
                    [PR(gbk), ("sge", hc % 2)], [("sge", hc % 2)])
                DVE(lambda e, ubk=ubk, sgb=sgb, bi=bi, hc=hc: e.tensor_tensor(out=hid2[bi][:, hc, :], in0=bank(ubk)[:, 0:256], in1=sgb, op=ALU.mult),
                    [PR(ubk), ("sge", hc % 2)], [("hid", bi, hc)])

        def DN_E(ex):
            s = ex % NR
            bi = ex % 2
            hr = [("hid", bi, hc) for hc in range(4)]
            for rb in range(2):
                for hf in range(2):
                    yb = 5 + ((rb * 2 + hf) % 3)
                    for hc in range(4):
                        mm(bank(yb), hid2[bi][:, hc, rb * 128:(rb + 1) * 128], Wd_r[s][:, hc, hf * 512:(hf + 1) * 512], hc == 0, hc == 3,
                           hr + [("Wd", s, kk) for kk in range(4)], [PR(yb)])
                    if hf == 0:
                        ACT(lambda e, yb=yb, bi=bi, rb=rb, hf=hf: e.copy(out=ysb2[bi][:, rb, hf * 512:(hf + 1) * 512], in_=bank(yb)), [PR(yb)], [("ysb", bi, rb, hf)])
                    else:
                        DVE(lambda e, yb=yb, bi=bi, rb=rb, hf=hf: e.tensor_copy(out=ysb2[bi][:, rb, hf * 512:(hf + 1) * 512], in_=bank(yb)), [PR(yb)], [("ysb", bi, rb, hf)])
            DMA("sp", YG[ex * CAP:(ex + 1) * CAP, :].rearrange("(b p) d -> p b d", p=128), ysb2[bi],
                [("ysb", bi, rb, hf) for rb in range(2) for hf in range(2)], [("YG", ex)], "yg%d" % bi)


        def XL_E(ex):
            DMA("sp", xg4[ex % 4], XG[ex * CAPR:ex * CAPR + CAP, :].rearrange("(b p) d -> p b d", p=128), allxg if ex < 4 else [], [("xg", ex % 4)], "xg%d" % (ex % 4))

        for ex in range(4):
            XL_E(ex)
        TG_E(0)
        for ex in range(NE):
            if ex + 4 < NE:
                XL_E(ex + 4)
            if ex + 1 < NE:
                TG_E(ex + 1)
            DN_E(ex)

        S.barrier()
        A.release(e_mark)

        gbc_C = A.alloc([128, D], F32)
        y12 = [[A.alloc([128, D], BF16) for _ in range(2)] for _ in range(2)]
        ot2 = [A.alloc([128, D], F32) for _ in range(2)]
        junk_C = A.alloc([128, D], BF16)
        DMA("sp", gbc_C, g_fin.partition_broadcast(128), [], ["gbc"], "gbc")
        outres = []

        def c_s1(tile):
            bi = tile % 2
            o_ = 24 + 3 * bi
            for col in range(2):
                S.add("pool", lambda e, col=col, bi=bi, tile=tile: e.indirect_dma_start(
                    out=y12[bi][col], out_offset=None, in_=YG, in_offset=bass.IndirectOffsetOnAxis(ap=gidx[:, tile, col:col + 1], axis=0)),
                    [("gidx", tile)], [("y12", bi, col)], dma=True, key="yga%d%d" % (bi, col))
            for col in range(2):
                DVE(lambda e, col=col, bi=bi, tile=tile: e.scalar_tensor_tensor(out=h[:, tile, :], in0=y12[bi][col], scalar=wts[:, tile, col:col + 1], in1=h[:, tile, :],
                                                                             op0=ALU.mult, op1=ALU.add),
                    [("y12", bi, col), ("wts", tile), ("h", tile)], [("h", tile)])
            ACT(lambda e, tile=tile, o_=o_: e.activation(out=junk_C, in_=h[:, tile, :], func=AF.Square, accum_out=stat[:, o_:o_ + 1]), [("h", tile)], ["junk", ("ssqC", bi)])
            ACT(lambda e, o_=o_: e.activation(out=stat[:, o_ + 1:o_ + 2], in_=stat[:, o_:o_ + 1], func=AF.Sqrt, bias=EPS, scale=1.0 / D), [("ssqC", bi)], [("stdC", bi)])

        def c_s2(tile):
            bi = tile % 2
            o_ = 24 + 3 * bi
            DVE(lambda e, o_=o_: e.reciprocal(out=stat[:, o_ + 2:o_ + 3], in_=stat[:, o_ + 1:o_ + 2]), [("stdC", bi)], [("rstdC", bi)])
            DVE(lambda e, tile=tile, bi=bi, o_=o_: e.scalar_tensor_tensor(out=ot2[bi], in0=h[:, tile, :], scalar=stat[:, o_ + 2:o_ + 3], in1=gbc_C, op0=ALU.mult, op1=ALU.mult),
                [("h", tile), ("rstdC", bi), "gbc"], [("ot", bi)])
            DMA("sp", out[tile * 128:(tile + 1) * 128, :], ot2[bi], [("ot", bi)], [("out", tile)], "out%d" % bi)
            outres.append(("out", tile))

        c_s1(0)
        for tile in range(NT):
            if tile + 1 < NT:
                c_s1(tile + 1)
            c_s2(tile)
        S.add("sp", None, outres)
        S.barrier()


    try:
        phases()
    except _Stop:
        S.barrier()

    S.resolve()
    sems = {}
    for e in ("pe", "act", "dve", "pool"):
        sems[("eng", e)] = es.enter_context(nc.semaphore("s_" + e))
    for k in S.keys:
        sems[("dma", k)] = es.enter_context(nc.semaphore("d_" + str(k)))
    with nc.Block() as block:
        block.sync(lambda e: S.run_engine("sp", e, sems))
        block.scalar(lambda e: S.run_engine("act", e, sems))
        block.vector(lambda e: S.run_engine("dve", e, sems))
        block.gpsimd(lambda e: S.run_engine("pool", e, sems))
        block.tensor(lambda e: S.run_engine("pe", e, sems))
    es.close()
    return nc, S, A


def _consts(half):
    p = np.arange(128, dtype=np.float64)
    inv_freq = 10000.0 ** (-np.arange(0, 64, 2, dtype=np.float64) / 64)

    def cs_tab(base):
        pos = base + np.arange(NT)[None, :] * 128 + p[:, None]
        ang = (pos[:, :, None].astype(np.float32) * inv_freq[None, None, :].astype(np.float32)).astype(np.float32)
        return np.stack([np.cos(ang), np.sin(ang)], axis=1).astype(np.float32)

    gam = 1.0 - 2.0 ** (-5.0 - np.arange(8, dtype=np.float64))
    lg = np.log(gam)
    gq = np.zeros((128, 4, 128), np.float32)
    gk = np.zeros((128, 4, 128), np.float32)
    ct = np.zeros((128, 4, 128), np.float32)
    i = np.arange(128, dtype=np.float64)
    for c in range(4):
        for hl in range(2):
            h = 2 * c + hl
            gq[hl * 64:(hl + 1) * 64, c, :] = np.exp((i + 1) * lg[h])[None, :]
            gk[hl * 64:(hl + 1) * 64, c, :] = (np.exp(-(i + 1) * lg[h]) / 8.0)[None, :]
            ct[hl * 64:(hl + 1) * 64, c, :] = np.exp(128 * lg[h])
    zt = (np.exp((127 - p)[:, None] * lg[None, :]) / 8.0).astype(np.float32)
    mask = (np.arange(128)[None, :] >= np.arange(128)[:, None]).astype(np.float32)
    ident = np.eye(128, dtype=np.float32)
    ustrict = (np.arange(128)[:, None] < np.arange(128)[None, :]).astype(np.float32)
    ones = np.ones((128, 128), np.float32)
    c_bf = np.concatenate([ident, ustrict, ones], axis=1)
    eb = np.concatenate([np.tile((np.arange(NE, dtype=np.float32) * CAPR)[None, :], (128, 1)),
                         np.tile((np.arange(NE, dtype=np.float32) * CAP)[None, :], (128, 1))], axis=1)
    return {
        "c_bf": c_bf, "c_cs_own": cs_tab(half * TOK), "c_cs_pre": cs_tab(0.0),
        "c_gq": gq, "c_gk": gk, "c_zt": zt, "c_ct": ct, "c_mask": np.ascontiguousarray(np.tile(mask[:, None, :], (1, 4, 1))), "c_eb": eb,
    }


_CACHE = {}


def kernel(x, mem, mix_norm_g, w_in, conv_w, w_conv_out, w_ret_out, w_mix_out,
           xa_norm_g, mem_norm_g, w_xa_q, w_xa_kv, w_xa_o, moe_norm_g,
           w_group, b_group, w_router, b_router, w_gate, w_up, w_down, final_norm_g):
    f = lambda a: np.ascontiguousarray(np.asarray(a, dtype=np.float32))
    x = f(x)
    mem = f(mem)
    if "nc" not in _CACHE:
        _CACHE["nc"] = build_program()
    nc = _CACHE["nc"][0]
    shared = {
        "w_in": f(w_in)[0], "conv_wT": np.ascontiguousarray(f(conv_w)[0].T), "w_conv_out": f(w_conv_out)[0],
        "w_ret_out": f(w_ret_out)[0], "w_mix_out": f(w_mix_out)[0], "w_xa_q": f(w_xa_q)[0], "w_xa_kv": f(w_xa_kv)[0],
        "w_xa_o": f(w_xa_o)[0], "g_mix": f(mix_norm_g)[0], "g_xa": f(xa_norm_g)[0], "g_mem": f(mem_norm_g)[0],
        "g_moe": f(moe_norm_g)[0], "g_fin": f(final_norm_g),
        "w_rt": np.ascontiguousarray(np.concatenate([f(w_group)[0], f(w_router)[0]], axis=1)),
        "b_rt": np.ascontiguousarray(np.concatenate([f(b_group)[0], f(b_router)[0]], axis=0)),
        "w_gate": f(w_gate)[0], "w_up": f(w_up)[0], "w_down": f(w_down)[0],
    }
    zeros = np.zeros((TOK, D), np.float32)
    in_maps = []
    for c in range(8):
        b, half = c // 2, c % 2
        m = dict(shared)
        m["xc"] = np.ascontiguousarray(x[b, half * TOK:(half + 1) * TOK])
        m["xp"] = np.ascontiguousarray(x[b, 0:TOK]) if half == 1 else zeros
        m["memc"] = np.ascontiguousarray(mem[b])
        m.update(_consts(half))
        in_maps.append(m)
    res = run_bass_kernel_spmd(nc, in_maps, core_ids=list(range(8)))
    _CACHE["res"] = res
    outp = np.empty((4, 2 * TOK, D), np.float32)
    for c in range(8):
        b, half = c // 2, c % 2
        outp[b, half * TOK:(half + 1) * TOK] = res.results[c]["out"]
    return outp
```

```python
import math
import numpy as np
from contextlib import ExitStack
import concourse.bass as bass
import concourse.mybir as mybir
from concourse.bass_utils import run_bass_kernel_spmd

F32 = mybir.dt.float32
BF16 = mybir.dt.bfloat16
I32 = mybir.dt.int32
U8 = mybir.dt.uint8
AF = mybir.ActivationFunctionType
ALU = mybir.AluOpType
AX = mybir.AxisListType

D = 1024
NT = 16
TOK = 2048
NE = 32
CAP = 256
CAPR = CAP + 1
EPS = 1e-6
INW = 6656
DBG = False


class Op:
    __slots__ = ("eng", "fn", "reads", "writes", "dma", "key", "idx", "sig", "ev", "deps", "xdeps", "bsize")

    def __init__(self, eng, fn, reads, writes, dma, key):
        self.eng = eng
        self.fn = fn
        self.reads = tuple(reads)
        self.writes = tuple(writes)
        self.dma = dma
        self.key = key
        self.sig = False
        self.ev = None
        self.deps = ()
        self.xdeps = ()
        self.bsize = 1


class Sched:
    ENGS = ("pe", "act", "dve", "pool", "sp")

    def __init__(self):
        self.ops = []
        self.last_eng = {}
        self.last_key = {}

    def add(self, eng, fn, reads=(), writes=(), dma=False, key=None, bsize=1):
        if dma:
            assert key is not None
        op = Op(eng, fn, reads, writes, dma, key)
        op.bsize = bsize
        op.idx = len(self.ops)
        self.ops.append(op)
        if dma:
            self.last_key[key] = op.idx
        elif fn is not None:
            self.last_eng[eng] = op.idx
        return op

    def barrier(self):
        deps = tuple(self.last_eng.values()) + tuple(self.last_key.values())
        for e in self.ENGS:
            op = self.add(e, None)
            op.xdeps = deps

    def resolve(self):
        last_w = {}
        readers = {}
        for op in self.ops:
            deps = set(op.xdeps)
            for r in op.reads:
                w = last_w.get(r)
                if w is not None:
                    deps.add(w)
            for w_ in op.writes:
                w = last_w.get(w_)
                if w is not None:
                    deps.add(w)
                for rd in readers.get(w_, {}).values():
                    deps.add(rd)
            deps.discard(op.idx)
            dl = []
            for d in sorted(deps):
                dop = self.ops[d]
                if dop.fn is None:
                    continue
                if op.eng == "pe" and dop.eng == "pe" and not dop.dma and not op.dma:
                    continue
                dop.sig = True
                dl.append(d)
            op.deps = tuple(dl)
            rk = ("dma", op.idx) if op.dma else op.eng
            for r in op.reads:
                readers.setdefault(r, {})[rk] = op.idx
            for w_ in op.writes:
                last_w[w_] = op.idx
                readers[w_] = {}
        cnt = {e: 0 for e in self.ENGS}
        keycnt = {}
        import os
        if os.environ.get("ALLSIG"):
            for op in self.ops:
                if not op.dma and op.fn is not None and op.eng != "sp":
                    op.sig = True
        for op in self.ops:
            if op.dma:
                keycnt[op.key] = keycnt.get(op.key, 0) + 16
                q = 16 * op.bsize
                op.ev = (("dma", op.key), (keycnt[op.key] + q - 1) // q * q)
            elif op.sig:
                cnt[op.eng] += 1
                op.ev = (("eng", op.eng), cnt[op.eng])
        self.keys = list(keycnt.keys())
        self.cnt = cnt
        return self

    def run_engine(self, eng, eobj, sems):
        waited = {}
        for op in self.ops:
            if op.eng != eng:
                continue
            need = {}
            for d in op.deps:
                sk, val = self.ops[d].ev
                if need.get(sk, 0) < val:
                    need[sk] = val
            for sk, val in need.items():
                if waited.get(sk, 0) >= val:
                    continue
                eobj.wait_ge(sems[sk], val)
                waited[sk] = val
            if op.fn is None:
                continue
            ins = op.fn(eobj)
            if op.dma:
                ins.then_inc(sems[op.ev[0]], 16)
            elif op.sig:
                ins.then_inc(sems[op.ev[0]], 1)


_DTSZ = {F32: 4, BF16: 2, I32: 4, U8: 1}


class Arena:
    def __init__(self, ap, size):
        self.ap = ap
        self.size = size
        self.off = 0
        self.peak = 0
        self.limit = size

    def mark(self):
        return self.off

    def release(self, m):
        self.off = m

    def alloc(self, shape, dt):
        n = 1
        for s in shape[1:]:
            n *= s
        nbytes = n * _DTSZ[dt]
        off = (self.off + 31) // 32 * 32
        assert off + nbytes <= self.limit, ("SBUF arena overflow", off, nbytes, self.limit)
        self.off = off + nbytes
        self.peak = max(self.peak, self.off)
        v = self.ap[:, off:off + nbytes].bitcast(dt)
        if len(shape) == 3:
            v = v.rearrange("p (a b) -> p a b", a=shape[1])
        elif len(shape) == 4:
            v = v.rearrange("p (a b c) -> p a b c", a=shape[1], b=shape[2])
        return v


def build_program(stop=None):
    nc = bass.Bass("TRN2", target_bir_lowering=False)

    def din(name, shape, dt=F32):
        return nc.dram_tensor(name, list(shape), dt, kind="ExternalInput").ap()

    xc = din("xc", [TOK, D])
    xp = din("xp", [TOK, D])
    memc = din("memc", [256, D])
    w_in = din("w_in", [D, INW])
    conv_wT = din("conv_wT", [512, 3])
    w_conv_out = din("w_conv_out", [512, D])
    w_ret_out = din("w_ret_out", [D, D])
    w_mix_out = din("w_mix_out", [D, D])
    w_xa_q = din("w_xa_q", [D, D])
    w_xa_kv = din("w_xa_kv", [D, 2 * D])
    w_xa_o = din("w_xa_o", [D, D])
    g_mix = din("g_mix", [D])
    g_xa = din("g_xa", [D])
    g_mem = din("g_mem", [D])
    g_moe = din("g_moe", [D])
    g_fin = din("g_fin", [D])
    w_rt = din("w_rt", [D, 36])
    b_rt = din("b_rt", [36])
    w_gate = din("w_gate", [NE, D, 512])
    w_up = din("w_up", [NE, D, 512])
    w_down = din("w_down", [NE, 512, D])
    c_bf = din("c_bf", [128, 384])
    c_cs_own = din("c_cs_own", [128, 2, NT, 32])
    c_cs_pre = din("c_cs_pre", [128, 2, NT, 32])
    c_gq = din("c_gq", [128, 4, 128])
    c_gk = din("c_gk", [128, 4, 128])
    c_zt = din("c_zt", [128, 8])
    c_ct = din("c_ct", [128, 4, 128])
    c_mask = din("c_mask", [128, 4, 128])
    c_eb = din("c_eb", [128, 2 * NE])
    out = nc.dram_tensor("out", [TOK, D], F32, kind="ExternalOutput").ap()
    XG = nc.dram_tensor("xg_scr", [NE * CAPR, D], BF16, kind="Internal").ap()
    YG = nc.dram_tensor("yg_scr", [NE * CAP, D], BF16, kind="Internal").ap()
    if DBG:
        dbg_m = nc.dram_tensor("dbg_m", [128, 8 * TOK], F32, kind="ExternalOutput").ap()
        dbg_h = nc.dram_tensor("dbg_h", [128, NT * D], F32, kind="ExternalOutput").ap()

    S = Sched()
    es = ExitStack()
    ARENA_BYTES = 207 * 1024
    arena_t = es.enter_context(nc.sbuf_tensor("arena", [128, ARENA_BYTES], U8))
    A = Arena(arena_t, ARENA_BYTES)
    ps_t = es.enter_context(nc.psum_tensor("ps", [128, 4096], F32))

    def bank(i, n=1):
        return ps_t[:, i * 512:(i + n) * 512]

    def bank_bf(i):
        return ps_t[:, i * 512:(i + 1) * 512].bitcast(BF16).rearrange("p (a b) -> p a b", a=8)

    def PR(i):
        return ("ps", i)

    uid = [0]

    def ukey(p):
        uid[0] += 1
        return "%s%d" % (p, uid[0])

    def PE(fn, r, w):
        return S.add("pe", fn, r, w)

    def ACT(fn, r, w):
        return S.add("act", fn, r, w)

    def DVE(fn, r, w):
        return S.add("dve", fn, r, w)

    def POOL(fn, r, w):
        return S.add("pool", fn, r, w)

    def DMA(eng, out_, in_, r, w, key, nb=1):
        return S.add(eng, lambda e: e.dma_start(out=out_, in_=in_), r, w, dma=True, key=key, bsize=nb)

    def mm(out_, lhsT, rhs, start, stop, r, w):
        return PE(lambda e: e.matmul(out_, lhsT=lhsT, rhs=rhs, start=start, stop=stop), r, w)

    def tp(out_, in_, r, w):
        return PE(lambda e: e.transpose(out=out_, in_=in_, identity=ident), r + ["ident"], w)

    def load_w(dst, src, rows_k, res, key):
        for k in range(rows_k):
            DMA("pool", dst[:, k, :], src[k * 128:(k + 1) * 128, :], [], [(res, k)], key, nb=rows_k)

    def wres(res, n):
        return [(res, k) for k in range(n)]

    ident3 = A.alloc([128, 3, 128], BF16)
    ident = ident3[:, 0, :]
    ustrict = ident3[:, 1, :]
    onesb = ident3[:, 2, :]
    DMA("pool", ident3, c_bf.rearrange("p (a b) -> p a b", a=3), [], ["ident"], "c_bf")
    MT_BYTES = 8 * TOK * 2
    mergedT = arena_t[:, ARENA_BYTES - MT_BYTES:ARENA_BYTES].bitcast(BF16).rearrange("p (a b) -> p a b", a=8)
    A.limit = ARENA_BYTES - MT_BYTES
    stat = A.alloc([128, 64], F32)
    wts = A.alloc([128, NT, 2], F32)
    sidx = A.alloc([128, NT, 2], I32)
    gidx = A.alloc([128, NT, 2], I32)
    ssq = stat[:, 0:1]
    std = stat[:, 1:2]
    rstd = stat[:, 2:3]
    persist_mark = A.mark()

    def rmsnorm(xt_ap, xt_res, g_bc, xs_ap, junk_ap, xs_res="xs"):
        ACT(lambda e: e.activation(out=junk_ap, in_=xt_ap, func=AF.Square, accum_out=ssq), [xt_res], ["junk", "ssq"])
        ACT(lambda e: e.activation(out=std, in_=ssq, func=AF.Sqrt, bias=EPS, scale=1.0 / D), ["ssq"], ["std"])
        DVE(lambda e: e.reciprocal(out=rstd, in_=std), ["std"], ["rstd"])
        DVE(lambda e: e.scalar_tensor_tensor(out=xs_ap, in0=xt_ap, scalar=rstd, in1=g_bc, op0=ALU.mult, op1=ALU.mult),
            [xt_res, "rstd", "gbc"], [xs_res])

    def transpose8(src_ap, src_res, dst_ap, dst_res, nblk=8, copy_eng="act"):
        pb = bank_bf(0)
        for k in range(nblk):
            tp(pb[:, k, :], src_ap[:, k * 128:(k + 1) * 128], [src_res], [PR(0)])
        if copy_eng == "act":
            ACT(lambda e: e.copy(out=dst_ap, in_=pb[:, 0:nblk, :]), [PR(0)], [dst_res])
        else:
            DVE(lambda e: e.tensor_copy(out=dst_ap, in_=pb[:, 0:nblk, :]), [PR(0)], [dst_res])

    def dump_m():
        dtmp = A.alloc([128, 8, 512], F32)
        for g in range(4):
            DVE(lambda e, g=g: e.tensor_copy(out=dtmp, in_=mergedT[:, :, g * 512:(g + 1) * 512]), [("mT", oc, g) for oc in range(8)] + ["dbgo"], ["dtmp"])
            DMA("sp", dbg_m.rearrange("p (k t) -> p k t", k=8)[:, :, g * 512:(g + 1) * 512], dtmp, ["dtmp"], ["dbgo"], "dbgo")
        S.barrier()

    class _Stop(Exception):
        pass

    def phases():
        WinA = A.alloc([128, 8, 2560], BF16)
        Wco = A.alloc([128, 4, 1024], BF16)
        gbc_A = A.alloc([128, D], F32)
        convw = A.alloc([128, 4, 3], F32)
        xt2_A = [A.alloc([128, D], F32) for _ in range(2)]
        xs_A = A.alloc([128, D], BF16)
        junk_A = A.alloc([128, D], BF16)
        xnT4 = [A.alloc([128, 8, 512], BF16) for _ in range(2)]
        xin_sb = A.alloc([128, 4, 512], F32)
        u = A.alloc([128, 4, 514], F32)
        cc = A.alloc([128, 4, 512], F32)
        bc = A.alloc([128, 4, 512], BF16)
        sg2 = [A.alloc([128, 512], F32) for _ in range(2)]

        zt_ = A.alloc([128, 4112], BF16)
        POOL(lambda e: e.memset(zt_, 0.0), [], ["zfill"])
        XGf = XG.rearrange("r d -> (r d)").rearrange("(p n) -> p n", p=128)
        DMA("sp", gbc_A, g_mix.partition_broadcast(128), [], ["gbc"], "gbc")
        DMA("sp", convw, conv_wT.rearrange("(c p) k -> p c k", p=128), [], ["convw"], "convw")
        for k in range(8):
            DMA("pool", WinA[:, k, 0:1536], w_in[k * 128:(k + 1) * 128, 0:1536], [], [("WinA", k)], "WinA", nb=8)
        for k in range(4):
            DMA("pool", Wco[:, k, :], w_conv_out[k * 128:(k + 1) * 128, :], [], [("Wco", k)], "Wco", nb=4)
        for k in range(8):
            DMA("pool", WinA[:, k, 1536:2560], w_in[k * 128:(k + 1) * 128, 4608:5632], [], [("WinAg", k)], "WinAg", nb=8)
        DMA("sp", xt2_A[1], xp[TOK - 128:TOK, :], [], [("xt", 1)], "xt1")
        rmsnorm(xt2_A[1], ("xt", 1), gbc_A, xs_A, junk_A, xs_res=("xsA", 0))
        transpose8(xs_A, ("xsA", 0), xnT4[1][:, :, 0:128], ("xnT4", 1, 0))
        for c in range(4):
            for k in range(8):
                mm(bank(1)[:, 0:128], WinA[:, k, c * 128:(c + 1) * 128], xnT4[1][:, k, 0:128], k == 0, k == 7, [("xnT4", 1, 0)] + wres("WinA", 8), [PR(1)])
            ACT(lambda e, c=c: e.copy(out=xin_sb[:, c, 0:128], in_=bank(1)[:, 0:128]), [PR(1)], [("xin", c)])
            for k in range(8):
                mm(bank(2)[:, 0:128], WinA[:, k, 1024 + c * 128:1024 + (c + 1) * 128], xnT4[1][:, k, 0:128], k == 0, k == 7, [("xnT4", 1, 0)] + wres("WinA", 8), [PR(2)])
            DVE(lambda e, c=c: e.tensor_tensor(out=u[:, c, 0:2], in0=bank(2)[:, 126:128], in1=xin_sb[:, c, 126:128], op=ALU.mult),
                [PR(2), ("xin", c)], ["u"])

        projA = [1, 2, 3, 6, 7]
        pa_i = [0]

        def next_proj(pool):
            b = pool[pa_i[0] % len(pool)]
            pa_i[0] += 1
            return b

        xs4 = [xs_A] + [A.alloc([128, D], BF16) for _ in range(3)]
        sg8 = list(sg2) + [A.alloc([128, 512], F32) for _ in range(6)]

        def hnA(g, t):
            tile = g * 4 + t
            xb = xt2_A[tile % 2]
            xr = ("xt", tile % 2)
            DMA("sp", xb, xc[tile * 128:(tile + 1) * 128, :], [], [xr], "xt%d" % (tile % 2))
            ACT(lambda e: e.activation(out=junk_A, in_=xb, func=AF.Square, accum_out=ssq), [xr], ["junk", "ssq"])
            ACT(lambda e: e.activation(out=std, in_=ssq, func=AF.Sqrt, bias=EPS, scale=1.0 / D), ["ssq"], ["std"])
            DVE(lambda e: e.reciprocal(out=rstd, in_=std), ["std"], ["rstd"])
            DVE(lambda e: e.scalar_tensor_tensor(out=xs4[t], in0=xb, scalar=rstd, in1=gbc_A, op0=ALU.mult, op1=ALU.mult),
                [xr, "rstd", "gbc"], [("xsA", t)])

        def htA(g, t):
            transpose8(xs4[t], ("xsA", t), xnT4[g % 2][:, :, t * 128:(t + 1) * 128], ("xnT4", g % 2, t))

        def xinA(g):
            gb = g % 2
            xnr = [("xnT4", gb, t) for t in range(4)]
            for c in range(4):
                b = next_proj(projA)
                for k in range(8):
                    mm(bank(b), WinA[:, k, c * 128:(c + 1) * 128], xnT4[gb][:, k, :], k == 0, k == 7, xnr + wres("WinA", 8), [PR(b)])
                ACT(lambda e, b=b, c=c: e.copy(out=xin_sb[:, c, :], in_=bank(b)), [PR(b)], [("xin", c)])

        def cgA(g):
            gb = g % 2
            xnr = [("xnT4", gb, t) for t in range(4)]
            for c in range(4):
                b = next_proj(projA)
                for k in range(8):
                    mm(bank(b), WinA[:, k, 1024 + c * 128:1024 + (c + 1) * 128], xnT4[gb][:, k, :], k == 0, k == 7, xnr + wres("WinA", 8), [PR(b)])
                DVE(lambda e, b=b, c=c: e.tensor_tensor(out=u[:, c, 2:514], in0=bank(b), in1=xin_sb[:, c, :], op=ALU.mult),
                    [PR(b), ("xin", c)], ["u"])
                POOL(lambda e, c=c: e.tensor_scalar(out=cc[:, c, :], in0=u[:, c, 0:512], scalar1=convw[:, c, 0:1], scalar2=None, op0=ALU.mult),
                     ["u", "convw"], [("cc", c)])
                DVE(lambda e, c=c: e.scalar_tensor_tensor(out=cc[:, c, :], in0=u[:, c, 1:513], scalar=convw[:, c, 1:2], in1=cc[:, c, :], op0=ALU.mult, op1=ALU.add),
                    ["u", "convw", ("cc", c)], [("cc", c)])
                DVE(lambda e, c=c: e.scalar_tensor_tensor(out=cc[:, c, :], in0=u[:, c, 2:514], scalar=convw[:, c, 2:3], in1=cc[:, c, :], op0=ALU.mult, op1=ALU.add),
                    ["u", "convw", ("cc", c)], [("cc", c)])
            POOL(lambda e: e.tensor_copy(out=u[:, :, 0:2], in_=u[:, :, 512:514]), ["u"], ["u"])

        def bgA(g):
            gb = g % 2
            xnr = [("xnT4", gb, t) for t in range(4)]
            for c in range(4):
                b = next_proj(projA)
                for k in range(8):
                    mm(bank(b), WinA[:, k, 512 + c * 128:512 + (c + 1) * 128], xnT4[gb][:, k, :], k == 0, k == 7, xnr + wres("WinA", 8), [PR(b)])
                DVE(lambda e, b=b, c=c: e.tensor_tensor(out=bc[:, c, :], in0=bank(b), in1=cc[:, c, :], op=ALU.mult),
                    [PR(b), ("cc", c)], [("bc", c)])

        def gateA(g, ocs):
            gb = g % 2
            xnr = [("xnT4", gb, t) for t in range(4)]
            for oc in ocs:
                b = next_proj(projA)
                for k in range(8):
                    mm(bank(b), WinA[:, k, 1536 + oc * 128:1536 + (oc + 1) * 128], xnT4[gb][:, k, :], k == 0, k == 7, xnr + wres("WinAg", 8), [PR(b)])
                ACT(lambda e, b=b, oc=oc: e.activation(out=sg8[oc], in_=bank(b), func=AF.Sigmoid), [PR(b)], [("sg", oc)])

        def yconvA(g, ocs):
            for oc in ocs:
                yb = 4 + (oc % 2)
                for k in range(4):
                    mm(bank(yb), Wco[:, k, oc * 128:(oc + 1) * 128], bc[:, k, :], k == 0, k == 3, [("bc", kk) for kk in range(4)] + wres("Wco", 4), [PR(yb)])
                DVE(lambda e, yb=yb, oc=oc, g=g: e.tensor_tensor(out=mergedT[:, oc, g * 512:(g + 1) * 512], in0=bank(yb), in1=sg8[oc], op=ALU.mult),
                    [PR(yb), ("sg", oc)], [("mT", oc, g)])

        for t in range(4):
            hnA(0, t)
            htA(0, t)
        for g in range(4):
            nx = g + 1 if g + 1 < 4 else None
            if nx is not None:
                hnA(nx, 0)
            xinA(g)
            if nx is not None:
                htA(nx, 0)
                hnA(nx, 1)
            cgA(g)
            if nx is not None:
                htA(nx, 1)
                hnA(nx, 2)
            gateA(g, range(0, 4))
            bgA(g)
            if nx is not None:
                htA(nx, 2)
                hnA(nx, 3)
            gateA(g, range(4, 8))
            if nx is not None:
                htA(nx, 3)
            yconvA(g, range(8))
            if g == 0:
                for i in range(16):
                    DMA("sp", XGf[:, i * 4112:(i + 1) * 4112], zt_, ["zfill"], [("XGz", i)], "xgz", nb=16)

        S.barrier()
        A.release(persist_mark)
        if stop == "A":
            dump_m()
            raise _Stop()

        WinB = A.alloc([128, 8, 4096], BF16)
        Wro = A.alloc([128, 8, 1024], BF16)
        gbc_B = A.alloc([128, D], F32)
        cs = A.alloc([128, 2, NT, 32], F32)
        gq = A.alloc([128, 4, 128], F32)
        gk = A.alloc([128, 4, 128], F32)
        zt = A.alloc([128, 8], F32)
        ct = A.alloc([128, 4, 128], F32)
        maskT = A.alloc([128, 4, 128], F32)
        Sst = A.alloc([128, 4, 128], F32)
        Sb = A.alloc([128, 4, 128], BF16)
        xt2_B = [A.alloc([128, D], F32) for _ in range(2)]
        xs_B = A.alloc([128, D], BF16)
        junk_B = A.alloc([128, D], BF16)
        xnT4b2 = [A.alloc([128, 8, 512], BF16) for _ in range(2)]
        xs_B2 = [xs_B, A.alloc([128, D], BF16)]
        qr2 = [A.alloc([128, 8, 2, 32], BF16) for _ in range(2)]
        kr2 = [A.alloc([128, 8, 2, 32], BF16) for _ in range(2)]
        kz2 = [A.alloc([128, 8, 64], BF16) for _ in range(2)]
        v2 = [A.alloc([128, D], BF16) for _ in range(2)]
        sgt2 = [A.alloc([128, D], BF16) for _ in range(2)]
        rt = [A.alloc([128, 8, 32], F32) for _ in range(4)]
        qTz = A.alloc([128, 4, 2, 128], BF16)
        kT = A.alloc([128, 4, 128], BF16)
        PT = A.alloc([128, 8, 128], BF16)
        osq = A.alloc([128, D], F32)
        zb = A.alloc([128, D], BF16)
        zT4 = A.alloc([128, 8, 512], BF16)
        sgr2 = [A.alloc([128, 512], F32) for _ in range(2)]
        tmpm = A.alloc([128, 512], F32)
        gst = A.alloc([128, 64], F32)

        DMA("sp", gbc_B, g_mix.partition_broadcast(128), [], ["gbc"], "gbc")
        DMA("sp", cs, c_cs_pre, [], ["cs"], "cs")
        DMA("sp", gq, c_gq, [], ["gq"], "c_gq")
        DMA("sp", gk, c_gk, [], ["gk"], "c_gk")
        DMA("sp", zt, c_zt, [], ["zt"], "c_zt")
        DMA("sp", ct, c_ct, [], ["ct"], "c_ct")
        DMA("sp", maskT, c_mask, [], ["maskT"], "c_mask")
        for k in range(8):
            DMA("pool", WinB[:, k, 512:2048], w_in[k * 128:(k + 1) * 128, 2048:3584], [], [("WinBkv", k)], "WinBkv", nb=8)
        for k in range(8):
            DMA("pool", WinB[:, k, 0:512], w_in[k * 128:(k + 1) * 128, 1536:2048], [], [("WinBq", k)], "WinBq", nb=8)
        for k in range(8):
            DMA("pool", WinB[:, k, 2048:3072], w_in[k * 128:(k + 1) * 128, 3584:4608], [], [("WinBg", k)], "WinBg", nb=8)
        for k in range(8):
            DMA("pool", WinB[:, k, 3072:4096], w_in[k * 128:(k + 1) * 128, 5632:6656], [], [("WinBr", k)], "WinBr", nb=8)
        load_w(Wro, w_ret_out, 8, "Wro", "Wro")
        POOL(lambda e: e.memset(Sst, 0.0), [], ["Sst"])
        POOL(lambda e: e.memset(Sb, 0.0), [], ["Sb"])
        POOL(lambda e: e.memset(qTz, 0.0), [], ["qT"])

        projB = [1, 2, 7]
        pa_i[0] = 0

        def rotary(pb_ap, pres, tile, dst, dres, ti):
            pv = pb_ap.rearrange("p (h t f) -> p h t f", h=8, t=2)
            cosb = cs[:, 0, tile, :].unsqueeze(1).broadcast_to([128, 8, 32])
            sinb = cs[:, 1, tile, :].unsqueeze(1).broadcast_to([128, 8, 32])
            ta, tb, tc, td = rt
            DVE(lambda e: e.tensor_tensor(out=ta, in0=pv[:, :, 0, :], in1=cosb, op=ALU.mult), [pres, "cs"], ["rta"])
            DVE(lambda e: e.tensor_tensor(out=tb, in0=pv[:, :, 1, :], in1=sinb, op=ALU.mult), [pres, "cs"], ["rtb"])
            DVE(lambda e: e.tensor_tensor(out=tc, in0=pv[:, :, 0, :], in1=sinb, op=ALU.mult), [pres, "cs"], ["rtc"])
            DVE(lambda e: e.tensor_tensor(out=td, in0=pv[:, :, 1, :], in1=cosb, op=ALU.mult), [pres, "cs"], ["rtd"])
            POOL(lambda e: e.tensor_tensor(out=dst[:, :, 0, :], in0=ta, in1=tb, op=ALU.subtract), ["rta", "rtb"], [(dres, 0)])
            POOL(lambda e: e.tensor_tensor(out=dst[:, :, 1, :], in0=tc, in1=td, op=ALU.add), ["rtc", "rtd"], [(dres, 1)])

        def state_update(bi):
            kzv = kz2[bi].rearrange("p h d -> p (h d)")
            for c in range(4):
                ob = ps_t[:, 3 * 512 + c * 256: 3 * 512 + (c + 1) * 256]
                mm(ob, kzv[:, c * 128:(c + 1) * 128], v2[bi][:, c * 256:(c + 1) * 256], True, True,
                   [("kz", bi), ("v", bi)], [PR(3), PR(4)])
            pS = bank(3, 2).rearrange("p (c n) -> p c n", c=4)
            POOL(lambda e: e.tensor_tensor(out=Sst, in0=Sst, in1=ct, op=ALU.mult), ["Sst", "ct"], ["Sst"])
            DVE(lambda e: e.tensor_tensor(out=Sst[0:64], in0=Sst[0:64], in1=pS[0:64, :, 0:128], op=ALU.add), ["Sst", PR(3), PR(4)], ["Sst"])
            DVE(lambda e: e.tensor_tensor(out=Sst[64:128], in0=Sst[64:128], in1=pS[64:128, :, 128:256], op=ALU.add), ["Sst", PR(3), PR(4)], ["Sst"])
            ACT(lambda e: e.copy(out=Sb, in_=Sst), ["Sst"], ["Sb"])

        def proj_tm(xnT_ap, xn_res, col0, wres_, b):
            for k in range(8):
                mm(bank(b), xnT_ap[:, k, :], WinB[:, k, col0:col0 + 512], k == 0, k == 7, xn_res + wres_, [PR(b)])

        def RPp(tile):
            bi = tile % 2
            xb = xt2_B[bi]
            xr = ("xt", bi)
            DMA("sp", xb, xp[tile * 128:(tile + 1) * 128, :], [], [xr], "xt%d" % bi)
            rmsnorm(xb, xr, gbc_B, xs_B2[bi], junk_B, xs_res=("xsB", bi))

        def RPt(tile):
            bi = tile % 2
            transpose8(xs_B2[bi], ("xsB", bi), xnT4b2[1][:, :, (tile % 4) * 128:(tile % 4 + 1) * 128], ("xnT4b", 1, tile % 4))

        def PPp(tile):
            bi = tile % 2
            xnTp_ = xnT4b2[1][:, :, (tile % 4) * 128:(tile % 4 + 1) * 128]
            xres_ = [("xnT4b", 1, tile % 4)]
            b = next_proj(projB)
            proj_tm(xnTp_, xres_, 512, wres("WinBkv", 8), b)
            rotary(bank(b), PR(b), tile, kr2[bi], ("kr", bi), 1)
            POOL(lambda e, bi=bi: e.tensor_tensor(out=kz2[bi], in0=kr2[bi].rearrange("p h t f -> p h (t f)"),
                                                  in1=zt.unsqueeze(2).broadcast_to([128, 8, 64]), op=ALU.mult),
                 [(("kr", bi), 0), (("kr", bi), 1), "zt"], [("kz", bi)])
            for hf in range(2):
                b = next_proj(projB)
                proj_tm(xnTp_, xres_, 1024 + hf * 512, wres("WinBkv", 8), b)
                ACT(lambda e, b=b, bi=bi, hf=hf: e.copy(out=v2[bi][:, hf * 512:(hf + 1) * 512], in_=bank(b)), [PR(b)], [("v", bi)])
            state_update(bi)

        RPp(0)
        RPt(0)
        RPp(1)
        RPt(1)
        for tile in range(NT):
            if tile + 2 < NT:
                RPp(tile + 2)
            PPp(tile)
            if tile + 2 < NT:
                RPt(tile + 2)

        if stop == "B1":
            raise _Stop()

        def chk(n):
            if stop == "B2:%d" % n:
                raise _Stop()
        DMA("sp", cs, c_cs_own, [], ["cs"], "cs")

        def RB(g, t):
            tile = g * 4 + t
            bi = tile % 2
            xb = xt2_B[bi]
            xr = ("xt", bi)
            DMA("sp", xb, xc[tile * 128:(tile + 1) * 128, :], [], [xr], "xt%d" % bi)
            rmsnorm(xb, xr, gbc_B, xs_B2[bi], junk_B, xs_res=("xsB", bi))

        def RBt(g, t):
            tile = g * 4 + t
            bi = tile % 2
            xnT_t = xnT4b2[g % 2][:, :, t * 128:(t + 1) * 128]
            transpose8(xs_B2[bi], ("xsB", bi), xnT_t, ("xnT4b", g % 2, t))

        def PB(g, t):
            tile = g * 4 + t
            bi = tile % 2
            xnT_t = xnT4b2[g % 2][:, :, t * 128:(t + 1) * 128]
            xnres = [("xnT4b", g % 2, t)]
            b = next_proj(projB)
            proj_tm(xnT_t, xnres, 0, wres("WinBq", 8), b)
            rotary(bank(b), PR(b), tile, qr2[bi], ("qr", bi), 0)
            b = next_proj(projB)
            proj_tm(xnT_t, xnres, 512, wres("WinBkv", 8), b)
            rotary(bank(b), PR(b), tile, kr2[bi], ("kr", bi), 1)
            POOL(lambda e, bi=bi: e.tensor_tensor(out=kz2[bi], in0=kr2[bi].rearrange("p h t f -> p h (t f)"),
                                                  in1=zt.unsqueeze(2).broadcast_to([128, 8, 64]), op=ALU.mult),
                 [(("kr", bi), 0), (("kr", bi), 1), "zt"], [("kz", bi)])
            yield
            for hf in range(2):
                b = next_proj(projB)
                proj_tm(xnT_t, xnres, 1024 + hf * 512, wres("WinBkv", 8), b)
                ACT(lambda e, b=b, bi=bi, hf=hf: e.copy(out=v2[bi][:, hf * 512:(hf + 1) * 512], in_=bank(b)), [PR(b)], [("v", bi)])
            yield
            for hf in range(2):
                b = next_proj(projB)
                proj_tm(xnT_t, xnres, 2048 + hf * 512, wres("WinBg", 8), b)
                ACT(lambda e, b=b, hf=hf: e.activation(out=sgr2[hf], in_=bank(b), func=AF.Sigmoid), [PR(b)], [("sgr", hf)])
                DVE(lambda e, b=b, bi=bi, hf=hf: e.tensor_tensor(out=sgt2[bi][:, hf * 512:(hf + 1) * 512], in0=bank(b), in1=sgr2[hf], op=ALU.mult),
                    [PR(b), ("sgr", hf)], [("sgt", bi)])

        def tailB(g, t):
            tile = g * 4 + t
            bi = tile % 2
            pb = bank_bf(0)
            qrv = qr2[bi].rearrange("p h t f -> p (h t f)")
            krv = kr2[bi].rearrange("p h t f -> p (h t f)")
            for c in range(4):
                tp(pb[:, c, :], qrv[:, c * 128:(c + 1) * 128], [(("qr", bi), 0), (("qr", bi), 1)], [PR(0)])
            for c in range(4):
                tp(pb[:, 4 + c, :], krv[:, c * 128:(c + 1) * 128], [(("kr", bi), 0), (("kr", bi), 1)], [PR(0)])
            DVE(lambda e: e.tensor_tensor(out=qTz[0:64, :, 0, :], in0=bank_bf(0)[0:64, 0:4, :], in1=gq[0:64], op=ALU.mult), [PR(0), "gq"], ["qT"])
            DVE(lambda e: e.tensor_tensor(out=qTz[64:128, :, 1, :], in0=bank_bf(0)[64:128, 0:4, :], in1=gq[64:128], op=ALU.mult), [PR(0), "gq", "qT"], ["qT"])
            DVE(lambda e: e.tensor_tensor(out=kT, in0=bank_bf(0)[:, 4:8, :], in1=gk, op=ALU.mult), [PR(0), "gk"], ["kT"])
            yield
            for c in range(4):
                mm(ps_t[:, 3 * 512 + c * 256: 3 * 512 + (c + 1) * 256], kT[:, c, :], qTz[:, c, :, :].rearrange("p a q -> p (a q)"), True, True,
                   ["qT", "kT"], [PR(3 + c // 2)])
            mb = maskT
            DVE(lambda e, mb=mb: e.tensor_tensor(out=PT[:, 0:4, :], in0=bank(3).rearrange("p (h q) -> p h q", h=4), in1=mb, op=ALU.mult),
                [PR(3), "maskT"], [("PT", 0)])
            DVE(lambda e, mb=mb: e.tensor_tensor(out=PT[:, 4:8, :], in0=bank(4).rearrange("p (h q) -> p h q", h=4), in1=mb, op=ALU.mult),
                [PR(4), "maskT"], [("PT", 1)])
            yield
            for h in range(8):
                p0 = (h % 2) * 64
                ob = ps_t[:, 5 * 512 + h * 128: 5 * 512 + (h + 1) * 128]
                mm(ob, PT[:, h, :], v2[bi][:, h * 128:(h + 1) * 128], True, False, [("PT", h // 4), ("v", bi)], [PR(5 + h // 4)])
                mm(ob, qTz[:, h // 2, h % 2, :], Sb[:, h // 2, :], False, True, ["qT", "Sb"], [PR(5 + h // 4)])
            yield
            for hb in range(2):
                ACT(lambda e, hb=hb: e.copy(out=osq[:, hb * 512:(hb + 1) * 512], in_=bank(5 + hb)), [PR(5 + hb)],
                    [("osq", hb)] + [("on", hh_) for hh_ in range(hb * 4, hb * 4 + 4)])
            DVE(lambda e: e.reduce_sum(out=gst[:, 0:8], in_=osq.rearrange("p (h e) -> p h e", h=8), axis=AX.X), [("osq", 0), ("osq", 1)], ["g_sum"])
            for h in range(8):
                ACT(lambda e, h=h: e.activation(out=junk_B[:, h * 128:(h + 1) * 128], in_=osq[:, h * 128:(h + 1) * 128], func=AF.Square,
                                                accum_out=gst[:, 8 + h:9 + h]),
                    [("osq", h // 4)], ["junk", ("g_sq", h)])
            DVE(lambda e: e.tensor_scalar(out=gst[:, 16:24], in0=gst[:, 0:8], scalar1=1.0 / 128, scalar2=None, op0=ALU.mult), ["g_sum"], ["g_mean"])
            DVE(lambda e: e.tensor_tensor(out=gst[:, 24:32], in0=gst[:, 16:24], in1=gst[:, 16:24], op=ALU.mult), ["g_mean"], ["g_msq"])
            DVE(lambda e: e.scalar_tensor_tensor(out=gst[:, 32:40], in0=gst[:, 8:16], scalar=1.0 / 128, in1=gst[:, 24:32], op0=ALU.mult, op1=ALU.subtract),
                [("g_sq", h) for h in range(8)] + ["g_msq"], ["g_var"])
            ACT(lambda e: e.activation(out=gst[:, 40:48], in_=gst[:, 32:40], func=AF.Sqrt, bias=EPS, scale=1.0), ["g_var"], ["g_std"])
            DVE(lambda e: e.reciprocal(out=gst[:, 48:56], in_=gst[:, 40:48]), ["g_std"], ["g_rstd"])
            DVE(lambda e: e.scalar_tensor_tensor(out=gst[:, 56:64], in0=gst[:, 16:24], scalar=-1.0, in1=gst[:, 48:56], op0=ALU.mult, op1=ALU.mult),
                ["g_mean", "g_rstd"], ["g_nmr"])
            for h in range(8):
                DVE(lambda e, h=h: e.tensor_scalar(out=osq[:, h * 128:(h + 1) * 128], in0=osq[:, h * 128:(h + 1) * 128],
                                                   scalar1=gst[:, 48 + h:49 + h], scalar2=gst[:, 56 + h:57 + h], op0=ALU.mult, op1=ALU.add),
                    [("osq", h // 4), "g_rstd", "g_nmr"] + [("g_sq", hh_) for hh_ in range(8)] + ["g_sum"], [("on", h)])
            yield
            POOL(lambda e, bi=bi: e.tensor_tensor(out=zb, in0=osq, in1=sgt2[bi], op=ALU.mult), [("on", hh_) for hh_ in range(8)] + [("sgt", bi)], ["zb"])
            transpose8(zb, "zb", zT4[:, :, t * 128:(t + 1) * 128], ("zT4", t))
            state_update(bi)

        def glevelB(g):
            xnr = [("xnT4b", g % 2, t) for t in range(4)]
            zr = [("zT4", t) for t in range(4)]
            for oc in range(8):
                yb = next_proj(projB)
                for k in range(8):
                    mm(bank(yb), Wro[:, k, oc * 128:(oc + 1) * 128], zT4[:, k, :], k == 0, k == 7, zr + wres("Wro", 8), [PR(yb)])
                b = next_proj(projB)
                for k in range(8):
                    mm(bank(b), WinB[:, k, 3072 + oc * 128:3072 + (oc + 1) * 128], xnT4b2[g % 2][:, k, :], k == 0, k == 7, xnr + wres("WinBr", 8), [PR(b)])
                sgb = sgr2[oc % 2]
                ACT(lambda e, b=b, sgb=sgb: e.activation(out=sgb, in_=bank(b), func=AF.Sigmoid), [PR(b)], [("sgr", oc % 2)])
                DVE(lambda e, yb=yb, sgb=sgb: e.tensor_tensor(out=tmpm, in0=bank(yb), in1=sgb, op=ALU.mult), [PR(yb), ("sgr", oc % 2)], ["tmpm"])
                POOL(lambda e, oc=oc, g=g: e.tensor_tensor(out=mergedT[:, oc, g * 512:(g + 1) * 512], in0=tmpm, in1=mergedT[:, oc, g * 512:(g + 1) * 512], op=ALU.add),
                     ["tmpm", ("mT", oc, g)], [("mT", oc, g)])


        def interleave(*gens):
            alive = [g_ for g_ in gens if g_ is not None]
            while alive:
                for g_ in list(alive):
                    try:
                        next(g_)
                    except StopIteration:
                        alive.remove(g_)

        def step(gen_):
            if gen_ is None:
                return
            try:
                next(gen_)
            except StopIteration:
                pass

        def drain(gen_):
            if gen_ is None:
                return
            for _ in gen_:
                pass

        orderB = [(g, t) for g in range(4) for t in range(4)]
        RB(*orderB[0])
        RBt(*orderB[0])
        RB(*orderB[1])
        RBt(*orderB[1])
        drain(PB(*orderB[0]))
        for i_, (g, t) in enumerate(orderB):
            if i_ + 2 < len(orderB):
                RB(*orderB[i_ + 2])
            tg = tailB(g, t)
            pg = PB(*orderB[i_ + 1]) if i_ + 1 < len(orderB) else None
            step(tg)
            step(pg)
            step(tg)
            step(tg)
            step(tg)
            step(pg)
            drain(pg)
            drain(tg)
            if i_ + 2 < len(orderB):
                RBt(*orderB[i_ + 2])
            if t == 3:
                glevelB(g)

        S.barrier()
        A.release(persist_mark)

        if DBG:
            dump_m()
            A.release(persist_mark)
        if stop == "B":
            raise _Stop()

        h = A.alloc([128, NT, D], F32)
        e_mark = A.mark()
        KT = A.alloc([128, 8, 256], BF16)
        Vm = A.alloc([128, 2, D], BF16)
        x_mark = A.mark()
        Wkv = A.alloc([128, 8, 2048], BF16)
        gbc_K = A.alloc([128, D], F32)
        memt = A.alloc([128, 2, D], F32)
        xs_K = A.alloc([128, D], BF16)
        junk_K = A.alloc([128, D], BF16)
        mnT = A.alloc([128, 8, 256], BF16)
        load_w(Wkv, w_xa_kv, 8, "Wkv", "Wkv")
        DMA("sp", gbc_K, g_mem.partition_broadcast(128), [], ["gbc"], "gbc")
        DMA("sp", memt, memc.rearrange("(c p) d -> p c d", p=128), [], ["memt"], "memt")
        for mc in range(2):
            rmsnorm(memt[:, mc, :], "memt", gbc_K, xs_K, junk_K)
            transpose8(xs_K, "xs", mnT[:, :, mc * 128:(mc + 1) * 128], ("mnT", mc))
        mnr = [("mnT", 0), ("mnT", 1)]
        for c in range(8):
            b = 1 + (c % 2)
            for k in range(8):
                mm(bank(b)[:, 0:256], Wkv[:, k, c * 128:(c + 1) * 128], mnT[:, k, :], k == 0, k == 7, mnr + wres("Wkv", 8), [PR(b)])
            ACT(lambda e, b=b, c=c: e.copy(out=KT[:, c, :], in_=bank(b)[:, 0:256]), [PR(b)], ["KT"])
        for mc in range(2):
            for hf in range(2):
                b = 3 + ((mc * 2 + hf) % 2)
                for k in range(8):
                    mm(bank(b), mnT[:, k, mc * 128:(mc + 1) * 128], Wkv[:, k, 1024 + hf * 512:1024 + (hf + 1) * 512], k == 0, k == 7, mnr + wres("Wkv", 8), [PR(b)])
                DVE(lambda e, b=b, mc=mc, hf=hf: e.tensor_copy(out=Vm[:, mc, hf * 512:(hf + 1) * 512], in_=bank(b)), [PR(b)], ["Vm"])
        S.barrier()
        A.release(x_mark)

        Wmix = A.alloc([128, 8, D], BF16)
        Wq = A.alloc([128, 8, D], BF16)
        Wo = A.alloc([128, 8, D], BF16)
        Wr = A.alloc([128, 8, 36], BF16)
        gbc_xa = A.alloc([128, D], F32)
        gbc_moe = A.alloc([128, D], F32)
        brt = A.alloc([128, 36], F32)
        ebase2 = A.alloc([128, 2 * NE], F32)
        ebase = ebase2[:, 0:NE]
        ebaseY = ebase2[:, NE:2 * NE]
        carry = A.alloc([128, NE], F32)
        xt2_X = [A.alloc([128, D], F32) for _ in range(2)]
        xs_X = A.alloc([128, D], BF16)
        junk_X = A.alloc([128, D], BF16)
        xn2T = A.alloc([128, 8, 256], BF16)
        qT2 = A.alloc([128, 8, 256], BF16)
        Pb = A.alloc([128, 4, 256], BF16)
        PTx = A.alloc([128, 8, 128], BF16)
        obx = A.alloc([128, 4, 256], BF16)
        oTx = A.alloc([128, 8, 128], BF16)
        xn3 = [A.alloc([128, D], BF16) for _ in range(2)]
        xn3T = A.alloc([128, 8, 128], BF16)
        rs = A.alloc([128, 256], F32)
        Mb = A.alloc([128, NE], BF16)

        load_w(Wmix, w_mix_out, 8, "Wmix", "Wmix")
        load_w(Wq, w_xa_q, 8, "Wq", "Wq")
        load_w(Wo, w_xa_o, 8, "Wo", "Wo")
        load_w(Wr, w_rt, 8, "Wr", "Wr")
        DMA("sp", gbc_xa, g_xa.partition_broadcast(128), [], ["gbc_xa"], "gbcx1")
        DMA("sp", gbc_moe, g_moe.partition_broadcast(128), [], ["gbc_moe"], "gbcx2")
        DMA("sp", brt, b_rt.partition_broadcast(128), [], ["brt"], "gbcx3")
        DMA("sp", ebase2, c_eb, [], ["ebase"], "gbcx4")
        POOL(lambda e: e.memset(carry, 0.0), [], ["carry"])


        def rmsnorm2(xt_ap, xt_res, g_bc, g_res, xs_ap, xs_res, rt=False):
            o_ = 4 if rt else 0
            sfx = "_r" if rt else ""
            ssq_, std_, rstd_ = stat[:, o_:o_ + 1], stat[:, o_ + 1:o_ + 2], stat[:, o_ + 2:o_ + 3]
            jk = junk_X
            ACT(lambda e: e.activation(out=jk, in_=xt_ap, func=AF.Square, accum_out=ssq_), [xt_res], ["junk", "ssq" + sfx])
            ACT(lambda e: e.activation(out=std_, in_=ssq_, func=AF.Sqrt, bias=EPS, scale=1.0 / D), ["ssq" + sfx], ["std" + sfx])
            DVE(lambda e: e.reciprocal(out=rstd_, in_=std_), ["std" + sfx], ["rstd" + sfx])
            DVE(lambda e: e.scalar_tensor_tensor(out=xs_ap, in0=xt_ap, scalar=rstd_, in1=g_bc, op0=ALU.mult, op1=ALU.mult),
                [xt_res, "rstd" + sfx, g_res], [xs_res])

        LG = rs[:, 0:36]
        GMAX = rs[:, 36:37]
        NGM = rs[:, 37:38]
        GE = rs[:, 40:44]
        GSUM = rs[:, 44:45]
        PG = rs[:, 45:46]
        GM = rs[:, 48:52]
        PEN = rs[:, 52:56]
        ELM = rs[:, 64:96]
        M1V = rs[:, 96:97]
        M2V = rs[:, 97:98]
        DD = rs[:, 98:99]
        S2 = rs[:, 99:100]
        W1 = rs[:, 100:101]
        W2 = rs[:, 101:102]
        I1 = rs[:, 102:103]
        I2 = rs[:, 103:104]
        V1 = rs[:, 104:105]
        V2 = rs[:, 105:106]
        J1 = rs[:, 106:107]
        J2 = rs[:, 107:108]
        M1 = rs[:, 128:160]
        M2 = rs[:, 160:192]
        ELM2 = rs[:, 192:224]
        POSB = rs[:, 224:256]
        TMPR = A.alloc([128, NE], F32)
        POSR = A.alloc([128, NE], F32)
        POSY = A.alloc([128, NE], F32)

        def router(tile, bi):
            hres = ("h", tile)
            rmsnorm2(h[:, tile, :], hres, gbc_moe, "gbc_moe", xn3[bi], ("xn3", bi), rt=True)
            yield
            transpose8(xn3[bi], ("xn3", bi), xn3T, "xn3T")
            yield
            for k in range(8):
                mm(bank(7)[:, 0:36], xn3T[:, k, :], Wr[:, k, :], k == 0, k == 7, ["xn3T"] + wres("Wr", 8), [PR(7)])
            R = "rt"
            DVE(lambda e: e.tensor_tensor(out=LG, in0=bank(7)[:, 0:36], in1=brt, op=ALU.add), [PR(7), "brt"], [R])
            DVE(lambda e: e.reduce_max(out=GMAX, in_=LG[:, 0:4], axis=AX.X), [R], [R])
            DVE(lambda e: e.tensor_scalar(out=NGM, in0=GMAX, scalar1=-1.0, scalar2=None, op0=ALU.mult), [R], [R])
            ACT(lambda e: e.activation(out=GE, in_=LG[:, 0:4], func=AF.Exp, bias=NGM, scale=1.0, accum_out=GSUM), [R], [R])
            DVE(lambda e: e.reciprocal(out=PG, in_=GSUM), [R], [R])
            DVE(lambda e: e.tensor_scalar(out=GM, in0=LG[:, 0:4], scalar1=GMAX, scalar2=None, op0=ALU.is_equal), [R], [R])
            DVE(lambda e: e.tensor_scalar(out=PEN, in0=GM, scalar1=-1.0, scalar2=1e30, op0=ALU.add, op1=ALU.mult), [R], [R])
            DVE(lambda e: e.tensor_tensor(out=ELM.rearrange("p (g j) -> p g j", g=4), in0=LG[:, 4:36].rearrange("p (g j) -> p g j", g=4),
                                          in1=PEN.unsqueeze(2).broadcast_to([128, 4, 8]), op=ALU.add), [R], [R])
            yield
            DVE(lambda e: e.reduce_max(out=M1V, in_=ELM, axis=AX.X), [R], [R])
            DVE(lambda e: e.tensor_scalar(out=M1, in0=ELM, scalar1=M1V, scalar2=None, op0=ALU.is_equal), [R], [R])
            DVE(lambda e: e.scalar_tensor_tensor(out=ELM2, in0=M1, scalar=-1e30, in1=ELM, op0=ALU.mult, op1=ALU.add), [R], [R])
            DVE(lambda e: e.reduce_max(out=M2V, in_=ELM2, axis=AX.X), [R], [R])
            DVE(lambda e: e.tensor_scalar(out=M2, in0=ELM2, scalar1=M2V, scalar2=None, op0=ALU.is_equal), [R], [R])
            yield
            DVE(lambda e: e.tensor_tensor(out=DD, in0=M2V, in1=M1V, op=ALU.subtract), [R], [R])
            ACT(lambda e: e.activation(out=S2, in_=DD, func=AF.Sigmoid), [R], [R])
            DVE(lambda e: e.tensor_tensor(out=W2, in0=PG, in1=S2, op=ALU.mult), [R], [R])
            DVE(lambda e: e.tensor_tensor(out=W1, in0=PG, in1=W2, op=ALU.subtract), [R], [R])
            DVE(lambda e: e.tensor_tensor(out=Mb, in0=M1, in1=M2, op=ALU.add), [R], ["Mb"])
            mm(bank(7)[:, 64:96], ustrict, Mb, True, True, ["Mb", "ident"], [PR(7)])
            mm(bank(7)[:, 96:128], onesb, Mb, True, True, ["Mb", "ident"], [PR(7)])
            DVE(lambda e: e.tensor_tensor(out=POSR, in0=bank(7)[:, 64:96], in1=carry, op=ALU.add), [PR(7), "carry"], [R])
            DVE(lambda e: e.tensor_tensor(out=carry, in0=bank(7)[:, 96:128], in1=carry, op=ALU.add), [PR(7), "carry"], ["carry"])
            yield
            DVE(lambda e: e.scalar_tensor_tensor(out=POSB, in0=POSR, scalar=float(CAP), in1=ebase, op0=ALU.min, op1=ALU.add), [R, "ebase"], [R])
            DVE(lambda e: e.scalar_tensor_tensor(out=POSY, in0=POSR, scalar=float(CAP - 1), in1=ebaseY, op0=ALU.min, op1=ALU.add), [R, "ebase"], [R])
            for (Mx, Ix, Jx, Vx, Wx, col) in ((M1, I1, J1, V1, W1, 0), (M2, I2, J2, V2, W2, 1)):
                yield
                DVE(lambda e, Mx=Mx: e.tensor_tensor(out=TMPR, in0=Mx, in1=POSB, op=ALU.mult), [R], [R])
                DVE(lambda e, Ix=Ix: e.reduce_sum(out=Ix, in_=TMPR, axis=AX.X), [R], [R])
                DVE(lambda e, Mx=Mx: e.tensor_tensor(out=TMPR, in0=Mx, in1=POSR, op=ALU.mult), [R], [R])
                DVE(lambda e, Vx=Vx: e.reduce_sum(out=Vx, in_=TMPR, axis=AX.X), [R], [R])
                DVE(lambda e, Ix=Ix, col=col: e.tensor_copy(out=sidx[:, tile, col:col + 1], in_=Ix), [R], [("sidx", tile)])
                DVE(lambda e, Mx=Mx: e.tensor_tensor(out=TMPR, in0=Mx, in1=POSY, op=ALU.mult), [R], [R])
                DVE(lambda e, Jx=Jx: e.reduce_sum(out=Jx, in_=TMPR, axis=AX.X), [R], [R])
                DVE(lambda e, Jx=Jx, col=col: e.tensor_copy(out=gidx[:, tile, col:col + 1], in_=Jx), [R], [("gidx", tile)])
                DVE(lambda e, Vx=Vx, Wx=Wx, col=col: e.scalar_tensor_tensor(out=wts[:, tile, col:col + 1], in0=Vx, scalar=float(CAP), in1=Wx, op0=ALU.is_lt, op1=ALU.mult),
                    [R], [("wts", tile)])
            for col in range(2):
                S.add("pool", lambda e, col=col, bi=bi: e.indirect_dma_start(
                    out=XG, out_offset=bass.IndirectOffsetOnAxis(ap=sidx[:, tile, col:col + 1], axis=0), in_=xn3[bi], in_offset=None,
                    bounds_check=NE * CAPR - 1, oob_is_err=False),
                    [("xn3", bi), ("sidx", tile)], [("XG", tile, col)], dma=True, key="xgs%d" % bi, bsize=2)

        projX = [1, 2]
        xn2T2 = [xn2T, A.alloc([128, 8, 256], BF16)]
        qT22 = [qT2, A.alloc([128, 8, 256], BF16)]

        def interleaveX(*gens):
            alive = [g_ for g_ in gens if g_ is not None]
            while alive:
                for g_ in list(alive):
                    try:
                        next(g_)
                    except StopIteration:
                        alive.remove(g_)

        def S1_X(g):
            gp = g % 2
            for tl in range(2):
                tile = g * 2 + tl
                bi = tile % 2
                xb = xt2_X[bi]
                xr = ("xt", bi)
                DMA("sp", xb, xc[tile * 128:(tile + 1) * 128, :], [], [xr], "xt%d" % bi)
                for hf in range(2):
                    b = projX[hf]
                    for k in range(8):
                        mm(bank(b), mergedT[:, k, tile * 128:(tile + 1) * 128], Wmix[:, k, hf * 512:(hf + 1) * 512], k == 0, k == 7,
                           [("mT", k, tile // 4)] + wres("Wmix", 8), [PR(b)])
                    DVE(lambda e, b=b, hf=hf, tile=tile, xb=xb: e.tensor_tensor(out=h[:, tile, hf * 512:(hf + 1) * 512], in0=bank(b), in1=xb[:, hf * 512:(hf + 1) * 512], op=ALU.add),
                        [PR(b), xr], [("h", tile)])
                yield
                rmsnorm2(h[:, tile, :], ("h", tile), gbc_xa, "gbc_xa", xs_X, "xs")
                transpose8(xs_X, "xs", xn2T2[gp][:, :, tl * 128:(tl + 1) * 128], ("xn2T", gp, tl))
                yield

        def QT_X(g):
            gp = g % 2
            for c in range(8):
                b = projX[c % 2]
                for k in range(8):
                    mm(bank(b)[:, 0:256], Wq[:, k, c * 128:(c + 1) * 128], xn2T2[gp][:, k, :], k == 0, k == 7,
                       [("xn2T", gp, 0), ("xn2T", gp, 1)] + wres("Wq", 8), [PR(b)])
                ACT(lambda e, b=b, c=c, gp=gp: e.copy(out=qT22[gp][:, c, :], in_=bank(b)[:, 0:256]), [PR(b)], [("qT2", gp, c)])
                if c % 2 == 1:
                    yield

        def XA_X(tile):
            g, tl = tile // 2, tile % 2
            gp = g % 2
            sc = bank(3, 2).rearrange("p (h m) -> p h m", h=4)
            for hh in range(4):
                for kc in range(2):
                    mm(ps_t[:, 3 * 512 + hh * 256: 3 * 512 + (hh + 1) * 256], qT22[gp][:, 2 * hh + kc, tl * 128:(tl + 1) * 128], KT[:, 2 * hh + kc, :], kc == 0, kc == 1,
                       [("qT2", gp, 2 * hh + kc), "KT"], [PR(3 + hh // 2)])
            DVE(lambda e, sc=sc: e.reduce_max(out=stat[:, 8:12], in_=sc, axis=AX.X), [PR(3), PR(4)], ["xmx"])
            DVE(lambda e: e.tensor_scalar(out=stat[:, 12:16], in0=stat[:, 8:12], scalar1=-1.0 / 16, scalar2=None, op0=ALU.mult), ["xmx"], ["xnb"])
            for hh in range(4):
                ACT(lambda e, hh=hh: e.activation(out=Pb[:, hh, :], in_=ps_t[:, 3 * 512 + hh * 256: 3 * 512 + (hh + 1) * 256], func=AF.Exp,
                                                  bias=stat[:, 12 + hh:13 + hh], scale=1.0 / 16, accum_out=stat[:, 16 + hh:17 + hh]),
                    [PR(3 + hh // 2), "xnb"], [("Pb", hh), ("xsum", hh)])
            DVE(lambda e: e.reciprocal(out=stat[:, 20:24], in_=stat[:, 16:20]), [("xsum", hh) for hh in range(4)], ["xrs"])
            yield
            pb = bank_bf(0)
            Pv = Pb.rearrange("p h m -> p (h m)")
            for j in range(8):
                tp(pb[:, j, :], Pv[:, j * 128:(j + 1) * 128], [("Pb", j // 2)], [PR(0)])
            ACT(lambda e: e.copy(out=PTx, in_=bank_bf(0)), [PR(0)], ["PTx"])
            yield
            for hh in range(4):
                for mc in range(2):
                    mm(ps_t[:, 5 * 512 + hh * 256: 5 * 512 + (hh + 1) * 256], PTx[:, 2 * hh + mc, :], Vm[:, mc, hh * 256:(hh + 1) * 256], mc == 0, mc == 1,
                       ["PTx", "Vm"], [PR(5 + hh // 2)])
            DVE(lambda e: e.tensor_tensor(out=obx, in0=bank(5, 2).rearrange("p (h m) -> p h m", h=4),
                                          in1=stat[:, 20:24].unsqueeze(2).broadcast_to([128, 4, 256]), op=ALU.mult),
                [PR(5), PR(6), "xrs"], ["obx"])
            yield
            transpose8(obx.rearrange("p h m -> p (h m)"), "obx", oTx, "oTx")
            yield
            for hf in range(2):
                b = projX[hf]
                for k in range(8):
                    mm(bank(b), oTx[:, k, :], Wo[:, k, hf * 512:(hf + 1) * 512], k == 0, k == 7, ["oTx"] + wres("Wo", 8), [PR(b)])
                DVE(lambda e, b=b, hf=hf, tile=tile: e.tensor_tensor(out=h[:, tile, hf * 512:(hf + 1) * 512], in0=bank(b), in1=h[:, tile, hf * 512:(hf + 1) * 512], op=ALU.add),
                    [PR(b), ("h", tile)], [("h", tile)])
            yield

        interleaveX(S1_X(0))
        interleaveX(QT_X(0))
        pending = None
        for g in range(8):
            interleaveX(XA_X(2 * g), pending)
            interleaveX(XA_X(2 * g + 1), router(2 * g, 0), S1_X(g + 1) if g + 1 < 8 else None)
            if g + 1 < 8:
                interleaveX(QT_X(g + 1))
            pending = router(2 * g + 1, 1)
        interleaveX(pending)

        S.barrier()
        A.release(e_mark)
        A.limit = ARENA_BYTES

        if DBG:
            for tile in range(NT):
                DMA("sp", dbg_h[:, tile * D:(tile + 1) * D], h[:, tile, :], [("h", tile)], [("dbgh", tile)], "dbgh")
            S.barrier()
        if stop in ("X", "XNR", "X1"):
            raise _Stop()

        NR = 4
        Wg_r = [A.alloc([128, 8, 512], BF16) for _ in range(NR)]
        Wu_r = [A.alloc([128, 8, 512], BF16) for _ in range(NR)]
        Wd_r = [A.alloc([128, 4, D], BF16) for _ in range(NR)]
        xg4 = [A.alloc([128, 2, D], BF16) for _ in range(4)]
        xgT2 = [A.alloc([128, 8, 256], BF16) for _ in range(2)]
        hid2 = [A.alloc([128, 4, 256], BF16) for _ in range(2)]
        sge2 = [A.alloc([128, 256], F32) for _ in range(2)]
        ysb2 = [A.alloc([128, 2, D], BF16) for _ in range(2)]

        allxg = [("XG", tile, col) for tile in range(NT) for col in range(2)]
        gb_i = [0]
        def TG_E(ex):
            s = ex % NR
            bi = ex % 2
            DMA("pool", Wg_r[s], w_gate[ex].rearrange("(k p) n -> p k n", p=128), [], [("Wg", s, k) for k in range(8)], "wg%d" % s)
            DMA("pool", Wu_r[s], w_up[ex].rearrange("(k p) n -> p k n", p=128), [], [("Wu", s, k) for k in range(8)], "wu%d" % s)
            DMA("pool", Wd_r[s], w_down[ex].rearrange("(k p) n -> p k n", p=128), [], [("Wd", s, k) for k in range(4)], "wd%d" % s)
            for rb in range(2):
                pb = bank_bf(0)
                for k in range(8):
                    tp(pb[:, k, :], xg4[ex % 4][:, rb, k * 128:(k + 1) * 128], [("xg", ex % 4)], [PR(0)])
                if rb == 0:
                    ACT(lambda e, bi=bi, rb=rb: e.copy(out=xgT2[bi][:, :, rb * 128:(rb + 1) * 128], in_=bank_bf(0)), [PR(0)], [("xgT", bi, rb)])
                else:
                    DVE(lambda e, bi=bi, rb=rb: e.tensor_copy(out=xgT2[bi][:, :, rb * 128:(rb + 1) * 128], in_=bank_bf(0)), [PR(0)], [("xgT", bi, rb)])
            xgr = [("xgT", bi, 0), ("xgT", bi, 1)]
            for hc in range(4):
                gbk = 1 + (gb_i[0] % 2)
                ubk = 3 + (gb_i[0] % 2)
                gb_i[0] += 1
                for k in range(8):
                    mm(bank(gbk)[:, 0:256], Wg_r[s][:, k, hc * 128:(hc + 1) * 128], xgT2[bi][:, k, :], k == 0, k == 7, xgr + [("Wg", s, kk) for kk in range(8)], [PR(gbk)])
                for k in range(8):
                    mm(bank(ubk)[:, 0:256], Wu_r[s][:, k, hc * 128:(hc + 1) * 128], xgT2[bi][:, k, :], k == 0, k == 7, xgr + [("Wu", s, kk) for kk in range(8)], [PR(ubk)])
                sgb = sge2[hc % 2]
                ACT(lambda e, gbk=gbk, sgb=sgb: e.activation(out=sgb, in_=bank(gbk)[:, 0:256], func=AF.Sigmoid), [PR(gbk)], [("sge", hc % 2)])
                DVE(lambda e, gbk=gbk, sgb=sgb: e.tensor_tensor(out=sgb, in0=bank(gbk)[:, 0:256], in1=sgb, op=ALU.mult),
                    [PR(gbk), ("sge", hc % 2)], [("sge", hc % 2)])
                DVE(lambda e, ubk=ubk, sgb=sgb, bi=bi, hc=hc: e.tensor_tensor(out=hid2[bi][:, hc, :], in0=bank(ubk)[:, 0:256], in1=sgb, op=ALU.mult),
                    [PR(ubk), ("sge", hc % 2)], [("hid", bi, hc)])

        def DN_E(ex):
            s = ex % NR
            bi = ex % 2
            hr = [("hid", bi, hc) for hc in range(4)]
            for rb in range(2):
                for hf in range(2):
                    yb = 5 + ((rb * 2 + hf) % 3)
                    for hc in range(4):
                        mm(bank(yb), hid2[bi][:, hc, rb * 128:(rb + 1) * 128], Wd_r[s][:, hc, hf * 512:(hf + 1) * 512], hc == 0, hc == 3,
                           hr + [("Wd", s, kk) for kk in range(4)], [PR(yb)])
                    if hf == 0:
                        ACT(lambda e, yb=yb, bi=bi, rb=rb, hf=hf: e.copy(out=ysb2[bi][:, rb, hf * 512:(hf + 1) * 512], in_=bank(yb)), [PR(yb)], [("ysb", bi, rb, hf)])
                    else:
                        DVE(lambda e, yb=yb, bi=bi, rb=rb, hf=hf: e.tensor_copy(out=ysb2[bi][:, rb, hf * 512:(hf + 1) * 512], in_=bank(yb)), [PR(yb)], [("ysb", bi, rb, hf)])
            DMA("sp", YG[ex * CAP:(ex + 1) * CAP, :].rearrange("(b p) d -> p b d", p=128), ysb2[bi],
                [("ysb", bi, rb, hf) for rb in range(2) for hf in range(2)], [("YG", ex)], "yg%d" % bi)


        def XL_E(ex):
            DMA("sp", xg4[ex % 4], XG[ex * CAPR:ex * CAPR + CAP, :].rearrange("(b p) d -> p b d", p=128), allxg if ex < 4 else [], [("xg", ex % 4)], "xg%d" % (ex % 4))

        for ex in range(4):
            XL_E(ex)
        TG_E(0)
        for ex in range(NE):
            if ex + 4 < NE:
                XL_E(ex + 4)
            if ex + 1 < NE:
                TG_E(ex + 1)
            DN_E(ex)

        S.barrier()
        A.release(e_mark)

        gbc_C = A.alloc([128, D], F32)
        y12 = [[A.alloc([128, D], BF16) for _ in range(2)] for _ in range(2)]
        ot2 = [A.alloc([128, D], F32) for _ in range(2)]
        junk_C = A.alloc([128, D], BF16)
        DMA("sp", gbc_C, g_fin.partition_broadcast(128), [], ["gbc"], "gbc")
        outres = []

        def c_s1(tile):
            bi = tile % 2
            o_ = 24 + 3 * bi
            for col in range(2):
                S.add("pool", lambda e, col=col, bi=bi, tile=tile: e.indirect_dma_start(
                    out=y12[bi][col], out_offset=None, in_=YG, in_offset=bass.IndirectOffsetOnAxis(ap=gidx[:, tile, col:col + 1], axis=0)),
                    [("gidx", tile)], [("y12", bi, col)], dma=True, key="yga%d%d" % (bi, col))
            for col in range(2):
                DVE(lambda e, col=col, bi=bi, tile=tile: e.scalar_tensor_tensor(out=h[:, tile, :], in0=y12[bi][col], scalar=wts[:, tile, col:col + 1], in1=h[:, tile, :],
                                                                             op0=ALU.mult, op1=ALU.add),
                    [("y12", bi, col), ("wts", tile), ("h", tile)], [("h", tile)])
            ACT(lambda e, tile=tile, o_=o_: e.activation(out=junk_C, in_=h[:, tile, :], func=AF.Square, accum_out=stat[:, o_:o_ + 1]), [("h", tile)], ["junk", ("ssqC", bi)])
            ACT(lambda e, o_=o_: e.activation(out=stat[:, o_ + 1:o_ + 2], in_=stat[:, o_:o_ + 1], func=AF.Sqrt, bias=EPS, scale=1.0 / D), [("ssqC", bi)], [("stdC", bi)])

        def c_s2(tile):
            bi = tile % 2
            o_ = 24 + 3 * bi
            DVE(lambda e, o_=o_: e.reciprocal(out=stat[:, o_ + 2:o_ + 3], in_=stat[:, o_ + 1:o_ + 2]), [("stdC", bi)], [("rstdC", bi)])
            DVE(lambda e, tile=tile, bi=bi, o_=o_: e.scalar_tensor_tensor(out=ot2[bi], in0=h[:, tile, :], scalar=stat[:, o_ + 2:o_ + 3], in1=gbc_C, op0=ALU.mult, op1=ALU.mult),
                [("h", tile), ("rstdC", bi), "gbc"], [("ot", bi)])
            DMA("sp", out[tile * 128:(tile + 1) * 128, :], ot2[bi], [("ot", bi)], [("out", tile)], "out%d" % bi)
            outres.append(("out", tile))

        c_s1(0)
        for tile in range(NT):
            if tile + 1 < NT:
                c_s1(tile + 1)
            c_s2(tile)
        S.add("sp", None, outres)
        S.barrier()


    try:
        phases()
    except _Stop:
        S.barrier()

    S.resolve()
    sems = {}
    for e in ("pe", "act", "dve", "pool"):
        sems[("eng", e)] = es.enter_context(nc.semaphore("s_" + e))
    for k in S.keys:
        sems[("dma", k)] = es.enter_context(nc.semaphore("d_" + str(k)))
    with nc.Block() as block:
        block.sync(lambda e: S.run_engine("sp", e, sems))
        block.scalar(lambda e: S.run_engine("act", e, sems))
        block.vector(lambda e: S.run_engine("dve", e, sems))
        block.gpsimd(lambda e: S.run_engine("pool", e, sems))
        block.tensor(lambda e: S.run_engine("pe", e, sems))
    es.close()
    return nc, S, A


def _consts(half):
    p = np.arange(128, dtype=np.float64)
    inv_freq = 10000.0 ** (-np.arange(0, 64, 2, dtype=np.float64) / 64)

    def cs_tab(base):
        pos = base + np.arange(NT)[None, :] * 128 + p[:, None]
        ang = (pos[:, :, None].astype(np.float32) * inv_freq[None, None, :].astype(np.float32)).astype(np.float32)
        return np.stack([np.cos(ang), np.sin(ang)], axis=1).astype(np.float32)

    gam = 1.0 - 2.0 ** (-5.0 - np.arange(8, dtype=np.float64))
    lg = np.log(gam)
    gq = np.zeros((128, 4, 128), np.float32)
    gk = np.zeros((128, 4, 128), np.float32)
    ct = np.zeros((128, 4, 128), np.float32)
    i = np.arange(128, dtype=np.float64)
    for c in range(4):
        for hl in range(2):
            h = 2 * c + hl
            gq[hl * 64:(hl + 1) * 64, c, :] = np.exp((i + 1) * lg[h])[None, :]
            gk[hl * 64:(hl + 1) * 64, c, :] = (np.exp(-(i + 1) * lg[h]) / 8.0)[None, :]
            ct[hl * 64:(hl + 1) * 64, c, :] = np.exp(128 * lg[h])
    zt = (np.exp((127 - p)[:, None] * lg[None, :]) / 8.0).astype(np.float32)
    mask = (np.arange(128)[None, :] >= np.arange(128)[:, None]).astype(np.float32)
    ident = np.eye(128, dtype=np.float32)
    ustrict = (np.arange(128)[:, None] < np.arange(128)[None, :]).astype(np.float32)
    ones = np.ones((128, 128), np.float32)
    c_bf = np.concatenate([ident, ustrict, ones], axis=1)
    eb = np.concatenate([np.tile((np.arange(NE, dtype=np.float32) * CAPR)[None, :], (128, 1)),
                         np.tile((np.arange(NE, dtype=np.float32) * CAP)[None, :], (128, 1))], axis=1)
    return {
        "c_bf": c_bf, "c_cs_own": cs_tab(half * TOK), "c_cs_pre": cs_tab(0.0),
        "c_gq": gq, "c_gk": gk, "c_zt": zt, "c_ct": ct, "c_mask": np.ascontiguousarray(np.tile(mask[:, None, :], (1, 4, 1))), "c_eb": eb,
    }


_CACHE = {}


def kernel(x, mem, mix_norm_g, w_in, conv_w, w_conv_out, w_ret_out, w_mix_out,
           xa_norm_g, mem_norm_g, w_xa_q, w_xa_kv, w_xa_o, moe_norm_g,
           w_group, b_group, w_router, b_router, w_gate, w_up, w_down, final_norm_g):
    f = lambda a: np.ascontiguousarray(np.asarray(a, dtype=np.float32))
    x = f(x)
    mem = f(mem)
    if "nc" not in _CACHE:
        _CACHE["nc"] = build_program()
    nc = _CACHE["nc"][0]
    shared = {
        "w_in": f(w_in)[0], "conv_wT": np.ascontiguousarray(f(conv_w)[0].T), "w_conv_out": f(w_conv_out)[0],
        "w_ret_out": f(w_ret_out)[0], "w_mix_out": f(w_mix_out)[0], "w_xa_q": f(w_xa_q)[0], "w_xa_kv": f(w_xa_kv)[0],
        "w_xa_o": f(w_xa_o)[0], "g_mix": f(mix_norm_g)[0], "g_xa": f(xa_norm_g)[0], "g_mem": f(mem_norm_g)[0],
        "g_moe": f(moe_norm_g)[0], "g_fin": f(final_norm_g),
        "w_rt": np.ascontiguousarray(np.concatenate([f(w_group)[0], f(w_router)[0]], axis=1)),
        "b_rt": np.ascontiguousarray(np.concatenate([f(b_group)[0], f(b_router)[0]], axis=0)),
        "w_gate": f(w_gate)[0], "w_up": f(w_up)[0], "w_down": f(w_down)[0],
    }
    zeros = np.zeros((TOK, D), np.float32)
    in_maps = []
    for c in range(8):
        b, half = c // 2, c % 2
        m = dict(shared)
        m["xc"] = np.ascontiguousarray(x[b, half * TOK:(half + 1) * TOK])
        m["xp"] = np.ascontiguousarray(x[b, 0:TOK]) if half == 1 else zeros
        m["memc"] = np.ascontiguousarray(mem[b])
        m.update(_consts(half))
        in_maps.append(m)
    res = run_bass_kernel_spmd(nc, in_maps, core_ids=list(range(8)))
    _CACHE["res"] = res
    outp = np.empty((4, 2 * TOK, D), np.float32)
    for c in range(8):
        b, half = c // 2, c % 2
        outp[b, half * TOK:(half + 1) * TOK] = res.results[c]["out"]
    return outp
```

```python
import math
import numpy as np
from contextlib import ExitStack
import concourse.bass as bass
import concourse.mybir as mybir
from concourse.bass_utils import run_bass_kernel_spmd

F32 = mybir.dt.float32
BF16 = mybir.dt.bfloat16
I32 = mybir.dt.int32
U8 = mybir.dt.uint8
AF = mybir.ActivationFunctionType
ALU = mybir.AluOpType
AX = mybir.AxisListType

D = 1024
NT = 16
TOK = 2048
NE = 32
CAP = 256
CAPR = CAP + 1
EPS = 1e-6
INW = 6656
DBG = False


class Op:
    __slots__ = ("eng", "fn", "reads", "writes", "dma", "key", "idx", "sig", "ev", "deps", "xdeps", "bsize")

    def __init__(self, eng, fn, reads, writes, dma, key):
        self.eng = eng
        self.fn = fn
        self.reads = tuple(reads)
        self.writes = tuple(writes)
        self.dma = dma
        self.key = key
        self.sig = False
        self.ev = None
        self.deps = ()
        self.xdeps = ()
        self.bsize = 1


class Sched:
    ENGS = ("pe", "act", "dve", "pool", "sp")

    def __init__(self):
        self.ops = []
        self.last_eng = {}
        self.last_key = {}

    def add(self, eng, fn, reads=(), writes=(), dma=False, key=None, bsize=1):
        if dma:
            assert key is not None
        op = Op(eng, fn, reads, writes, dma, key)
        op.bsize = bsize
        op.idx = len(self.ops)
        self.ops.append(op)
        if dma:
            self.last_key[key] = op.idx
        elif fn is not None:
            self.last_eng[eng] = op.idx
        return op

    def barrier(self):
        deps = tuple(self.last_eng.values()) + tuple(self.last_key.values())
        for e in self.ENGS:
            op = self.add(e, None)
            op.xdeps = deps

    def resolve(self):
        last_w = {}
        readers = {}
        for op in self.ops:
            deps = set(op.xdeps)
            for r in op.reads:
                w = last_w.get(r)
                if w is not None:
                    deps.add(w)
            for w_ in op.writes:
                w = last_w.get(w_)
                if w is not None:
                    deps.add(w)
                for rd in readers.get(w_, {}).values():
                    deps.add(rd)
            deps.discard(op.idx)
            dl = []
            for d in sorted(deps):
                dop = self.ops[d]
                if dop.fn is None:
                    continue
                if op.eng == "pe" and dop.eng == "pe" and not dop.dma and not op.dma:
                    continue
                dop.sig = True
                dl.append(d)
            op.deps = tuple(dl)
            rk = ("dma", op.idx) if op.dma else op.eng
            for r in op.reads:
                readers.setdefault(r, {})[rk] = op.idx
            for w_ in op.writes:
                last_w[w_] = op.idx
                readers[w_] = {}
        cnt = {e: 0 for e in self.ENGS}
        keycnt = {}
        import os
        if os.environ.get("ALLSIG"):
            for op in self.ops:
                if not op.dma and op.fn is not None and op.eng != "sp":
                    op.sig = True
        for op in self.ops:
            if op.dma:
                keycnt[op.key] = keycnt.get(op.key, 0) + 16
                q = 16 * op.bsize
                op.ev = (("dma", op.key), (keycnt[op.key] + q - 1) // q * q)
            elif op.sig:
                cnt[op.eng] += 1
                op.ev = (("eng", op.eng), cnt[op.eng])
        self.keys = list(keycnt.keys())
        self.cnt = cnt
        return self

    def run_engine(self, eng, eobj, sems):
        waited = {}
        for op in self.ops:
            if op.eng != eng:
                continue
            need = {}
            for d in op.deps:
                sk, val = self.ops[d].ev
                if need.get(sk, 0) < val:
                    need[sk] = val
            for sk, val in need.items():
                if waited.get(sk, 0) >= val:
                    continue
                eobj.wait_ge(sems[sk], val)
                waited[sk] = val
            if op.fn is None:
                continue
            ins = op.fn(eobj)
            if op.dma:
                ins.then_inc(sems[op.ev[0]], 16)
            elif op.sig:
                ins.then_inc(sems[op.ev[0]], 1)


_DTSZ = {F32: 4, BF16: 2, I32: 4, U8: 1}


class Arena:
    def __init__(self, ap, size):
        self.ap = ap
        self.size = size
        self.off = 0
        self.peak = 0
        self.limit = size

    def mark(self):
        return self.off

    def release(self, m):
        self.off = m

    def alloc(self, shape, dt):
        n = 1
        for s in shape[1:]:
            n *= s
        nbytes = n * _DTSZ[dt]
        off = (self.off + 31) // 32 * 32
        assert off + nbytes <= self.limit, ("SBUF arena overflow", off, nbytes, self.limit)
        self.off = off + nbytes
        self.peak = max(self.peak, self.off)
        v = self.ap[:, off:off + nbytes].bitcast(dt)
        if len(shape) == 3:
            v = v.rearrange("p (a b) -> p a b", a=shape[1])
        elif len(shape) == 4:
            v = v.rearrange("p (a b c) -> p a b c", a=shape[1], b=shape[2])
        return v


def build_program(stop=None):
    nc = bass.Bass("TRN2", target_bir_lowering=False)

    def din(name, shape, dt=F32):
        return nc.dram_tensor(name, list(shape), dt, kind="ExternalInput").ap()

    xc = din("xc", [TOK, D])
    xp = din("xp", [TOK, D])
    memc = din("memc", [256, D])
    w_in = din("w_in", [D, INW])
    conv_wT = din("conv_wT", [512, 3])
    w_conv_out = din("w_conv_out", [512, D])
    w_ret_out = din("w_ret_out", [D, D])
    w_mix_out = din("w_mix_out", [D, D])
    w_xa_q = din("w_xa_q", [D, D])
    w_xa_kv = din("w_xa_kv", [D, 2 * D])
    w_xa_o = din("w_xa_o", [D, D])
    g_mix = din("g_mix", [D])
    g_xa = din("g_xa", [D])
    g_mem = din("g_mem", [D])
    g_moe = din("g_moe", [D])
    g_fin = din("g_fin", [D])
    w_rt = din("w_rt", [D, 36])
    b_rt = din("b_rt", [36])
    w_gate = din("w_gate", [NE, D, 512])
    w_up = din("w_up", [NE, D, 512])
    w_down = din("w_down", [NE, 512, D])
    c_bf = din("c_bf", [128, 384])
    c_cs_own = din("c_cs_own", [128, 2, NT, 32])
    c_cs_pre = din("c_cs_pre", [128, 2, NT, 32])
    c_gq = din("c_gq", [128, 4, 128])
    c_gk = din("c_gk", [128, 4, 128])
    c_zt = din("c_zt", [128, 8])
    c_ct = din("c_ct", [128, 4, 128])
    c_mask = din("c_mask", [128, 4, 128])
    c_eb = din("c_eb", [128, 2 * NE])
    out = nc.dram_tensor("out", [TOK, D], F32, kind="ExternalOutput").ap()
    XG = nc.dram_tensor("xg_scr", [NE * CAPR, D], BF16, kind="Internal").ap()
    YG = nc.dram_tensor("yg_scr", [NE * CAP, D], BF16, kind="Internal").ap()
    if DBG:
        dbg_m = nc.dram_tensor("dbg_m", [128, 8 * TOK], F32, kind="ExternalOutput").ap()
        dbg_h = nc.dram_tensor("dbg_h", [128, NT * D], F32, kind="ExternalOutput").ap()

    S = Sched()
    es = ExitStack()
    ARENA_BYTES = 207 * 1024
    arena_t = es.enter_context(nc.sbuf_tensor("arena", [128, ARENA_BYTES], U8))
    A = Arena(arena_t, ARENA_BYTES)
    ps_t = es.enter_context(nc.psum_tensor("ps", [128, 4096], F32))

    def bank(i, n=1):
        return ps_t[:, i * 512:(i + n) * 512]

    def bank_bf(i):
        return ps_t[:, i * 512:(i + 1) * 512].bitcast(BF16).rearrange("p (a b) -> p a b", a=8)

    def PR(i):
        return ("ps", i)

    uid = [0]

    def ukey(p):
        uid[0] += 1
        return "%s%d" % (p, uid[0])

    def PE(fn, r, w):
        return S.add("pe", fn, r, w)

    def ACT(fn, r, w):
        return S.add("act", fn, r, w)

    def DVE(fn, r, w):
        return S.add("dve", fn, r, w)

    def POOL(fn, r, w):
        return S.add("pool", fn, r, w)

    def DMA(eng, out_, in_, r, w, key, nb=1):
        return S.add(eng, lambda e: e.dma_start(out=out_, in_=in_), r, w, dma=True, key=key, bsize=nb)

    def mm(out_, lhsT, rhs, start, stop, r, w):
        return PE(lambda e: e.matmul(out_, lhsT=lhsT, rhs=rhs, start=start, stop=stop), r, w)

    def tp(out_, in_, r, w):
        return PE(lambda e: e.transpose(out=out_, in_=in_, identity=ident), r + ["ident"], w)

    def load_w(dst, src, rows_k, res, key):
        for k in range(rows_k):
            DMA("pool", dst[:, k, :], src[k * 128:(k + 1) * 128, :], [], [(res, k)], key, nb=rows_k)

    def wres(res, n):
        return [(res, k) for k in range(n)]

    ident3 = A.alloc([128, 3, 128], BF16)
    ident = ident3[:, 0, :]
    ustrict = ident3[:, 1, :]
    onesb = ident3[:, 2, :]
    DMA("pool", ident3, c_bf.rearrange("p (a b) -> p a b", a=3), [], ["ident"], "c_bf")
    MT_BYTES = 8 * TOK * 2
    mergedT = arena_t[:, ARENA_BYTES - MT_BYTES:ARENA_BYTES].bitcast(BF16).rearrange("p (a b) -> p a b", a=8)
    A.limit = ARENA_BYTES - MT_BYTES
    stat = A.alloc([128, 64], F32)
    wts = A.alloc([128, NT, 2], F32)
    sidx = A.alloc([128, NT, 2], I32)
    gidx = A.alloc([128, NT, 2], I32)
    ssq = stat[:, 0:1]
    std = stat[:, 1:2]
    rstd = stat[:, 2:3]
    persist_mark = A.mark()

    def rmsnorm(xt_ap, xt_res, g_bc, xs_ap, junk_ap, xs_res="xs"):
        ACT(lambda e: e.activation(out=junk_ap, in_=xt_ap, func=AF.Square, accum_out=ssq), [xt_res], ["junk", "ssq"])
        ACT(lambda e: e.activation(out=std, in_=ssq, func=AF.Sqrt, bias=EPS, scale=1.0 / D), ["ssq"], ["std"])
        DVE(lambda e: e.reciprocal(out=rstd, in_=std), ["std"], ["rstd"])
        DVE(lambda e: e.scalar_tensor_tensor(out=xs_ap, in0=xt_ap, scalar=rstd, in1=g_bc, op0=ALU.mult, op1=ALU.mult),
            [xt_res, "rstd", "gbc"], [xs_res])

    def transpose8(src_ap, src_res, dst_ap, dst_res, nblk=8, copy_eng="act"):
        pb = bank_bf(0)
        for k in range(nblk):
            tp(pb[:, k, :], src_ap[:, k * 128:(k + 1) * 128], [src_res], [PR(0)])
        if copy_eng == "act":
            ACT(lambda e: e.copy(out=dst_ap, in_=pb[:, 0:nblk, :]), [PR(0)], [dst_res])
        else:
            DVE(lambda e: e.tensor_copy(out=dst_ap, in_=pb[:, 0:nblk, :]), [PR(0)], [dst_res])

    def dump_m():
        dtmp = A.alloc([128, 8, 512], F32)
        for g in range(4):
            DVE(lambda e, g=g: e.tensor_copy(out=dtmp, in_=mergedT[:, :, g * 512:(g + 1) * 512]), [("mT", oc, g) for oc in range(8)] + ["dbgo"], ["dtmp"])
            DMA("sp", dbg_m.rearrange("p (k t) -> p k t", k=8)[:, :, g * 512:(g + 1) * 512], dtmp, ["dtmp"], ["dbgo"], "dbgo")
        S.barrier()

    class _Stop(Exception):
        pass

    def phases():
        WinA = A.alloc([128, 8, 2560], BF16)
        Wco = A.alloc([128, 4, 1024], BF16)
        gbc_A = A.alloc([128, D], F32)
        convw = A.alloc([128, 4, 3], F32)
        xt2_A = [A.alloc([128, D], F32) for _ in range(2)]
        xs_A = A.alloc([128, D], BF16)
        junk_A = A.alloc([128, D], BF16)
        xnT4 = [A.alloc([128, 8, 512], BF16) for _ in range(2)]
        xin_sb = A.alloc([128, 4, 512], F32)
        u = A.alloc([128, 4, 514], F32)
        cc = A.alloc([128, 4, 512], F32)
        bc = A.alloc([128, 4, 512], BF16)
        sg2 = [A.alloc([128, 512], F32) for _ in range(2)]

        zt_ = A.alloc([128, 4112], BF16)
        POOL(lambda e: e.memset(zt_, 0.0), [], ["zfill"])
        XGf = XG.rearrange("r d -> (r d)").rearrange("(p n) -> p n", p=128)
        DMA("sp", gbc_A, g_mix.partition_broadcast(128), [], ["gbc"], "gbc")
        DMA("sp", convw, conv_wT.rearrange("(c p) k -> p c k", p=128), [], ["convw"], "convw")
        for k in range(8):
            DMA("pool", WinA[:, k, 0:1536], w_in[k * 128:(k + 1) * 128, 0:1536], [], [("WinA", k)], "WinA", nb=8)
        for k in range(4):
            DMA("pool", Wco[:, k, :], w_conv_out[k * 128:(k + 1) * 128, :], [], [("Wco", k)], "Wco", nb=4)
        for k in range(8):
            DMA("pool", WinA[:, k, 1536:2560], w_in[k * 128:(k + 1) * 128, 4608:5632], [], [("WinAg", k)], "WinAg", nb=8)
        DMA("sp", xt2_A[1], xp[TOK - 128:TOK, :], [], [("xt", 1)], "xt1")
        rmsnorm(xt2_A[1], ("xt", 1), gbc_A, xs_A, junk_A, xs_res=("xsA", 0))
        transpose8(xs_A, ("xsA", 0), xnT4[1][:, :, 0:128], ("xnT4", 1, 0))
        for c in range(4):
            for k in range(8):
                mm(bank(1)[:, 0:128], WinA[:, k, c * 128:(c + 1) * 128], xnT4[1][:, k, 0:128], k == 0, k == 7, [("xnT4", 1, 0)] + wres("WinA", 8), [PR(1)])
            ACT(lambda e, c=c: e.copy(out=xin_sb[:, c, 0:128], in_=bank(1)[:, 0:128]), [PR(1)], [("xin", c)])
            for k in range(8):
                mm(bank(2)[:, 0:128], WinA[:, k, 1024 + c * 128:1024 + (c + 1) * 128], xnT4[1][:, k, 0:128], k == 0, k == 7, [("xnT4", 1, 0)] + wres("WinA", 8), [PR(2)])
            DVE(lambda e, c=c: e.tensor_tensor(out=u[:, c, 0:2], in0=bank(2)[:, 126:128], in1=xin_sb[:, c, 126:128], op=ALU.mult),
                [PR(2), ("xin", c)], ["u"])

        projA = [1, 2, 3, 6, 7]
        pa_i = [0]

        def next_proj(pool):
            b = pool[pa_i[0] % len(pool)]
            pa_i[0] += 1
            return b

        xs4 = [xs_A] + [A.alloc([128, D], BF16) for _ in range(3)]
        sg8 = list(sg2) + [A.alloc([128, 512], F32) for _ in range(6)]

        def hnA(g, t):
            tile = g * 4 + t
            xb = xt2_A[tile % 2]
            xr = ("xt", tile % 2)
            DMA("sp", xb, xc[tile * 128:(tile + 1) * 128, :], [], [xr], "xt%d" % (tile % 2))
            ACT(lambda e: e.activation(out=junk_A, in_=xb, func=AF.Square, accum_out=ssq), [xr], ["junk", "ssq"])
            ACT(lambda e: e.activation(out=std, in_=ssq, func=AF.Sqrt, bias=EPS, scale=1.0 / D), ["ssq"], ["std"])
            DVE(lambda e: e.reciprocal(out=rstd, in_=std), ["std"], ["rstd"])
            DVE(lambda e: e.scalar_tensor_tensor(out=xs4[t], in0=xb, scalar=rstd, in1=gbc_A, op0=ALU.mult, op1=ALU.mult),
                [xr, "rstd", "gbc"], [("xsA", t)])

        def htA(g, t):
            transpose8(xs4[t], ("xsA", t), xnT4[g % 2][:, :, t * 128:(t + 1) * 128], ("xnT4", g % 2, t))

        def xinA(g):
            gb = g % 2
            xnr = [("xnT4", gb, t) for t in range(4)]
            for c in range(4):
                b = next_proj(projA)
                for k in range(8):
                    mm(bank(b), WinA[:, k, c * 128:(c + 1) * 128], xnT4[gb][:, k, :], k == 0, k == 7, xnr + wres("WinA", 8), [PR(b)])
                ACT(lambda e, b=b, c=c: e.copy(out=xin_sb[:, c, :], in_=bank(b)), [PR(b)], [("xin", c)])

        def cgA(g):
            gb = g % 2
            xnr = [("xnT4", gb, t) for t in range(4)]
            for c in range(4):
                b = next_proj(projA)
                for k in range(8):
                    mm(bank(b), WinA[:, k, 1024 + c * 128:1024 + (c + 1) * 128], xnT4[gb][:, k, :], k == 0, k == 7, xnr + wres("WinA", 8), [PR(b)])
                DVE(lambda e, b=b, c=c: e.tensor_tensor(out=u[:, c, 2:514], in0=bank(b), in1=xin_sb[:, c, :], op=ALU.mult),
                    [PR(b), ("xin", c)], ["u"])
                POOL(lambda e, c=c: e.tensor_scalar(out=cc[:, c, :], in0=u[:, c, 0:512], scalar1=convw[:, c, 0:1], scalar2=None, op0=ALU.mult),
                     ["u", "convw"], [("cc", c)])
                DVE(lambda e, c=c: e.scalar_tensor_tensor(out=cc[:, c, :], in0=u[:, c, 1:513], scalar=convw[:, c, 1:2], in1=cc[:, c, :], op0=ALU.mult, op1=ALU.add),
                    ["u", "convw", ("cc", c)], [("cc", c)])
                DVE(lambda e, c=c: e.scalar_tensor_tensor(out=cc[:, c, :], in0=u[:, c, 2:514], scalar=convw[:, c, 2:3], in1=cc[:, c, :], op0=ALU.mult, op1=ALU.add),
                    ["u", "convw", ("cc", c)], [("cc", c)])
            POOL(lambda e: e.tensor_copy(out=u[:, :, 0:2], in_=u[:, :, 512:514]), ["u"], ["u"])

        def bgA(g):
            gb = g % 2
            xnr = [("xnT4", gb, t) for t in range(4)]
            for c in range(4):
                b = next_proj(projA)
                for k in range(8):
                    mm(bank(b), WinA[:, k, 512 + c * 128:512 + (c + 1) * 128], xnT4[gb][:, k, :], k == 0, k == 7, xnr + wres("WinA", 8), [PR(b)])
                DVE(lambda e, b=b, c=c: e.tensor_tensor(out=bc[:, c, :], in0=bank(b), in1=cc[:, c, :], op=ALU.mult),
                    [PR(b), ("cc", c)], [("bc", c)])

        def gateA(g, ocs):
            gb = g % 2
            xnr = [("xnT4", gb, t) for t in range(4)]
            for oc in ocs:
                b = next_proj(projA)
                for k in range(8):
                    mm(bank(b), WinA[:, k, 1536 + oc * 128:1536 + (oc + 1) * 128], xnT4[gb][:, k, :], k == 0, k == 7, xnr + wres("WinAg", 8), [PR(b)])
                ACT(lambda e, b=b, oc=oc: e.activation(out=sg8[oc], in_=bank(b), func=AF.Sigmoid), [PR(b)], [("sg", oc)])

        def yconvA(g, ocs):
            for oc in ocs:
                yb = 4 + (oc % 2)
                for k in range(4):
                    mm(bank(yb), Wco[:, k, oc * 128:(oc + 1) * 128], bc[:, k, :], k == 0, k == 3, [("bc", kk) for kk in range(4)] + wres("Wco", 4), [PR(yb)])
                DVE(lambda e, yb=yb, oc=oc, g=g: e.tensor_tensor(out=mergedT[:, oc, g * 512:(g + 1) * 512], in0=bank(yb), in1=sg8[oc], op=ALU.mult),
                    [PR(yb), ("sg", oc)], [("mT", oc, g)])

        for t in range(4):
            hnA(0, t)
            htA(0, t)
        for g in range(4):
            nx = g + 1 if g + 1 < 4 else None
            if nx is not None:
                hnA(nx, 0)
            xinA(g)
            if nx is not None:
                htA(nx, 0)
                hnA(nx, 1)
            cgA(g)
            if nx is not None:
                htA(nx, 1)
                hnA(nx, 2)
            gateA(g, range(0, 4))
            bgA(g)
            if nx is not None:
                htA(nx, 2)
                hnA(nx, 3)
            gateA(g, range(4, 8))
            if nx is not None:
                htA(nx, 3)
            yconvA(g, range(8))
            if g == 0:
                for i in range(16):
                    DMA("sp", XGf[:, i * 4112:(i + 1) * 4112], zt_, ["zfill"], [("XGz", i)], "xgz", nb=16)

        S.barrier()
        A.release(persist_mark)
        if stop == "A":
            dump_m()
            raise _Stop()

        WinB = A.alloc([128, 8, 4096], BF16)
        Wro = A.alloc([128, 8, 1024], BF16)
        gbc_B = A.alloc([128, D], F32)
        cs = A.alloc([128, 2, NT, 32], F32)
        gq = A.alloc([128, 4, 128], F32)
        gk = A.alloc([128, 4, 128], F32)
        zt = A.alloc([128, 8], F32)
        ct = A.alloc([128, 4, 128], F32)
        maskT = A.alloc([128, 4, 128], F32)
        Sst = A.alloc([128, 4, 128], F32)
        Sb = A.alloc([128, 4, 128], BF16)
        xt2_B = [A.alloc([128, D], F32) for _ in range(2)]
        xs_B = A.alloc([128, D], BF16)
        junk_B = A.alloc([128, D], BF16)
        xnT4b2 = [A.alloc([128, 8, 512], BF16) for _ in range(2)]
        xs_B2 = [xs_B, A.alloc([128, D], BF16)]
        qr2 = [A.alloc([128, 8, 2, 32], BF16) for _ in range(2)]
        kr2 = [A.alloc([128, 8, 2, 32], BF16) for _ in range(2)]
        kz2 = [A.alloc([128, 8, 64], BF16) for _ in range(2)]
        v2 = [A.alloc([128, D], BF16) for _ in range(2)]
        sgt2 = [A.alloc([128, D], BF16) for _ in range(2)]
        rt = [A.alloc([128, 8, 32], F32) for _ in range(4)]
        qTz = A.alloc([128, 4, 2, 128], BF16)
        kT = A.alloc([128, 4, 128], BF16)
        PT = A.alloc([128, 8, 128], BF16)
        osq = A.alloc([128, D], F32)
        zb = A.alloc([128, D], BF16)
        zT4 = A.alloc([128, 8, 512], BF16)
        sgr2 = [A.alloc([128, 512], F32) for _ in range(2)]
        tmpm = A.alloc([128, 512], F32)
        gst = A.alloc([128, 64], F32)

        DMA("sp", gbc_B, g_mix.partition_broadcast(128), [], ["gbc"], "gbc")
        DMA("sp", cs, c_cs_pre, [], ["cs"], "cs")
        DMA("sp", gq, c_gq, [], ["gq"], "c_gq")
        DMA("sp", gk, c_gk, [], ["gk"], "c_gk")
        DMA("sp", zt, c_zt, [], ["zt"], "c_zt")
        DMA("sp", ct, c_ct, [], ["ct"], "c_ct")
        DMA("sp", maskT, c_mask, [], ["maskT"], "c_mask")
        for k in range(8):
            DMA("pool", WinB[:, k, 512:2048], w_in[k * 128:(k + 1) * 128, 2048:3584], [], [("WinBkv", k)], "WinBkv", nb=8)
        for k in range(8):
            DMA("pool", WinB[:, k, 0:512], w_in[k * 128:(k + 1) * 128, 1536:2048], [], [("WinBq", k)], "WinBq", nb=8)
        for k in range(8):
            DMA("pool", WinB[:, k, 2048:3072], w_in[k * 128:(k + 1) * 128, 3584:4608], [], [("WinBg", k)], "WinBg", nb=8)
        for k in range(8):
            DMA("pool", WinB[:, k, 3072:4096], w_in[k * 128:(k + 1) * 128, 5632:6656], [], [("WinBr", k)], "WinBr", nb=8)
        load_w(Wro, w_ret_out, 8, "Wro", "Wro")
        POOL(lambda e: e.memset(Sst, 0.0), [], ["Sst"])
        POOL(lambda e: e.memset(Sb, 0.0), [], ["Sb"])
        POOL(lambda e: e.memset(qTz, 0.0), [], ["qT"])

        projB = [1, 2, 7]
        pa_i[0] = 0

        def rotary(pb_ap, pres, tile, dst, dres, ti):
            pv = pb_ap.rearrange("p (h t f) -> p h t f", h=8, t=2)
            cosb = cs[:, 0, tile, :].unsqueeze(1).broadcast_to([128, 8, 32])
            sinb = cs[:, 1, tile, :].unsqueeze(1).broadcast_to([128, 8, 32])
            ta, tb, tc, td = rt
            DVE(lambda e: e.tensor_tensor(out=ta, in0=pv[:, :, 0, :], in1=cosb, op=ALU.mult), [pres, "cs"], ["rta"])
            DVE(lambda e: e.tensor_tensor(out=tb, in0=pv[:, :, 1, :], in1=sinb, op=ALU.mult), [pres, "cs"], ["rtb"])
            DVE(lambda e: e.tensor_tensor(out=tc, in0=pv[:, :, 0, :], in1=sinb, op=ALU.mult), [pres, "cs"], ["rtc"])
            DVE(lambda e: e.tensor_tensor(out=td, in0=pv[:, :, 1, :], in1=cosb, op=ALU.mult), [pres, "cs"], ["rtd"])
            POOL(lambda e: e.tensor_tensor(out=dst[:, :, 0, :], in0=ta, in1=tb, op=ALU.subtract), ["rta", "rtb"], [(dres, 0)])
            POOL(lambda e: e.tensor_tensor(out=dst[:, :, 1, :], in0=tc, in1=td, op=ALU.add), ["rtc", "rtd"], [(dres, 1)])

        def state_update(bi):
            kzv = kz2[bi].rearrange("p h d -> p (h d)")
            for c in range(4):
                ob = ps_t[:, 3 * 512 + c * 256: 3 * 512 + (c + 1) * 256]
                mm(ob, kzv[:, c * 128:(c + 1) * 128], v2[bi][:, c * 256:(c + 1) * 256], True, True,
                   [("kz", bi), ("v", bi)], [PR(3), PR(4)])
            pS = bank(3, 2).rearrange("p (c n) -> p c n", c=4)
            POOL(lambda e: e.tensor_tensor(out=Sst, in0=Sst, in1=ct, op=ALU.mult), ["Sst", "ct"], ["Sst"])
            DVE(lambda e: e.tensor_tensor(out=Sst[0:64], in0=Sst[0:64], in1=pS[0:64, :, 0:128], op=ALU.add), ["Sst", PR(3), PR(4)], ["Sst"])
            DVE(lambda e: e.tensor_tensor(out=Sst[64:128], in0=Sst[64:128], in1=pS[64:128, :, 128:256], op=ALU.add), ["Sst", PR(3), PR(4)], ["Sst"])
            ACT(lambda e: e.copy(out=Sb, in_=Sst), ["Sst"], ["Sb"])

        def proj_tm(xnT_ap, xn_res, col0, wres_, b):
            for k in range(8):
                mm(bank(b), xnT_ap[:, k, :], WinB[:, k, col0:col0 + 512], k == 0, k == 7, xn_res + wres_, [PR(b)])

        def RPp(tile):
            bi = tile % 2
            xb = xt2_B[bi]
            xr = ("xt", bi)
            DMA("sp", xb, xp[tile * 128:(tile + 1) * 128, :], [], [xr], "xt%d" % bi)
            rmsnorm(xb, xr, gbc_B, xs_B2[bi], junk_B, xs_res=("xsB", bi))

        def RPt(tile):
            bi = tile % 2
            transpose8(xs_B2[bi], ("xsB", bi), xnT4b2[1][:, :, (tile % 4) * 128:(tile % 4 + 1) * 128], ("xnT4b", 1, tile % 4))

        def PPp(tile):
            bi = tile % 2
            xnTp_ = xnT4b2[1][:, :, (tile % 4) * 128:(tile % 4 + 1) * 128]
            xres_ = [("xnT4b", 1, tile % 4)]
            b = next_proj(projB)
            proj_tm(xnTp_, xres_, 512, wres("WinBkv", 8), b)
            rotary(bank(b), PR(b), tile, kr2[bi], ("kr", bi), 1)
            POOL(lambda e, bi=bi: e.tensor_tensor(out=kz2[bi], in0=kr2[bi].rearrange("p h t f -> p h (t f)"),
                                                  in1=zt.unsqueeze(2).broadcast_to([128, 8, 64]), op=ALU.mult),
                 [(("kr", bi), 0), (("kr", bi), 1), "zt"], [("kz", bi)])
            for hf in range(2):
                b = next_proj(projB)
                proj_tm(xnTp_, xres_, 1024 + hf * 512, wres("WinBkv", 8), b)
                ACT(lambda e, b=b, bi=bi, hf=hf: e.copy(out=v2[bi][:, hf * 512:(hf + 1) * 512], in_=bank(b)), [PR(b)], [("v", bi)])
            state_update(bi)

        RPp(0)
        RPt(0)
        RPp(1)
        RPt(1)
        for tile in range(NT):
            if tile + 2 < NT:
                RPp(tile + 2)
            PPp(tile)
            if tile + 2 < NT:
                RPt(tile + 2)

        if stop == "B1":
            raise _Stop()

        def chk(n):
            if stop == "B2:%d" % n:
                raise _Stop()
        DMA("sp", cs, c_cs_own, [], ["cs"], "cs")

        def RB(g, t):
            tile = g * 4 + t
            bi = tile % 2
            xb = xt2_B[bi]
            xr = ("xt", bi)
            DMA("sp", xb, xc[tile * 128:(tile + 1) * 128, :], [], [xr], "xt%d" % bi)
            rmsnorm(xb, xr, gbc_B, xs_B2[bi], junk_B, xs_res=("xsB", bi))

        def RBt(g, t):
            tile = g * 4 + t
            bi = tile % 2
            xnT_t = xnT4b2[g % 2][:, :, t * 128:(t + 1) * 128]
            transpose8(xs_B2[bi], ("xsB", bi), xnT_t, ("xnT4b", g % 2, t))

        def PB(g, t):
            tile = g * 4 + t
            bi = tile % 2
            xnT_t = xnT4b2[g % 2][:, :, t * 128:(t + 1) * 128]
            xnres = [("xnT4b", g % 2, t)]
            b = next_proj(projB)
            proj_tm(xnT_t, xnres, 0, wres("WinBq", 8), b)
            rotary(bank(b), PR(b), tile, qr2[bi], ("qr", bi), 0)
            b = next_proj(projB)
            proj_tm(xnT_t, xnres, 512, wres("WinBkv", 8), b)
            rotary(bank(b), PR(b), tile, kr2[bi], ("kr", bi), 1)
            POOL(lambda e, bi=bi: e.tensor_tensor(out=kz2[bi], in0=kr2[bi].rearrange("p h t f -> p h (t f)"),
                                                  in1=zt.unsqueeze(2).broadcast_to([128, 8, 64]), op=ALU.mult),
                 [(("kr", bi), 0), (("kr", bi), 1), "zt"], [("kz", bi)])
            yield
            for hf in range(2):
                b = next_proj(projB)
                proj_tm(xnT_t, xnres, 1024 + hf * 512, wres("WinBkv", 8), b)
                ACT(lambda e, b=b, bi=bi, hf=hf: e.copy(out=v2[bi][:, hf * 512:(hf + 1) * 512], in_=bank(b)), [PR(b)], [("v", bi)])
            yield
            for hf in range(2):
                b = next_proj(projB)
                proj_tm(xnT_t, xnres, 2048 + hf * 512, wres("WinBg", 8), b)
                ACT(lambda e, b=b, hf=hf: e.activation(out=sgr2[hf], in_=bank(b), func=AF.Sigmoid), [PR(b)], [("sgr", hf)])
                DVE(lambda e, b=b, bi=bi, hf=hf: e.tensor_tensor(out=sgt2[bi][:, hf * 512:(hf + 1) * 512], in0=bank(b), in1=sgr2[hf], op=ALU.mult),
                    [PR(b), ("sgr", hf)], [("sgt", bi)])

        def tailB(g, t):
            tile = g * 4 + t
            bi = tile % 2
            pb = bank_bf(0)
            qrv = qr2[bi].rearrange("p h t f -> p (h t f)")
            krv = kr2[bi].rearrange("p h t f -> p (h t f)")
            for c in range(4):
                tp(pb[:, c, :], qrv[:, c * 128:(c + 1) * 128], [(("qr", bi), 0), (("qr", bi), 1)], [PR(0)])
            for c in range(4):
                tp(pb[:, 4 + c, :], krv[:, c * 128:(c + 1) * 128], [(("kr", bi), 0), (("kr", bi), 1)], [PR(0)])
            DVE(lambda e: e.tensor_tensor(out=qTz[0:64, :, 0, :], in0=bank_bf(0)[0:64, 0:4, :], in1=gq[0:64], op=ALU.mult), [PR(0), "gq"], ["qT"])
            DVE(lambda e: e.tensor_tensor(out=qTz[64:128, :, 1, :], in0=bank_bf(0)[64:128, 0:4, :], in1=gq[64:128], op=ALU.mult), [PR(0), "gq", "qT"], ["qT"])
            DVE(lambda e: e.tensor_tensor(out=kT, in0=bank_bf(0)[:, 4:8, :], in1=gk, op=ALU.mult), [PR(0), "gk"], ["kT"])
            yield
            for c in range(4):
                mm(ps_t[:, 3 * 512 + c * 256: 3 * 512 + (c + 1) * 256], kT[:, c, :], qTz[:, c, :, :].rearrange("p a q -> p (a q)"), True, True,
                   ["qT", "kT"], [PR(3 + c // 2)])
            mb = maskT
            DVE(lambda e, mb=mb: e.tensor_tensor(out=PT[:, 0:4, :], in0=bank(3).rearrange("p (h q) -> p h q", h=4), in1=mb, op=ALU.mult),
                [PR(3), "maskT"], [("PT", 0)])
            DVE(lambda e, mb=mb: e.tensor_tensor(out=PT[:, 4:8, :], in0=bank(4).rearrange("p (h q) -> p h q", h=4), in1=mb, op=ALU.mult),
                [PR(4), "maskT"], [("PT", 1)])
            yield
            for h in range(8):
                p0 = (h % 2) * 64
                ob = ps_t[:, 5 * 512 + h * 128: 5 * 512 + (h + 1) * 128]
                mm(ob, PT[:, h, :], v2[bi][:, h * 128:(h + 1) * 128], True, False, [("PT", h // 4), ("v", bi)], [PR(5 + h // 4)])
                mm(ob, qTz[:, h // 2, h % 2, :], Sb[:, h // 2, :], False, True, ["qT", "Sb"], [PR(5 + h // 4)])
            yield
            for hb in range(2):
                ACT(lambda e, hb=hb: e.copy(out=osq[:, hb * 512:(hb + 1) * 512], in_=bank(5 + hb)), [PR(5 + hb)],
                    [("osq", hb)] + [("on", hh_) for hh_ in range(hb * 4, hb * 4 + 4)])
            DVE(lambda e: e.reduce_sum(out=gst[:, 0:8], in_=osq.rearrange("p (h e) -> p h e", h=8), axis=AX.X), [("osq", 0), ("osq", 1)], ["g_sum"])
            for h in range(8):
                ACT(lambda e, h=h: e.activation(out=junk_B[:, h * 128:(h + 1) * 128], in_=osq[:, h * 128:(h + 1) * 128], func=AF.Square,
                                                accum_out=gst[:, 8 + h:9 + h]),
                    [("osq", h // 4)], ["junk", ("g_sq", h)])
            DVE(lambda e: e.tensor_scalar(out=gst[:, 16:24], in0=gst[:, 0:8], scalar1=1.0 / 128, scalar2=None, op0=ALU.mult), ["g_sum"], ["g_mean"])
            DVE(lambda e: e.tensor_tensor(out=gst[:, 24:32], in0=gst[:, 16:24], in1=gst[:, 16:24], op=ALU.mult), ["g_mean"], ["g_msq"])
            DVE(lambda e: e.scalar_tensor_tensor(out=gst[:, 32:40], in0=gst[:, 8:16], scalar=1.0 / 128, in1=gst[:, 24:32], op0=ALU.mult, op1=ALU.subtract),
                [("g_sq", h) for h in range(8)] + ["g_msq"], ["g_var"])
            ACT(lambda e: e.activation(out=gst[:, 40:48], in_=gst[:, 32:40], func=AF.Sqrt, bias=EPS, scale=1.0), ["g_var"], ["g_std"])
            DVE(lambda e: e.reciprocal(out=gst[:, 48:56], in_=gst[:, 40:48]), ["g_std"], ["g_rstd"])
            DVE(lambda e: e.scalar_tensor_tensor(out=gst[:, 56:64], in0=gst[:, 16:24], scalar=-1.0, in1=gst[:, 48:56], op0=ALU.mult, op1=ALU.mult),
                ["g_mean", "g_rstd"], ["g_nmr"])
            for h in range(8):
                DVE(lambda e, h=h: e.tensor_scalar(out=osq[:, h * 128:(h + 1) * 128], in0=osq[:, h * 128:(h + 1) * 128],
                                                   scalar1=gst[:, 48 + h:49 + h], scalar2=gst[:, 56 + h:57 + h], op0=ALU.mult, op1=ALU.add),
                    [("osq", h // 4), "g_rstd", "g_nmr"] + [("g_sq", hh_) for hh_ in range(8)] + ["g_sum"], [("on", h)])
            yield
            POOL(lambda e, bi=bi: e.tensor_tensor(out=zb, in0=osq, in1=sgt2[bi], op=ALU.mult), [("on", hh_) for hh_ in range(8)] + [("sgt", bi)], ["zb"])
            transpose8(zb, "zb", zT4[:, :, t * 128:(t + 1) * 128], ("zT4", t))
            state_update(bi)

        def glevelB(g):
            xnr = [("xnT4b", g % 2, t) for t in range(4)]
            zr = [("zT4", t) for t in range(4)]
            for oc in range(8):
                yb = next_proj(projB)
                for k in range(8):
                    mm(bank(yb), Wro[:, k, oc * 128:(oc + 1) * 128], zT4[:, k, :], k == 0, k == 7, zr + wres("Wro", 8), [PR(yb)])
                b = next_proj(projB)
                for k in range(8):
                    mm(bank(b), WinB[:, k, 3072 + oc * 128:3072 + (oc + 1) * 128], xnT4b2[g % 2][:, k, :], k == 0, k == 7, xnr + wres("WinBr", 8), [PR(b)])
                sgb = sgr2[oc % 2]
                ACT(lambda e, b=b, sgb=sgb: e.activation(out=sgb, in_=bank(b), func=AF.Sigmoid), [PR(b)], [("sgr", oc % 2)])
                DVE(lambda e, yb=yb, sgb=sgb: e.tensor_tensor(out=tmpm, in0=bank(yb), in1=sgb, op=ALU.mult), [PR(yb), ("sgr", oc % 2)], ["tmpm"])
                POOL(lambda e, oc=oc, g=g: e.tensor_tensor(out=mergedT[:, oc, g * 512:(g + 1) * 512], in0=tmpm, in1=mergedT[:, oc, g * 512:(g + 1) * 512], op=ALU.add),
                     ["tmpm", ("mT", oc, g)], [("mT", oc, g)])


        def interleave(*gens):
            alive = [g_ for g_ in gens if g_ is not None]
            while alive:
                for g_ in list(alive):
                    try:
                        next(g_)
                    except StopIteration:
                        alive.remove(g_)

        def step(gen_):
            if gen_ is None:
                return
            try:
                next(gen_)
            except StopIteration:
                pass

        def drain(gen_):
            if gen_ is None:
                return
            for _ in gen_:
                pass

        orderB = [(g, t) for g in range(4) for t in range(4)]
        RB(*orderB[0])
        RBt(*orderB[0])
        RB(*orderB[1])
        RBt(*orderB[1])
        drain(PB(*orderB[0]))
        for i_, (g, t) in enumerate(orderB):
            if i_ + 2 < len(orderB):
                RB(*orderB[i_ + 2])
            tg = tailB(g, t)
            pg = PB(*orderB[i_ + 1]) if i_ + 1 < len(orderB) else None
            step(tg)
            step(pg)
            step(tg)
            step(tg)
            step(tg)
            step(pg)
            drain(pg)
            drain(tg)
            if i_ + 2 < len(orderB):
                RBt(*orderB[i_ + 2])
            if t == 3:
                glevelB(g)

        S.barrier()
        A.release(persist_mark)

        if DBG:
            dump_m()
            A.release(persist_mark)
        if stop == "B":
            raise _Stop()

        h = A.alloc([128, NT, D], F32)
        e_mark = A.mark()
        KT = A.alloc([128, 8, 256], BF16)
        Vm = A.alloc([128, 2, D], BF16)
        x_mark = A.mark()
        Wkv = A.alloc([128, 8, 2048], BF16)
        gbc_K = A.alloc([128, D], F32)
        memt = A.alloc([128, 2, D], F32)
        xs_K = A.alloc([128, D], BF16)
        junk_K = A.alloc([128, D], BF16)
        mnT = A.alloc([128, 8, 256], BF16)
        load_w(Wkv, w_xa_kv, 8, "Wkv", "Wkv")
        DMA("sp", gbc_K, g_mem.partition_broadcast(128), [], ["gbc"], "gbc")
        DMA("sp", memt, memc.rearrange("(c p) d -> p c d", p=128), [], ["memt"], "memt")
        for mc in range(2):
            rmsnorm(memt[:, mc, :], "memt", gbc_K, xs_K, junk_K)
            transpose8(xs_K, "xs", mnT[:, :, mc * 128:(mc + 1) * 128], ("mnT", mc))
        mnr = [("mnT", 0), ("mnT", 1)]
        for c in range(8):
            b = 1 + (c % 2)
            for k in range(8):
                mm(bank(b)[:, 0:256], Wkv[:, k, c * 128:(c + 1) * 128], mnT[:, k, :], k == 0, k == 7, mnr + wres("Wkv", 8), [PR(b)])
            ACT(lambda e, b=b, c=c: e.copy(out=KT[:, c, :], in_=bank(b)[:, 0:256]), [PR(b)], ["KT"])
        for mc in range(2):
            for hf in range(2):
                b = 3 + ((mc * 2 + hf) % 2)
                for k in range(8):
                    mm(bank(b), mnT[:, k, mc * 128:(mc + 1) * 128], Wkv[:, k, 1024 + hf * 512:1024 + (hf + 1) * 512], k == 0, k == 7, mnr + wres("Wkv", 8), [PR(b)])
                DVE(lambda e, b=b, mc=mc, hf=hf: e.tensor_copy(out=Vm[:, mc, hf * 512:(hf + 1) * 512], in_=bank(b)), [PR(b)], ["Vm"])
        S.barrier()
        A.release(x_mark)

        Wmix = A.alloc([128, 8, D], BF16)
        Wq = A.alloc([128, 8, D], BF16)
        Wo = A.alloc([128, 8, D], BF16)
        Wr = A.alloc([128, 8, 36], BF16)
        gbc_xa = A.alloc([128, D], F32)
        gbc_moe = A.alloc([128, D], F32)
        brt = A.alloc([128, 36], F32)
        ebase2 = A.alloc([128, 2 * NE], F32)
        ebase = ebase2[:, 0:NE]
        ebaseY = ebase2[:, NE:2 * NE]
        carry = A.alloc([128, NE], F32)
        xt2_X = [A.alloc([128, D], F32) for _ in range(2)]
        xs_X = A.alloc([128, D], BF16)
        junk_X = A.alloc([128, D], BF16)
        xn2T = A.alloc([128, 8, 256], BF16)
        qT2 = A.alloc([128, 8, 256], BF16)
        Pb = A.alloc([128, 4, 256], BF16)
        PTx = A.alloc([128, 8, 128], BF16)
        obx = A.alloc([128, 4, 256], BF16)
        oTx = A.alloc([128, 8, 128], BF16)
        xn3 = [A.alloc([128, D], BF16) for _ in range(2)]
        xn3T = A.alloc([128, 8, 128], BF16)
        rs = A.alloc([128, 256], F32)
        Mb = A.alloc([128, NE], BF16)

        load_w(Wmix, w_mix_out, 8, "Wmix", "Wmix")
        load_w(Wq, w_xa_q, 8, "Wq", "Wq")
        load_w(Wo, w_xa_o, 8, "Wo", "Wo")
        load_w(Wr, w_rt, 8, "Wr", "Wr")
        DMA("sp", gbc_xa, g_xa.partition_broadcast(128), [], ["gbc_xa"], "gbcx1")
        DMA("sp", gbc_moe, g_moe.partition_broadcast(128), [], ["gbc_moe"], "gbcx2")
        DMA("sp", brt, b_rt.partition_broadcast(128), [], ["brt"], "gbcx3")
        DMA("sp", ebase2, c_eb, [], ["ebase"], "gbcx4")
        POOL(lambda e: e.memset(carry, 0.0), [], ["carry"])


        def rmsnorm2(xt_ap, xt_res, g_bc, g_res, xs_ap, xs_res, rt=False):
            o_ = 4 if rt else 0
            sfx = "_r" if rt else ""
            ssq_, std_, rstd_ = stat[:, o_:o_ + 1], stat[:, o_ + 1:o_ + 2], stat[:, o_ + 2:o_ + 3]
            jk = junk_X
            ACT(lambda e: e.activation(out=jk, in_=xt_ap, func=AF.Square, accum_out=ssq_), [xt_res], ["junk", "ssq" + sfx])
            ACT(lambda e: e.activation(out=std_, in_=ssq_, func=AF.Sqrt, bias=EPS, scale=1.0 / D), ["ssq" + sfx], ["std" + sfx])
            DVE(lambda e: e.reciprocal(out=rstd_, in_=std_), ["std" + sfx], ["rstd" + sfx])
            DVE(lambda e: e.scalar_tensor_tensor(out=xs_ap, in0=xt_ap, scalar=rstd_, in1=g_bc, op0=ALU.mult, op1=ALU.mult),
                [xt_res, "rstd" + sfx, g_res], [xs_res])

        LG = rs[:, 0:36]
        GMAX = rs[:, 36:37]
        NGM = rs[:, 37:38]
        GE = rs[:, 40:44]
        GSUM = rs[:, 44:45]
        PG = rs[:, 45:46]
        GM = rs[:, 48:52]
        PEN = rs[:, 52:56]
        ELM = rs[:, 64:96]
        M1V = rs[:, 96:97]
        M2V = rs[:, 97:98]
        DD = rs[:, 98:99]
        S2 = rs[:, 99:100]
        W1 = rs[:, 100:101]
        W2 = rs[:, 101:102]
        I1 = rs[:, 102:103]
        I2 = rs[:, 103:104]
        V1 = rs[:, 104:105]
        V2 = rs[:, 105:106]
        J1 = rs[:, 106:107]
        J2 = rs[:, 107:108]
        M1 = rs[:, 128:160]
        M2 = rs[:, 160:192]
        ELM2 = rs[:, 192:224]
        POSB = rs[:, 224:256]
        TMPR = A.alloc([128, NE], F32)
        POSR = A.alloc([128, NE], F32)
        POSY = A.alloc([128, NE], F32)

        def router(tile, bi):
            hres = ("h", tile)
            rmsnorm2(h[:, tile, :], hres, gbc_moe, "gbc_moe", xn3[bi], ("xn3", bi), rt=True)
            yield
            transpose8(xn3[bi], ("xn3", bi), xn3T, "xn3T")
            yield
            for k in range(8):
                mm(bank(7)[:, 0:36], xn3T[:, k, :], Wr[:, k, :], k == 0, k == 7, ["xn3T"] + wres("Wr", 8), [PR(7)])
            R = "rt"
            DVE(lambda e: e.tensor_tensor(out=LG, in0=bank(7)[:, 0:36], in1=brt, op=ALU.add), [PR(7), "brt"], [R])
            DVE(lambda e: e.reduce_max(out=GMAX, in_=LG[:, 0:4], axis=AX.X), [R], [R])
            DVE(lambda e: e.tensor_scalar(out=NGM, in0=GMAX, scalar1=-1.0, scalar2=None, op0=ALU.mult), [R], [R])
            ACT(lambda e: e.activation(out=GE, in_=LG[:, 0:4], func=AF.Exp, bias=NGM, scale=1.0, accum_out=GSUM), [R], [R])
            DVE(lambda e: e.reciprocal(out=PG, in_=GSUM), [R], [R])
            DVE(lambda e: e.tensor_scalar(out=GM, in0=LG[:, 0:4], scalar1=GMAX, scalar2=None, op0=ALU.is_equal), [R], [R])
            DVE(lambda e: e.tensor_scalar(out=PEN, in0=GM, scalar1=-1.0, scalar2=1e30, op0=ALU.add, op1=ALU.mult), [R], [R])
            DVE(lambda e: e.tensor_tensor(out=ELM.rearrange("p (g j) -> p g j", g=4), in0=LG[:, 4:36].rearrange("p (g j) -> p g j", g=4),
                                          in1=PEN.unsqueeze(2).broadcast_to([128, 4, 8]), op=ALU.add), [R], [R])
            yield
            DVE(lambda e: e.reduce_max(out=M1V, in_=ELM, axis=AX.X), [R], [R])
            DVE(lambda e: e.tensor_scalar(out=M1, in0=ELM, scalar1=M1V, scalar2=None, op0=ALU.is_equal), [R], [R])
            DVE(lambda e: e.scalar_tensor_tensor(out=ELM2, in0=M1, scalar=-1e30, in1=ELM, op0=ALU.mult, op1=ALU.add), [R], [R])
            DVE(lambda e: e.reduce_max(out=M2V, in_=ELM2, axis=AX.X), [R], [R])
            DVE(lambda e: e.tensor_scalar(out=M2, in0=ELM2, scalar1=M2V, scalar2=None, op0=ALU.is_equal), [R], [R])
            yield
            DVE(lambda e: e.tensor_tensor(out=DD, in0=M2V, in1=M1V, op=ALU.subtract), [R], [R])
            ACT(lambda e: e.activation(out=S2, in_=DD, func=AF.Sigmoid), [R], [R])
            DVE(lambda e: e.tensor_tensor(out=W2, in0=PG, in1=S2, op=ALU.mult), [R], [R])
            DVE(lambda e: e.tensor_tensor(out=W1, in0=PG, in1=W2, op=ALU.subtract), [R], [R])
            DVE(lambda e: e.tensor_tensor(out=Mb, in0=M1, in1=M2, op=ALU.add), [R], ["Mb"])
            mm(bank(7)[:, 64:96], ustrict, Mb, True, True, ["Mb", "ident"], [PR(7)])
            mm(bank(7)[:, 96:128], onesb, Mb, True, True, ["Mb", "ident"], [PR(7)])
            DVE(lambda e: e.tensor_tensor(out=POSR, in0=bank(7)[:, 64:96], in1=carry, op=ALU.add), [PR(7), "carry"], [R])
            DVE(lambda e: e.tensor_tensor(out=carry, in0=bank(7)[:, 96:128], in1=carry, op=ALU.add), [PR(7), "carry"], ["carry"])
            yield
            DVE(lambda e: e.scalar_tensor_tensor(out=POSB, in0=POSR, scalar=float(CAP), in1=ebase, op0=ALU.min, op1=ALU.add), [R, "ebase"], [R])
            DVE(lambda e: e.scalar_tensor_tensor(out=POSY, in0=POSR, scalar=float(CAP - 1), in1=ebaseY, op0=ALU.min, op1=ALU.add), [R, "ebase"], [R])
            for (Mx, Ix, Jx, Vx, Wx, col) in ((M1, I1, J1, V1, W1, 0), (M2, I2, J2, V2, W2, 1)):
                yield
                DVE(lambda e, Mx=Mx: e.tensor_tensor(out=TMPR, in0=Mx, in1=POSB, op=ALU.mult), [R], [R])
                DVE(lambda e, Ix=Ix: e.reduce_sum(out=Ix, in_=TMPR, axis=AX.X), [R], [R])
                DVE(lambda e, Mx=Mx: e.tensor_tensor(out=TMPR, in0=Mx, in1=POSR, op=ALU.mult), [R], [R])
                DVE(lambda e, Vx=Vx: e.reduce_sum(out=Vx, in_=TMPR, axis=AX.X), [R], [R])
                DVE(lambda e, Ix=Ix, col=col: e.tensor_copy(out=sidx[:, tile, col:col + 1], in_=Ix), [R], [("sidx", tile)])
                DVE(lambda e, Mx=Mx: e.tensor_tensor(out=TMPR, in0=Mx, in1=POSY, op=ALU.mult), [R], [R])
                DVE(lambda e, Jx=Jx: e.reduce_sum(out=Jx, in_=TMPR, axis=AX.X), [R], [R])
                DVE(lambda e, Jx=Jx, col=col: e.tensor_copy(out=gidx[:, tile, col:col + 1], in_=Jx), [R], [("gidx", tile)])
                DVE(lambda e, Vx=Vx, Wx=Wx, col=col: e.scalar_tensor_tensor(out=wts[:, tile, col:col + 1], in0=Vx, scalar=float(CAP), in1=Wx, op0=ALU.is_lt, op1=ALU.mult),
                    [R], [("wts", tile)])
            for col in range(2):
                S.add("pool", lambda e, col=col, bi=bi: e.indirect_dma_start(
                    out=XG, out_offset=bass.IndirectOffsetOnAxis(ap=sidx[:, tile, col:col + 1], axis=0), in_=xn3[bi], in_offset=None,
                    bounds_check=NE * CAPR - 1, oob_is_err=False),
                    [("xn3", bi), ("sidx", tile)], [("XG", tile, col)], dma=True, key="xgs%d" % bi, bsize=2)

        projX = [1, 2]
        xn2T2 = [xn2T, A.alloc([128, 8, 256], BF16)]
        qT22 = [qT2, A.alloc([128, 8, 256], BF16)]

        def interleaveX(*gens):
            alive = [g_ for g_ in gens if g_ is not None]
            while alive:
                for g_ in list(alive):
                    try:
                        next(g_)
                    except StopIteration:
                        alive.remove(g_)

        def S1_X(g):
            gp = g % 2
            for tl in range(2):
                tile = g * 2 + tl
                bi = tile % 2
                xb = xt2_X[bi]
                xr = ("xt", bi)
                DMA("sp", xb, xc[tile * 128:(tile + 1) * 128, :], [], [xr], "xt%d" % bi)
                for hf in range(2):
                    b = projX[hf]
                    for k in range(8):
                        mm(bank(b), mergedT[:, k, tile * 128:(tile + 1) * 128], Wmix[:, k, hf * 512:(hf + 1) * 512], k == 0, k == 7,
                           [("mT", k, tile // 4)] + wres("Wmix", 8), [PR(b)])
                    DVE(lambda e, b=b, hf=hf, tile=tile, xb=xb: e.tensor_tensor(out=h[:, tile, hf * 512:(hf + 1) * 512], in0=bank(b), in1=xb[:, hf * 512:(hf + 1) * 512], op=ALU.add),
                        [PR(b), xr], [("h", tile)])
                yield
                rmsnorm2(h[:, tile, :], ("h", tile), gbc_xa, "gbc_xa", xs_X, "xs")
                transpose8(xs_X, "xs", xn2T2[gp][:, :, tl * 128:(tl + 1) * 128], ("xn2T", gp, tl))
                yield

        def QT_X(g):
            gp = g % 2
            for c in range(8):
                b = projX[c % 2]
                for k in range(8):
                    mm(bank(b)[:, 0:256], Wq[:, k, c * 128:(c + 1) * 128], xn2T2[gp][:, k, :], k == 0, k == 7,
                       [("xn2T", gp, 0), ("xn2T", gp, 1)] + wres("Wq", 8), [PR(b)])
                ACT(lambda e, b=b, c=c, gp=gp: e.copy(out=qT22[gp][:, c, :], in_=bank(b)[:, 0:256]), [PR(b)], [("qT2", gp, c)])
                if c % 2 == 1:
                    yield

        def XA_X(tile):
            g, tl = tile // 2, tile % 2
            gp = g % 2
            sc = bank(3, 2).rearrange("p (h m) -> p h m", h=4)
            for hh in range(4):
                for kc in range(2):
                    mm(ps_t[:, 3 * 512 + hh * 256: 3 * 512 + (hh + 1) * 256], qT22[gp][:, 2 * hh + kc, tl * 128:(tl + 1) * 128], KT[:, 2 * hh + kc, :], kc == 0, kc == 1,
                       [("qT2", gp, 2 * hh + kc), "KT"], [PR(3 + hh // 2)])
            DVE(lambda e, sc=sc: e.reduce_max(out=stat[:, 8:12], in_=sc, axis=AX.X), [PR(3), PR(4)], ["xmx"])
            DVE(lambda e: e.tensor_scalar(out=stat[:, 12:16], in0=stat[:, 8:12], scalar1=-1.0 / 16, scalar2=None, op0=ALU.mult), ["xmx"], ["xnb"])
            for hh in range(4):
                ACT(lambda e, hh=hh: e.activation(out=Pb[:, hh, :], in_=ps_t[:, 3 * 512 + hh * 256: 3 * 512 + (hh + 1) * 256], func=AF.Exp,
                                                  bias=stat[:, 12 + hh:13 + hh], scale=1.0 / 16, accum_out=stat[:, 16 + hh:17 + hh]),
                    [PR(3 + hh // 2), "xnb"], [("Pb", hh), ("xsum", hh)])
            DVE(lambda e: e.reciprocal(out=stat[:, 20:24], in_=stat[:, 16:20]), [("xsum", hh) for hh in range(4)], ["xrs"])
            yield
            pb = bank_bf(0)
            Pv = Pb.rearrange("p h m -> p (h m)")
            for j in range(8):
                tp(pb[:, j, :], Pv[:, j * 128:(j + 1) * 128], [("Pb", j // 2)], [PR(0)])
            ACT(lambda e: e.copy(out=PTx, in_=bank_bf(0)), [PR(0)], ["PTx"])
            yield
            for hh in range(4):
                for mc in range(2):
                    mm(ps_t[:, 5 * 512 + hh * 256: 5 * 512 + (hh + 1) * 256], PTx[:, 2 * hh + mc, :], Vm[:, mc, hh * 256:(hh + 1) * 256], mc == 0, mc == 1,
                       ["PTx", "Vm"], [PR(5 + hh // 2)])
            DVE(lambda e: e.tensor_tensor(out=obx, in0=bank(5, 2).rearrange("p (h m) -> p h m", h=4),
                                          in1=stat[:, 20:24].unsqueeze(2).broadcast_to([128, 4, 256]), op=ALU.mult),
                [PR(5), PR(6), "xrs"], ["obx"])
            yield
            transpose8(obx.rearrange("p h m -> p (h m)"), "obx", oTx, "oTx")
            yield
            for hf in range(2):
                b = projX[hf]
                for k in range(8):
                    mm(bank(b), oTx[:, k, :], Wo[:, k, hf * 512:(hf + 1) * 512], k == 0, k == 7, ["oTx"] + wres("Wo", 8), [PR(b)])
                DVE(lambda e, b=b, hf=hf, tile=tile: e.tensor_tensor(out=h[:, tile, hf * 512:(hf + 1) * 512], in0=bank(b), in1=h[:, tile, hf * 512:(hf + 1) * 512], op=ALU.add),
                    [PR(b), ("h", tile)], [("h", tile)])
            yield

        interleaveX(S1_X(0))
        interleaveX(QT_X(0))
        pending = None
        def takeX(gen_, n):
            for _ in range(n):
                try:
                    next(gen_)
                except StopIteration:
                    return
                yield

        for g in range(8):
            s1n = S1_X(g + 1) if g + 1 < 8 else None
            interleaveX(XA_X(2 * g), pending, takeX(s1n, 2) if s1n is not None else None)
            interleaveX(XA_X(2 * g + 1), router(2 * g, 0), s1n)
            if g + 1 < 8:
                interleaveX(QT_X(g + 1))
            pending = router(2 * g + 1, 1)
        interleaveX(pending)

        S.barrier()
        A.release(e_mark)
        A.limit = ARENA_BYTES

        if DBG:
            for tile in range(NT):
                DMA("sp", dbg_h[:, tile * D:(tile + 1) * D], h[:, tile, :], [("h", tile)], [("dbgh", tile)], "dbgh")
            S.barrier()
        if stop in ("X", "XNR", "X1"):
            raise _Stop()

        NR = 4
        Wg_r = [A.alloc([128, 8, 512], BF16) for _ in range(NR)]
        Wu_r = [A.alloc([128, 8, 512], BF16) for _ in range(NR)]
        Wd_r = [A.alloc([128, 4, D], BF16) for _ in range(NR)]
        xg4 = [A.alloc([128, 2, D], BF16) for _ in range(4)]
        xgT2 = [A.alloc([128, 8, 256], BF16) for _ in range(2)]
        hid2 = [A.alloc([128, 4, 256], BF16) for _ in range(2)]
        sge2 = [A.alloc([128, 256], F32) for _ in range(2)]
        ysb2 = [A.alloc([128, 2, D], BF16) for _ in range(2)]

        allxg = [("XG", tile, col) for tile in range(NT) for col in range(2)]
        gb_i = [0]
        def TG_E(ex):
            s = ex % NR
            bi = ex % 2
            DMA("pool", Wg_r[s], w_gate[ex].rearrange("(k p) n -> p k n", p=128), [], [("Wg", s, k) for k in range(8)], "wg%d" % s)
            DMA("pool", Wu_r[s], w_up[ex].rearrange("(k p) n -> p k n", p=128), [], [("Wu", s, k) for k in range(8)], "wu%d" % s)
            DMA("pool", Wd_r[s], w_down[ex].rearrange("(k p) n -> p k n", p=128), [], [("Wd", s, k) for k in range(4)], "wd%d" % s)
            for rb in range(2):
                pb = bank_bf(0)
                for k in range(8):
                    tp(pb[:, k, :], xg4[ex % 4][:, rb, k * 128:(k + 1) * 128], [("xg", ex % 4)], [PR(0)])
                if rb == 0:
                    ACT(lambda e, bi=bi, rb=rb: e.copy(out=xgT2[bi][:, :, rb * 128:(rb + 1) * 128], in_=bank_bf(0)), [PR(0)], [("xgT", bi, rb)])
                else:
                    DVE(lambda e, bi=bi, rb=rb: e.tensor_copy(out=xgT2[bi][:, :, rb * 128:(rb + 1) * 128], in_=bank_bf(0)), [PR(0)], [("xgT", bi, rb)])
            xgr = [("xgT", bi, 0), ("xgT", bi, 1)]
            for hc in range(4):
                gbk = 1 + (gb_i[0] % 2)
                ubk = 3 + (gb_i[0] % 2)
                gb_i[0] += 1
                for k in range(8):
                    mm(bank(gbk)[:, 0:256], Wg_r[s][:, k, hc * 128:(hc + 1) * 128], xgT2[bi][:, k, :], k == 0, k == 7, xgr + [("Wg", s, kk) for kk in range(8)], [PR(gbk)])
                for k in range(8):
                    mm(bank(ubk)[:, 0:256], Wu_r[s][:, k, hc * 128:(hc + 1) * 128], xgT2[bi][:, k, :], k == 0, k == 7, xgr + [("Wu", s, kk) for kk in range(8)], [PR(ubk)])
                sgb = sge2[hc % 2]
                ACT(lambda e, gbk=gbk, sgb=sgb: e.activation(out=sgb, in_=bank(gbk)[:, 0:256], func=AF.Sigmoid), [PR(gbk)], [("sge", hc % 2)])
                DVE(lambda e, gbk=gbk, sgb=sgb: e.tensor_tensor(out=sgb, in0=bank(gbk)[:, 0:256], in1=sgb, op=ALU.mult),
                    [PR(gbk), ("sge", hc % 2)], [("sge", hc % 2)])
                DVE(lambda e, ubk=ubk, sgb=sgb, bi=bi, hc=hc: e.tensor_tensor(out=hid2[bi][:, hc, :], in0=bank(ubk)[:, 0:256], in1=sgb, op=ALU.mult),
                    [PR(ubk), ("sge", hc % 2)], [("hid", bi, hc)])

        def DN_E(ex):
            s = ex % NR
            bi = ex % 2
            hr = [("hid", bi, hc) for hc in range(4)]
            for rb in range(2):
                for hf in range(2):
                    yb = 5 + ((rb * 2 + hf) % 3)
                    for hc in range(4):
                        mm(bank(yb), hid2[bi][:, hc, rb * 128:(rb + 1) * 128], Wd_r[s][:, hc, hf * 512:(hf + 1) * 512], hc == 0, hc == 3,
                           hr + [("Wd", s, kk) for kk in range(4)], [PR(yb)])
                    if hf == 0:
                        ACT(lambda e, yb=yb, bi=bi, rb=rb, hf=hf: e.copy(out=ysb2[bi][:, rb, hf * 512:(hf + 1) * 512], in_=bank(yb)), [PR(yb)], [("ysb", bi, rb, hf)])
                    else:
                        DVE(lambda e, yb=yb, bi=bi, rb=rb, hf=hf: e.tensor_copy(out=ysb2[bi][:, rb, hf * 512:(hf + 1) * 512], in_=bank(yb)), [PR(yb)], [("ysb", bi, rb, hf)])
            DMA("sp", YG[ex * CAP:(ex + 1) * CAP, :].rearrange("(b p) d -> p b d", p=128), ysb2[bi],
                [("ysb", bi, rb, hf) for rb in range(2) for hf in range(2)], [("YG", ex)], "yg%d" % bi)


        def XL_E(ex):
            DMA("sp", xg4[ex % 4], XG[ex * CAPR:ex * CAPR + CAP, :].rearrange("(b p) d -> p b d", p=128), allxg if ex < 4 else [], [("xg", ex % 4)], "xg%d" % (ex % 4))

        for ex in range(4):
            XL_E(ex)
        TG_E(0)
        for ex in range(NE):
            if ex + 4 < NE:
                XL_E(ex + 4)
            if ex + 1 < NE:
                TG_E(ex + 1)
            DN_E(ex)

        S.barrier()
        A.release(e_mark)

        gbc_C = A.alloc([128, D], F32)
        y12 = [[A.alloc([128, D], BF16) for _ in range(2)] for _ in range(2)]
        ot2 = [A.alloc([128, D], F32) for _ in range(2)]
        junk_C = A.alloc([128, D], BF16)
        DMA("sp", gbc_C, g_fin.partition_broadcast(128), [], ["gbc"], "gbc")
        outres = []

        def c_s1(tile):
            bi = tile % 2
            o_ = 24 + 3 * bi
            for col in range(2):
                S.add("pool", lambda e, col=col, bi=bi, tile=tile: e.indirect_dma_start(
                    out=y12[bi][col], out_offset=None, in_=YG, in_offset=bass.IndirectOffsetOnAxis(ap=gidx[:, tile, col:col + 1], axis=0)),
                    [("gidx", tile)], [("y12", bi, col)], dma=True, key="yga%d%d" % (bi, col))
            for col in range(2):
                DVE(lambda e, col=col, bi=bi, tile=tile: e.scalar_tensor_tensor(out=h[:, tile, :], in0=y12[bi][col], scalar=wts[:, tile, col:col + 1], in1=h[:, tile, :],
                                                                             op0=ALU.mult, op1=ALU.add),
                    [("y12", bi, col), ("wts", tile), ("h", tile)], [("h", tile)])
            ACT(lambda e, tile=tile, o_=o_: e.activation(out=junk_C, in_=h[:, tile, :], func=AF.Square, accum_out=stat[:, o_:o_ + 1]), [("h", tile)], ["junk", ("ssqC", bi)])
            ACT(lambda e, o_=o_: e.activation(out=stat[:, o_ + 1:o_ + 2], in_=stat[:, o_:o_ + 1], func=AF.Sqrt, bias=EPS, scale=1.0 / D), [("ssqC", bi)], [("stdC", bi)])

        def c_s2(tile):
            bi = tile % 2
            o_ = 24 + 3 * bi
            DVE(lambda e, o_=o_: e.reciprocal(out=stat[:, o_ + 2:o_ + 3], in_=stat[:, o_ + 1:o_ + 2]), [("stdC", bi)], [("rstdC", bi)])
            DVE(lambda e, tile=tile, bi=bi, o_=o_: e.scalar_tensor_tensor(out=ot2[bi], in0=h[:, tile, :], scalar=stat[:, o_ + 2:o_ + 3], in1=gbc_C, op0=ALU.mult, op1=ALU.mult),
                [("h", tile), ("rstdC", bi), "gbc"], [("ot", bi)])
            DMA("sp", out[tile * 128:(tile + 1) * 128, :], ot2[bi], [("ot", bi)], [("out", tile)], "out%d" % bi)
            outres.append(("out", tile))

        c_s1(0)
        for tile in range(NT):
            if tile + 1 < NT:
                c_s1(tile + 1)
            c_s2(tile)
        S.add("sp", None, outres)
        S.barrier()


    try:
        phases()
    except _Stop:
        S.barrier()

    S.resolve()
    sems = {}
    for e in ("pe", "act", "dve", "pool"):
        sems[("eng", e)] = es.enter_context(nc.semaphore("s_" + e))
    for k in S.keys:
        sems[("dma", k)] = es.enter_context(nc.semaphore("d_" + str(k)))
    with nc.Block() as block:
        block.sync(lambda e: S.run_engine("sp", e, sems))
        block.scalar(lambda e: S.run_engine("act", e, sems))
        block.vector(lambda e: S.run_engine("dve", e, sems))
        block.gpsimd(lambda e: S.run_engine("pool", e, sems))
        block.tensor(lambda e: S.run_engine("pe", e, sems))
    es.close()
    return nc, S, A


def _consts(half):
    p = np.arange(128, dtype=np.float64)
    inv_freq = 10000.0 ** (-np.arange(0, 64, 2, dtype=np.float64) / 64)

    def cs_tab(base):
        pos = base + np.arange(NT)[None, :] * 128 + p[:, None]
        ang = (pos[:, :, None].astype(np.float32) * inv_freq[None, None, :].astype(np.float32)).astype(np.float32)
        return np.stack([np.cos(ang), np.sin(ang)], axis=1).astype(np.float32)

    gam = 1.0 - 2.0 ** (-5.0 - np.arange(8, dtype=np.float64))
    lg = np.log(gam)
    gq = np.zeros((128, 4, 128), np.float32)
    gk = np.zeros((128, 4, 128), np.float32)
    ct = np.zeros((128, 4, 128), np.float32)
    i = np.arange(128, dtype=np.float64)
    for c in range(4):
        for hl in range(2):
            h = 2 * c + hl
            gq[hl * 64:(hl + 1) * 64, c, :] = np.exp((i + 1) * lg[h])[None, :]
            gk[hl * 64:(hl + 1) * 64, c, :] = (np.exp(-(i + 1) * lg[h]) / 8.0)[None, :]
            ct[hl * 64:(hl + 1) * 64, c, :] = np.exp(128 * lg[h])
    zt = (np.exp((127 - p)[:, None] * lg[None, :]) / 8.0).astype(np.float32)
    mask = (np.arange(128)[None, :] >= np.arange(128)[:, None]).astype(np.float32)
    ident = np.eye(128, dtype=np.float32)
    ustrict = (np.arange(128)[:, None] < np.arange(128)[None, :]).astype(np.float32)
    ones = np.ones((128, 128), np.float32)
    c_bf = np.concatenate([ident, ustrict, ones], axis=1)
    eb = np.concatenate([np.tile((np.arange(NE, dtype=np.float32) * CAPR)[None, :], (128, 1)),
                         np.tile((np.arange(NE, dtype=np.float32) * CAP)[None, :], (128, 1))], axis=1)
    return {
        "c_bf": c_bf, "c_cs_own": cs_tab(half * TOK), "c_cs_pre": cs_tab(0.0),
        "c_gq": gq, "c_gk": gk, "c_zt": zt, "c_ct": ct, "c_mask": np.ascontiguousarray(np.tile(mask[:, None, :], (1, 4, 1))), "c_eb": eb,
    }


_CACHE = {}


def kernel(x, mem, mix_norm_g, w_in, conv_w, w_conv_out, w_ret_out, w_mix_out,
           xa_norm_g, mem_norm_g, w_xa_q, w_xa_kv, w_xa_o, moe_norm_g,
           w_group, b_group, w_router, b_router, w_gate, w_up, w_down, final_norm_g):
    f = lambda a: np.ascontiguousarray(np.asarray(a, dtype=np.float32))
    x = f(x)
    mem = f(mem)
    if "nc" not in _CACHE:
        _CACHE["nc"] = build_program()
    nc = _CACHE["nc"][0]
    shared = {
        "w_in": f(w_in)[0], "conv_wT": np.ascontiguousarray(f(conv_w)[0].T), "w_conv_out": f(w_conv_out)[0],
        "w_ret_out": f(w_ret_out)[0], "w_mix_out": f(w_mix_out)[0], "w_xa_q": f(w_xa_q)[0], "w_xa_kv": f(w_xa_kv)[0],
        "w_xa_o": f(w_xa_o)[0], "g_mix": f(mix_norm_g)[0], "g_xa": f(xa_norm_g)[0], "g_mem": f(mem_norm_g)[0],
        "g_moe": f(moe_norm_g)[0], "g_fin": f(final_norm_g),
        "w_rt": np.ascontiguousarray(np.concatenate([f(w_group)[0], f(w_router)[0]], axis=1)),
        "b_rt": np.ascontiguousarray(np.concatenate([f(b_group)[0], f(b_router)[0]], axis=0)),
        "w_gate": f(w_gate)[0], "w_up": f(w_up)[0], "w_down": f(w_down)[0],
    }
    zeros = np.zeros((TOK, D), np.float32)
    in_maps = []
    for c in range(8):
        b, half = c // 2, c % 2
        m = dict(shared)
        m["xc"] = np.ascontiguousarray(x[b, half * TOK:(half + 1) * TOK])
        m["xp"] = np.ascontiguousarray(x[b, 0:TOK]) if half == 1 else zeros
        m["memc"] = np.ascontiguousarray(mem[b])
        m.update(_consts(half))
        in_maps.append(m)
    res = run_bass_kernel_spmd(nc, in_maps, core_ids=list(range(8)))
    _CACHE["res"] = res
    outp = np.empty((4, 2 * TOK, D), np.float32)
    for c in range(8):
        b, half = c // 2, c % 2
        outp[b, half * TOK:(half + 1) * TOK] = res.results[c]["out"]
    return outp
```
